# Optimizing a Trainium2 kernel written in Bass

```python
import math
import jax, jax.numpy as jnp
from jax import lax
import numpy as np

D_MODEL = 1024
BATCH = 8
SEQ = 4096
DEPTH = 1

MEM_LEN = 256
EPS = 1e-6

LRU_WIDTH = 512
LRU_BLOCKS = 8
LRU_BLOCK = LRU_WIDTH // LRU_BLOCKS
CONV_WIDTH = 4
LRU_C = 8.0

MLA_HEADS = 8
Q_LORA = 256
KV_LORA = 128
QK_NOPE = 64
QK_ROPE = 32
V_HEAD = 64
MLA_WIDTH = MLA_HEADS * V_HEAD
ROPE_THETA = 10000.0
Q_BLOCK = 128

MEM_HEADS = 4
MEM_HEAD_DIM = 128
MEM_WIDTH = MEM_HEADS * MEM_HEAD_DIM

N_BRANCH = 3
BRANCH_WIDTH = 512

N_GROUPS = 4
EXPERTS_PER_GROUP = 8
N_EXPERTS = N_GROUPS * EXPERTS_PER_GROUP
TOP_K = 2
D_EXPERT = 256

IN_SIZES = (LRU_WIDTH, LRU_WIDTH, Q_LORA, KV_LORA + QK_ROPE, MEM_WIDTH, N_BRANCH * D_MODEL)
D_IN = LRU_WIDTH * 2 + Q_LORA + KV_LORA + QK_ROPE + MEM_WIDTH + N_BRANCH * D_MODEL

kernel_name = "hybrid_rglru_mla_memxattn_hiermoe"


def _split_points(sizes):
    pts, acc = [], 0
    for s in sizes[:-1]:
        acc += s
        pts.append(acc)
    return pts


def rmsnorm(x, g):
    xf = x.astype(jnp.float32)
    y = xf * lax.rsqrt(jnp.mean(xf * xf, axis=-1, keepdims=True) + EPS)
    return (y * g.astype(jnp.float32)).astype(x.dtype)


def rope_angles(positions):
    inv_freq = ROPE_THETA ** (-jnp.arange(0, QK_ROPE, 2, dtype=jnp.float32) / QK_ROPE)
    ang = positions.astype(jnp.float32)[..., None] * inv_freq
    return jnp.cos(ang), jnp.sin(ang)


def apply_rope(t, cos, sin):
    tf = t.astype(jnp.float32)
    half = tf.shape[-1] // 2
    t1, t2 = tf[..., :half], tf[..., half:]
    return jnp.concatenate([t1 * cos - t2 * sin, t2 * cos + t1 * sin], axis=-1).astype(t.dtype)


def causal_depthwise_conv(x, w, b):
    R = x.shape[-1]
    y = lax.conv_general_dilated(
        x, w.astype(x.dtype)[:, None, :], window_strides=(1,), padding=[(CONV_WIDTH - 1, 0)],
        dimension_numbers=('NWC', 'WIO', 'NWC'), feature_group_count=R)
    return y + b.astype(x.dtype)


def rg_lru(xc, w_a, b_a, w_x, b_x, lam):
    B, S, R = xc.shape
    xb = xc.reshape(B, S, LRU_BLOCKS, LRU_BLOCK)
    gate_a = jnp.einsum('bsnc,ncd->bsnd', xb, w_a).reshape(B, S, R) + b_a
    gate_x = jnp.einsum('bsnc,ncd->bsnd', xb, w_x).reshape(B, S, R) + b_x
    r = jax.nn.sigmoid(gate_a.astype(jnp.float32))
    i = jax.nn.sigmoid(gate_x.astype(jnp.float32))
    log_a = -LRU_C * r * jax.nn.softplus(-lam.astype(jnp.float32))
    a = jnp.exp(log_a)
    u = jnp.sqrt(-jnp.expm1(2.0 * log_a)) * (i * xc.astype(jnp.float32))

    def combine(left, right):
        a_l, b_l = left
        a_r, b_r = right
        return a_l * a_r, a_r * b_l + b_r

    _, h = lax.associative_scan(combine, (a, u), axis=1)
    return h.astype(xc.dtype)


def mla_attention(q_nope, q_rope, k_nope, k_rope, v):
    B, S, H, _ = q_nope.shape
    nb = S // Q_BLOCK
    scale = 1.0 / math.sqrt(QK_NOPE + QK_ROPE)

    def to_blocks(t):
        return jnp.moveaxis(t.reshape((B, nb, Q_BLOCK) + t.shape[2:]), 1, 0)

    k_pos = jnp.arange(S)

    def one_block(args):
        qn, qr, start = args
        s = (jnp.einsum('bqhd,bkhd->bhqk', qn, k_nope, preferred_element_type=jnp.float32)
             + jnp.einsum('bqhr,bkr->bhqk', qr, k_rope, preferred_element_type=jnp.float32)) * scale
        q_pos = start + jnp.arange(Q_BLOCK)
        s = jnp.where(k_pos[None, :] <= q_pos[:, None], s, -jnp.inf)
        p = jax.nn.softmax(s, axis=-1).astype(v.dtype)
        return jnp.einsum('bhqk,bkhd->bqhd', p, v)

    starts = jnp.arange(nb) * Q_BLOCK
    out = lax.map(one_block, (to_blocks(q_nope), to_blocks(q_rope), starts))
    return jnp.moveaxis(out, 0, 1).reshape(B, S, H * V_HEAD)


def memory_attention(q, mem_n, w_kv):
    B, S, _ = q.shape
    M = mem_n.shape[1]
    qh = q.reshape(B, S, MEM_HEADS, MEM_HEAD_DIM)
    k, v = jnp.split(mem_n @ w_kv, 2, axis=-1)
    k = k.reshape(B, M, MEM_HEADS, MEM_HEAD_DIM)
    v = v.reshape(B, M, MEM_HEADS, MEM_HEAD_DIM)
    s = jnp.einsum('bshd,bmhd->bhsm', qh, k, preferred_element_type=jnp.float32) / math.sqrt(MEM_HEAD_DIM)
    p = jax.nn.softmax(s, axis=-1).astype(v.dtype)
    return jnp.einsum('bhsm,bmhd->bshd', p, v).reshape(B, S, MEM_WIDTH)


def hier_moe(n2, w_group, b_group, w_router, b_router, w_e_gate, w_e_up, w_e_down):
    B, S, D = n2.shape
    t = n2.reshape(B * S, D)
    g_logits = (t @ w_group).astype(jnp.float32) + b_group.astype(jnp.float32)
    g_prob = jax.nn.softmax(g_logits, axis=-1)
    g_w, g_idx = lax.top_k(g_prob, 1)
    e_logits = ((t @ w_router).astype(jnp.float32) + b_router.astype(jnp.float32)).reshape(-1, N_GROUPS, EXPERTS_PER_GROUP)
    e_in_group = jnp.take_along_axis(e_logits, g_idx[:, :, None], axis=1)[:, 0]
    top_vals, top_idx = lax.top_k(e_in_group, TOP_K)
    top_w = jax.nn.softmax(top_vals, axis=-1) * g_w
    expert_id = g_idx * EXPERTS_PER_GROUP + top_idx
    combine = jnp.einsum('tk,tke->te', top_w, jax.nn.one_hot(expert_id, N_EXPERTS, dtype=jnp.float32)).astype(t.dtype)
    y = jnp.zeros_like(t)
    for e in range(N_EXPERTS):
        hidden = jax.nn.silu(t @ w_e_gate[e]) * (t @ w_e_up[e])
        y = y + combine[:, e:e + 1] * (hidden @ w_e_down[e])
    return y.reshape(B, S, D)


def setup_inputs(seed: int = 0) -> dict:
    key = jax.random.key(seed)
    ks = iter(jax.random.split(key, 40))
    f32 = jnp.float32

    def nrm(shape, fan_in):
        return jax.random.normal(next(ks), shape, f32) * (fan_in ** -0.5)

    def gain(shape):
        return 1.0 + 0.02 * jax.random.normal(next(ks), shape, f32)

    def bias(shape, scale=0.02):
        return scale * jax.random.normal(next(ks), shape, f32)

    x = jax.random.normal(next(ks), (BATCH, SEQ, D_MODEL), f32)
    mem = jax.random.normal(next(ks), (BATCH, MEM_LEN, D_MODEL), f32)
    offsets = jax.random.randint(next(ks), (BATCH, 1), 0, 1024, dtype=jnp.int32)
    positions = offsets + jnp.arange(SEQ, dtype=jnp.int32)[None, :]

    u = jax.random.uniform(next(ks), (DEPTH, LRU_WIDTH), f32, 0.9, 0.999)
    a0 = u ** (1.0 / LRU_C)
    lru_lambda = jnp.log(a0) - jnp.log1p(-a0)

    return {
        "x": x,
        "mem": mem,
        "positions": positions,
        "g_mix": gain((DEPTH, D_MODEL)),
        "w_in": nrm((DEPTH, D_MODEL, D_IN), D_MODEL),
        "conv_w": nrm((DEPTH, CONV_WIDTH, LRU_WIDTH), CONV_WIDTH),
        "conv_b": bias((DEPTH, LRU_WIDTH)),
        "lru_wa": nrm((DEPTH, LRU_BLOCKS, LRU_BLOCK, LRU_BLOCK), LRU_BLOCK),
        "lru_ba": bias((DEPTH, LRU_WIDTH)),
        "lru_wx": nrm((DEPTH, LRU_BLOCKS, LRU_BLOCK, LRU_BLOCK), LRU_BLOCK),
        "lru_bx": bias((DEPTH, LRU_WIDTH)),
        "lru_lambda": lru_lambda,
        "g_q": gain((DEPTH, Q_LORA)),
        "w_uq": nrm((DEPTH, Q_LORA, MLA_HEADS * (QK_NOPE + QK_ROPE)), Q_LORA),
        "g_kv": gain((DEPTH, KV_LORA)),
        "w_ukv": nrm((DEPTH, KV_LORA, MLA_HEADS * (QK_NOPE + V_HEAD)), KV_LORA),
        "g_mem": gain((DEPTH, D_MODEL)),
        "w_mem_kv": nrm((DEPTH, D_MODEL, 2 * MEM_WIDTH), D_MODEL),
        "w_branch": nrm((DEPTH, N_BRANCH, BRANCH_WIDTH, D_MODEL), BRANCH_WIDTH),
        "w_o": nrm((DEPTH, D_MODEL, D_MODEL), D_MODEL),
        "g_ffn": gain((DEPTH, D_MODEL)),
        "w_group": nrm((DEPTH, D_MODEL, N_GROUPS), D_MODEL),
        "b_group": bias((DEPTH, N_GROUPS), 0.01),
        "w_router": nrm((DEPTH, D_MODEL, N_EXPERTS), D_MODEL),
        "b_router": bias((DEPTH, N_EXPERTS), 0.01),
        "w_e_gate": nrm((DEPTH, N_EXPERTS, D_MODEL, D_EXPERT), D_MODEL),
        "w_e_up": nrm((DEPTH, N_EXPERTS, D_MODEL, D_EXPERT), D_MODEL),
        "w_e_down": nrm((DEPTH, N_EXPERTS, D_EXPERT, D_MODEL), D_EXPERT),
        "g_final": gain((D_MODEL,)),
    }


def reference(x, mem, positions, g_mix, w_in, conv_w, conv_b, lru_wa, lru_ba, lru_wx, lru_bx, lru_lambda,
              g_q, w_uq, g_kv, w_ukv, g_mem, w_mem_kv, w_branch, w_o, g_ffn, w_group, b_group,
              w_router, b_router, w_e_gate, w_e_up, w_e_down, g_final):
    B, S, D = x.shape
    cos, sin = rope_angles(positions)
    split_pts = _split_points(IN_SIZES)
    h = x
    for l in range(DEPTH):
        n = rmsnorm(h, g_mix[l])
        proj = n @ w_in[l]
        x_lru, gate_lru, c_q, kv_a, q_mem, gate_logits = jnp.split(proj, split_pts, axis=-1)

        xa = causal_depthwise_conv(x_lru, conv_w[l], conv_b[l])
        y_a = rg_lru(xa, lru_wa[l], lru_ba[l], lru_wx[l], lru_bx[l], lru_lambda[l]) * jax.nn.gelu(gate_lru)

        cq = rmsnorm(c_q, g_q[l])
        q = (cq @ w_uq[l]).reshape(B, S, MLA_HEADS, QK_NOPE + QK_ROPE)
        q_nope = q[..., :QK_NOPE]
        q_rope = apply_rope(q[..., QK_NOPE:], cos[:, :, None, :], sin[:, :, None, :])
        ckv = rmsnorm(kv_a[..., :KV_LORA], g_kv[l])
        k_rope = apply_rope(kv_a[..., KV_LORA:], cos, sin)
        kv = (ckv @ w_ukv[l]).reshape(B, S, MLA_HEADS, QK_NOPE + V_HEAD)
        y_b = mla_attention(q_nope, q_rope, kv[..., :QK_NOPE], k_rope, kv[..., QK_NOPE:])

        mem_n = rmsnorm(mem, g_mem[l])
        y_c = memory_attention(q_mem, mem_n, w_mem_kv[l])

        gl = gate_logits.reshape(B, S, N_BRANCH, D)
        merged = (jax.nn.sigmoid(gl[:, :, 0].astype(jnp.float32)).astype(h.dtype) * (y_a @ w_branch[l, 0])
                  + jax.nn.sigmoid(gl[:, :, 1].astype(jnp.float32)).astype(h.dtype) * (y_b @ w_branch[l, 1])
                  + jax.nn.sigmoid(gl[:, :, 2].astype(jnp.float32)).astype(h.dtype) * (y_c @ w_branch[l, 2]))
        h = h + merged @ w_o[l]

        n2 = rmsnorm(h, g_ffn[l])
        h = h + hier_moe(n2, w_group[l], b_group[l], w_router[l], b_router[l],
                         w_e_gate[l], w_e_up[l], w_e_down[l])
    return rmsnorm(h, g_final)
```

```python
import math
import numpy as np
from contextlib import ExitStack
import concourse.bass as bass
import concourse.mybir as mybir
from concourse.bass_utils import run_bass_kernel_spmd

F32 = mybir.dt.float32
BF16 = mybir.dt.bfloat16
I32 = mybir.dt.int32
AF = mybir.ActivationFunctionType
ALU = mybir.AluOpType
AX = mybir.AxisListType

S = 4096
D = 1024
NT = S // 128
NB = S // 512
EPS = 1e-6
DIN = 5024
NE = 32
NHALF = 2


class Trk:
    ENG = ("pe", "act", "dve", "pool", "sp")
    SELF = True
    STEP = 0
    FINAL = None

    def __init__(self, nc, stack, n_dma_sems=12):
        self.nc = nc
        self.prog = {e: [] for e in self.ENG}
        self.sem = {e: stack.enter_context(nc.semaphore("sem_" + e)) for e in self.ENG}
        self.nops = {e: 0 for e in self.ENG}
        self.waited = {e: set() for e in self.ENG}
        self.known_c = {e: {e2: 0 for e2 in self.ENG} for e in self.ENG}
        self.known_d = {e: {} for e in self.ENG}
        self.dsem = {}
        self.drr = {}
        self.dcnt = {}
        self.dobj = {}
        for q in ("sp", "pool", "act"):
            self.dsem[q] = [stack.enter_context(nc.semaphore("dsem_%s_%d" % (q, i))) for i in range(n_dma_sems)]
            self.drr[q] = 0
            for s in self.dsem[q]:
                self.dcnt[id(s)] = 0
                self.dobj[id(s)] = s
        self.tiles = {}
        self.mute = False

    def _st(self, k):
        st = self.tiles.get(k)
        if st is None:
            st = {"w": None, "r": []}
            self.tiles[k] = st
        return st

    def _deps(self, reads, writes):
        evs = []
        for k in reads:
            st = self._st(k)
            if st["w"] is not None:
                evs.append(st["w"])
            if k.startswith("ps"):
                evs.extend(st["r"])
        for k in writes:
            st = self._st(k)
            if st["w"] is not None:
                evs.append(st["w"])
            evs.extend(st["r"])
        return evs

    def _emit_waits(self, e, evs, is_dma=False):
        cmax = {}
        dmax = {}
        for ev in evs:
            if ev[0] == "c":
                _, e2, idx = ev
                if e2 == e and (e == "pe" or (not is_dma and not self.SELF)):
                    continue
                if cmax.get(e2, 0) < idx:
                    cmax[e2] = idx
            else:
                _, sid, v = ev
                if dmax.get(sid, 0) < v:
                    dmax[sid] = v
        for e2, idx in cmax.items():
            if self.known_c[e][e2] >= idx:
                continue
            k0 = self.known_c[e][e2]
            if self.STEP and e2 != e:
                for v in range(k0 + self.STEP, idx, self.STEP):
                    self.waited[e2].add(v)
                    self.prog[e].append(("wc", e2, v))
            self.known_c[e][e2] = idx
            self.waited[e2].add(idx)
            self.prog[e].append(("wc", e2, idx))
        for sid, v in dmax.items():
            if self.known_d[e].get(sid, 0) >= v:
                continue
            self.known_d[e][sid] = v
            self.prog[e].append(("wd", sid, v))

    def _mark(self, ev, reads, writes):
        for k in reads:
            r = self._st(k)["r"]
            r.append(ev)
            if len(r) > 24:
                best = {}
                for x in r:
                    key = (x[0], x[1])
                    if key not in best or best[key][2] < x[2]:
                        best[key] = x
                r[:] = list(best.values())
        for k in writes:
            st = self._st(k)
            st["w"] = ev
            st["r"] = []

    def op(self, e, fn, reads=(), writes=()):
        return self.group(e, [fn], reads, writes)

    def group(self, e, fns, reads=(), writes=()):
        if self.mute:
            return None
        self._emit_waits(e, self._deps(reads, writes))
        self.nops[e] += 1
        idx = self.nops[e]
        self.prog[e].append(("op", list(fns), idx))
        ev = ("c", e, idx)
        self._mark(ev, reads, writes)
        return ev

    def dma(self, q, out, in_, reads=(), writes=(), **kw):
        if self.mute:
            return None
        pool = self.dsem[q]
        s = pool[self.drr[q] % len(pool)]
        self.drr[q] += 1
        sid = id(s)
        evs = self._deps(reads, writes)
        if self.dcnt[sid] > 0:
            evs.append(("d", sid, self.dcnt[sid]))
        self._emit_waits(q, evs, is_dma=True)
        self.dcnt[sid] += 16
        ev = ("d", sid, self.dcnt[sid])
        self.prog[q].append(("dma", s, out, in_, kw))
        self._mark(ev, reads, writes)
        return ev

    def barrier(self, engines=None):
        if self.mute:
            return
        evs = [("c", e2, self.nops[e2]) for e2 in self.ENG if self.nops[e2] > 0]
        evs += [("d", sid, v) for sid, v in self.dcnt.items() if v > 0]
        for e in (engines or self.ENG):
            self._emit_waits(e, evs)
        self.tiles = {}

    def finish_early(self):
        self.barrier(self.FINAL)
        self.mute = True

    def finish(self, block):
        self.mute = False
        self.barrier(self.FINAL)
        rank = {e: {idx: i + 1 for i, idx in enumerate(sorted(self.waited[e]))} for e in self.ENG}
        t = self

        def run(e, en):
            for it in t.prog[e]:
                if it[0] == "wc":
                    en.wait_ge(t.sem[it[1]], rank[it[1]][it[2]])
                elif it[0] == "wd":
                    en.wait_ge(t.dobj[it[1]], it[2])
                elif it[0] == "dma":
                    en.dma_start(out=it[2], in_=it[3], **it[4]).then_inc(it[1], 16)
                else:
                    fns, idx = it[1], it[2]
                    for i, fn in enumerate(fns):
                        r = fn(en)
                        if i == len(fns) - 1 and idx in rank[e]:
                            r.then_inc(t.sem[e], 1)

        @block.sync
        def _(en):
            run("sp", en)

        @block.tensor
        def _(en):
            run("pe", en)

        @block.scalar
        def _(en):
            run("act", en)

        @block.vector
        def _(en):
            run("dve", en)

        @block.gpsimd
        def _(en):
            run("pool", en)


class Rot:
    def __init__(self, bufs, name, shared=False):
        self.bufs = bufs
        self.name = name
        self.i = 0
        self.shared = shared

    def next(self):
        j = self.i % len(self.bufs)
        self.i += 1
        return self.bufs[j], "%s#%d" % (self.name, 0 if self.shared else j)


def build_nc(dbg=False, stop=99, substop=99):
    EXP = ""
    nc = bass.Bass("TRN2", target_bir_lowering=False)

    def din(name, shape, dt=F32):
        return nc.dram_tensor(name, list(shape), dt, kind="ExternalInput").ap()

    def dscr(name, shape, dt):
        return nc.dram_tensor(name, list(shape), dt, kind=("ExternalOutput" if dbg else "Internal")).ap()

    x = din("x", [S, D])
    mem = din("mem", [256, D])
    pos = din("pos", [128, NT], I32)
    g_mix = din("g_mix", [D]); g_mem = din("g_mem", [D]); g_ffn = din("g_ffn", [D]); g_fin = din("g_fin", [D])
    g_q = din("g_q", [256]); g_kv = din("g_kv", [128])
    w_in = din("w_in", [D, DIN])
    cw = din("cw", [128, 4, 4]); cb = din("cb", [128, 4]); lba = din("lba", [128, 4]); lbx = din("lbx", [128, 4]); llam = din("llam", [128, 4])
    lwa = din("lwa", [8, 64, 64]); lwx = din("lwx", [8, 64, 64])
    w_uq = din("w_uq", [256, 768]); w_ukv = din("w_ukv", [128, 1024]); w_mkv = din("w_mkv", [D, D])
    w_br = din("w_br", [3, 512, D]); w_o = din("w_o", [D, D])
    w_gr = din("w_gr", [D, 36]); b_gr = din("b_gr", [36])
    w_eg = din("w_eg", [NE, D, 256]); w_eu = din("w_eu", [NE, D, 256]); w_ed = din("w_ed", [NE, 256, D])
    ident = din("ident", [128, 128]); cmask = din("cmask", [4, 128, 512]); invf = din("invf", [128, 16])
    out = nc.dram_tensor("out", [S, D], F32, kind="ExternalOutput").ap()

    ya_s = dscr("ya_s", [512, S], BF16); yb_s = dscr("yb_s", [512, S], BF16); yc_s = dscr("yc_s", [512, S], BF16)
    h_s = dscr("h_s", [S, D], F32); n2T_s = dscr("n2T_s", [D, S], BF16)
    comb_s = dscr("comb_s", [128, NT, NE], F32) if dbg else None

    with ExitStack() as top:
        T = Trk(nc, top)

        def SB(st, name, shape, dt):
            return st.enter_context(nc.sbuf_tensor(name, list(shape), dt))

        PS = [top.enter_context(nc.psum_tensor("ps%d" % i, [128, 512], F32)) for i in range(8)]

        def mm(out_ap, pairs, reads, writes):
            n = len(pairs)
            fns = []
            for i, (l, r) in enumerate(pairs):
                fns.append(lambda e, l=l, r=r, i=i: e.matmul(out_ap, lhsT=l, rhs=r, start=(i == 0), stop=(i == n - 1)))
            return T.group("pe", fns, reads, writes)

        idf = SB(top, "idf", [128, 128], F32)
        idb = SB(top, "idb", [128, 128], BF16)
        onesb = SB(top, "onesb", [128, 128], BF16)
        mh = SB(top, "mh", [128, 2], F32)
        T.dma("sp", idf[:], ident, writes=["idf"])
        T.dma("pool", idb[:], ident, writes=["idb"])
        T.op("dve", lambda e: e.memset(onesb[:], 1.0), writes=["onesb"])
        T.op("dve", lambda e: e.memset(mh[:], -0.5), writes=["mh"])
        gbc = {}
        gsrc = {"g_mix": g_mix, "g_mem": g_mem, "g_ffn": g_ffn, "g_fin": g_fin}

        def load_g(st_, nm, tag):
            t_ = SB(st_, "bc_%s_%s" % (nm, tag), [128, D], F32)
            T.dma("sp", t_[:], gsrc[nm].partition_broadcast(128), writes=["bc_" + nm])
            gbc[nm] = t_
        gq_bc = SB(top, "gq_bc", [128, 256], F32); T.dma("sp", gq_bc[:], g_q.partition_broadcast(128), writes=["gq_bc"])
        gkv_bc = SB(top, "gkv_bc", [128, 128], F32); T.dma("sp", gkv_bc[:], g_kv.partition_broadcast(128), writes=["gkv_bc"])
        bgr_bc = SB(top, "bgr_bc", [128, 36], F32); T.dma("sp", bgr_bc[:], b_gr.partition_broadcast(128), writes=["bgr_bc"])
        stAB = ExitStack()
        cosT = SB(stAB, "cosT", [128, NT, 16], F32)
        sinT = SB(stAB, "sinT", [128, NT, 16], F32)
        cqnT = SB(stAB, "cqnT", [128, 2, S], BF16)
        ckvnT = SB(stAB, "ckvnT", [128, S], BF16)
        kropeT = SB(stAB, "kropeT", [96, S], BF16)

        with ExitStack() as st:
            posi = SB(st, "posi", [128, NT], I32)
            posf = SB(st, "posf", [128, NT], F32)
            ivf = SB(st, "ivf", [128, 16], F32)
            ang = SB(st, "ang", [128, NT, 16], F32)
            kf = SB(st, "kf", [128, NT, 16], F32)
            ki = SB(st, "ki", [128, NT, 16], I32)
            T.dma("sp", posi[:], pos, writes=["posi"])
            T.dma("sp", ivf[:], invf, writes=["ivf"])
            T.op("dve", lambda e: e.tensor_copy(out=posf[:], in_=posi[:]), reads=["posi"], writes=["posf"])
            T.op("dve", lambda e: e.tensor_tensor(out=ang[:], in0=posf[:].unsqueeze(2).to_broadcast([128, NT, 16]),
                                                  in1=ivf[:].unsqueeze(1).to_broadcast([128, NT, 16]), op=ALU.mult),
                 reads=["posf", "ivf"], writes=["ang"])
            TWO_PI = 2.0 * math.pi
            for shift, dst, nm in ((0.0, sinT, "sinT"), (math.pi / 2.0, cosT, "cosT")):
                T.op("dve", lambda e, shift=shift: e.tensor_scalar(out=kf[:], in0=ang[:], scalar1=shift, scalar2=1.0 / TWO_PI, op0=ALU.add, op1=ALU.mult),
                     reads=["ang"], writes=["kf"])
                T.op("dve", lambda e: e.tensor_copy(out=ki[:], in_=kf[:]), reads=["kf"], writes=["ki"])
                T.op("dve", lambda e: e.tensor_copy(out=kf[:], in_=ki[:]), reads=["ki"], writes=["kf"])
                T.op("dve", lambda e, shift=shift: e.tensor_scalar(out=kf[:], in0=kf[:], scalar1=-TWO_PI, scalar2=shift, op0=ALU.mult, op1=ALU.add),
                     reads=["kf"], writes=["kf"])
                T.op("dve", lambda e: e.tensor_tensor(out=kf[:], in0=kf[:], in1=ang[:], op=ALU.add), reads=["kf", "ang"], writes=["kf"])
                T.op("dve", lambda e: e.tensor_scalar(out=kf[:], in0=kf[:], scalar1=math.pi, scalar2=-math.pi, op0=ALU.min, op1=ALU.max),
                     reads=["kf"], writes=["kf"])
                T.op("act", lambda e, dst=dst: e.activation(out=dst[:], in_=kf[:], func=AF.Sin), reads=["kf"], writes=[nm])
            T.barrier()
            if stop == 0:
                T.finish_early()

        def norm_T(st_pools, src_rows, gb, gkey, dstT, dst_cols, dkey, keep_x=None):
            xt, xk = st_pools["xt"].next() if keep_x is None else keep_x
            T.dma("sp", xt[:], src_rows, writes=[xk])
            jk, jkk = st_pools["junk"].next()
            ss, sk = st_pools["ss"].next()
            T.op("act", lambda e: e.activation(out=jk[:], in_=xt[:], func=AF.Square, accum_out=ss[:, 0:1]), reads=[xk], writes=[sk, jkk])
            T.op("dve", lambda e: e.tensor_scalar(out=ss[:, 1:2], in0=ss[:, 0:1], scalar1=1.0 / D, scalar2=EPS, op0=ALU.mult, op1=ALU.add), reads=[sk], writes=[sk])
            T.op("pool", lambda e: e.tensor_tensor(out=ss[:, 2:3], in0=ss[:, 1:2], in1=mh[:, 0:1], op=ALU.pow), reads=[sk, "mh"], writes=[sk])
            nb, nk = st_pools["nb"].next()
            T.op("dve", lambda e: e.scalar_tensor_tensor(out=nb[:], in0=xt[:], scalar=ss[:, 2:3], in1=gb[:], op0=ALU.mult, op1=ALU.mult),
                 reads=[xk, sk, gkey], writes=[nk])
            pt, pk = st_pools["ps"].next()
            ptb = pt[:].bitcast(BF16)
            T.group("pe", [(lambda e, k=k: e.transpose(out=ptb[:, k * 128:(k + 1) * 128], in_=nb[:, k * 128:(k + 1) * 128], identity=idb[:])) for k in range(8)],
                    reads=[nk, "idb"], writes=[pk])
            T.op("act", lambda e: e.activation(out=dstT[:, :, dst_cols], in_=ptb.rearrange("p (k t) -> p k t", k=8), func=AF.Copy),
                 reads=[pk], writes=[dkey])
            return xt, xk

        kmemT = SB(stAB, "kmemT", [128, 4, 256], BF16)
        vmem = SB(stAB, "vmem", [128, 2, 512], BF16)
        with ExitStack() as st:
            load_g(st, "g_mem", "a0")
            pools = {
                "xt": Rot([SB(st, "a0xt%d" % i, [128, D], F32) for i in range(2)], "a0xt"),
                "junk": Rot([SB(st, "a0jk", [128, D], BF16)], "a0jk"),
                "ss": Rot([SB(st, "a0ss%d" % i, [128, 4], F32) for i in range(2)], "a0ss"),
                "nb": Rot([SB(st, "a0nb%d" % i, [128, D], BF16) for i in range(2)], "a0nb"),
                "ps": Rot(PS[0:4], "ps"),
            }
            psr = Rot(PS[4:8], "psb")
            memT = SB(st, "memT", [128, 8, 256], BF16)
            wmk = SB(st, "wmk", [128, 8, D], BF16)
            for k in range(8):
                T.dma("pool", wmk[:, k, :], w_mkv[k * 128:(k + 1) * 128, :], writes=["wmk%d" % k])
            for mt in range(2):
                norm_T(pools, mem[mt * 128:(mt + 1) * 128, :], gbc["g_mem"], "bc_g_mem", memT, slice(mt * 128, (mt + 1) * 128), "memT%d" % mt)
            wk = ["wmk%d" % k for k in range(8)]
            for h in range(4):
                p, pk = psr.next()
                mm(p[:, 0:256], [(wmk[:, k, h * 128:(h + 1) * 128], memT[:, k, :]) for k in range(8)], reads=wk + ["memT0", "memT1"], writes=[pk])
                T.op("act", lambda e, p=p, h=h: e.activation(out=kmemT[:, h, :], in_=p[:, 0:256], func=AF.Copy), reads=[pk], writes=["kmemT"])
            for mt in range(2):
                p, pk = psr.next()
                mm(p[:, :], [(memT[:, k, mt * 128:(mt + 1) * 128], wmk[:, k, 512:1024]) for k in range(8)], reads=wk + ["memT%d" % mt], writes=[pk])
                T.op("act", lambda e, p=p, mt=mt: e.activation(out=vmem[:, mt, :], in_=p[:, :], func=AF.Copy), reads=[pk], writes=["vmem"])
            T.barrier()
            if stop == 1:
                T.finish_early()

        with ExitStack() as st:
            load_g(st, "g_mix", "a")
            pools = {
                "xt": Rot([SB(st, "axt%d" % i, [128, D], F32) for i in range(4)], "axt"),
                "junk": Rot([SB(st, "ajk", [128, D], BF16)], "ajk"),
                "ss": Rot([SB(st, "ass%d" % i, [128, 4], F32) for i in range(8)], "ass"),
                "nb": Rot([SB(st, "anb%d" % i, [128, D], BF16) for i in range(4)], "anb"),
                "ps": Rot(PS[0:2], "psn"),
            }
            psr = Rot(PS[2:8], "psa")
            NA = 1952
            win = SB(st, "win", [128, 8, NA], BF16)
            for k in range(8):
                for (c0, c1) in ((0, 1024), (1024, NA)):
                    T.dma("pool", win[:, k, c0:c1], w_in[k * 128:(k + 1) * 128, c0:c1], writes=["win%d_%d" % (k, c0)])
            winkeys = ["win%d_%d" % (k, c0) for k in range(8) for c0 in (0, 1024)]
            wabd = SB(st, "wabd", [128, 4, 128], BF16)
            wxbd = SB(st, "wxbd", [128, 4, 128], BF16)
            T.op("dve", lambda e: e.memset(wabd[:], 0.0), writes=["wabd"])
            T.op("dve", lambda e: e.memset(wxbd[:], 0.0), writes=["wxbd"])
            for c in range(4):
                for j in range(2):
                    T.dma("pool", wabd[j * 64:(j + 1) * 64, c, j * 64:(j + 1) * 64], lwa[2 * c + j], reads=["wabd"], writes=["wabd"])
                    T.dma("pool", wxbd[j * 64:(j + 1) * 64, c, j * 64:(j + 1) * 64], lwx[2 * c + j], reads=["wxbd"], writes=["wxbd"])
            cws = SB(st, "cws", [128, 4, 4], F32); T.dma("sp", cws[:], cw, writes=["cws"])
            cbs = SB(st, "cbs", [128, 4], F32); T.dma("sp", cbs[:], cb, writes=["cbs"])
            bas = SB(st, "bas", [128, 4], F32); T.dma("sp", bas[:], lba, writes=["bas"])
            bxs = SB(st, "bxs", [128, 4], F32); T.dma("sp", bxs[:], lbx, writes=["bxs"])
            lam = SB(st, "lam", [128, 4], F32); T.dma("sp", lam[:], llam, writes=["lam"])
            sp1 = SB(st, "sp1", [128, 4], F32); sp2 = SB(st, "sp2", [128, 4], F32); c1s = SB(st, "c1s", [128, 4], F32)
            T.op("dve", lambda e: e.tensor_scalar(out=sp1[:], in0=lam[:], scalar1=-1.0, scalar2=None, op0=ALU.mult), reads=["lam"], writes=["sp1"])
            T.op("dve", lambda e: e.tensor_tensor(out=sp1[:], in0=sp1[:], in1=lam[:], op=ALU.max), reads=["lam", "sp1"], writes=["sp1"])
            T.op("act", lambda e: e.activation(out=sp1[:], in_=sp1[:], func=AF.Exp, scale=-1.0), reads=["sp1"], writes=["sp1"])
            T.op("act", lambda e: e.activation(out=sp1[:], in_=sp1[:], func=AF.Ln, bias=1.0, scale=1.0), reads=["sp1"], writes=["sp1"])
            T.op("dve", lambda e: e.tensor_scalar(out=sp2[:], in0=lam[:], scalar1=-1.0, scalar2=0.0, op0=ALU.mult, op1=ALU.max), reads=["lam"], writes=["sp2"])
            T.op("dve", lambda e: e.tensor_tensor(out=c1s[:], in0=sp1[:], in1=sp2[:], op=ALU.add), reads=["sp1", "sp2"], writes=["c1s"])
            T.op("dve", lambda e: e.tensor_scalar(out=c1s[:], in0=c1s[:], scalar1=-8.0, scalar2=None, op0=ALU.mult), reads=["c1s"], writes=["c1s"])

            if stop == 2 and substop == 1:
                T.finish_early()
            nTp = Rot([SB(st, "anT%d" % i, [128, 8, 512], BF16) for i in range(1)], "anT")
            xl = [SB(st, "xl%d" % c, [128, 515], F32) for c in range(4)]
            hprev = SB(st, "hprev", [128, 4], F32)
            T.op("dve", lambda e: e.memset(hprev[:], 0.0), writes=["hprev"])
            for c in range(4):
                T.op("dve", lambda e, c=c: e.memset(xl[c][:], 0.0), writes=["xl%d" % c])
            xa = [SB(st, "xa%d" % c, [128, 512], F32) for c in range(4)]
            xab = [SB(st, "xab%d" % c, [128, 512], BF16) for c in range(4)]
            rr = [SB(st, "rr%d" % c, [128, 512], F32) for c in range(4)]
            ig = [SB(st, "ig%d" % c, [128, 512], F32) for c in range(4)]
            gl = [SB(st, "gl%d" % c, [128, 512], BF16) for c in range(4)]
            sq = [SB(st, "sq%d" % c, [128, 512], F32) for c in range(4)]
            hs = sq
            yaT = Rot([SB(st, "yaT%d" % i, [128, 4, 512], BF16) for i in range(1)], "yaT")
            ycT = Rot([SB(st, "ycT%d" % i, [128, 4, 512], BF16) for i in range(1)], "ycT")
            qmT = Rot([SB(st, "qmT%d" % i, [128, 512], BF16) for i in range(3)], "qmT")
            pTm = Rot([SB(st, "pTm%d" % i, [128, 512], BF16) for i in range(6)], "pTm")
            rden = Rot([SB(st, "rdm%d" % i, [128, 512], F32) for i in range(2)], "rdm")
            latn = Rot([SB(st, "latn%d" % i, [128, 480], BF16) for i in range(4)], "latn")
            for i in range(4):
                T.op("pool", lambda e, i=i: e.memset(latn.bufs[i][:], 0.0), writes=["latn#%d" % i])
            ss2 = Rot([SB(st, "ss2_%d" % i, [128, 8], F32) for i in range(4)], "ss2")
            rt = Rot([SB(st, "rt%d" % i, [128, 4, 16], F32) for i in range(4)], "rt")
            ya_v = ya_s.rearrange("(c p) t -> p c t", p=128)
            yc_v = yc_s.rearrange("(c p) t -> p c t", p=128)

            if stop == 2 and substop == 11:
                T.finish_early()
            def a_s1(i):
                nbs = []
                for sub_ in range(4):
                    r0 = i * 512 + sub_ * 128
                    xt, xk = pools["xt"].next()
                    T.dma("sp", xt[:], x[r0:r0 + 128, :], writes=[xk])
                    jk, jkk = pools["junk"].next()
                    ss, sk = pools["ss"].next()
                    T.op("act", lambda e, jk=jk, xt=xt, ss=ss: e.activation(out=jk[:], in_=xt[:], func=AF.Square, accum_out=ss[:, 0:1]), reads=[xk], writes=[sk, jkk])
                    T.op("dve", lambda e, ss=ss: e.tensor_scalar(out=ss[:, 1:2], in0=ss[:, 0:1], scalar1=1.0 / D, scalar2=EPS, op0=ALU.mult, op1=ALU.add), reads=[sk], writes=[sk])
                    T.op("pool", lambda e, ss=ss: e.tensor_tensor(out=ss[:, 2:3], in0=ss[:, 1:2], in1=mh[:, 0:1], op=ALU.pow), reads=[sk, "mh"], writes=[sk])
                    nb, nk = pools["nb"].next()
                    T.op("dve", lambda e, nb=nb, xt=xt, ss=ss, gb=gbc["g_mix"]: e.scalar_tensor_tensor(out=nb[:], in0=xt[:], scalar=ss[:, 2:3], in1=gb[:], op0=ALU.mult, op1=ALU.mult),
                         reads=[xk, sk, "bc_g_mix"], writes=[nk])
                    nbs.append((nb, nk))
                return nbs

            def a_s2(nbs):
                nT, nTk = nTp.next()
                nTkeys = []
                for sub_ in range(4):
                    nb, nk = nbs[sub_]
                    pt, pk = pools["ps"].next()
                    ptb = pt[:].bitcast(BF16)
                    T.group("pe", [(lambda e, ptb=ptb, nb=nb, k=k: e.transpose(out=ptb[:, k * 128:(k + 1) * 128], in_=nb[:, k * 128:(k + 1) * 128], identity=idb[:])) for k in range(8)],
                            reads=[nk, "idb"], writes=[pk])
                    dk = "%s_s%d" % (nTk, sub_)
                    T.op("act", lambda e, ptb=ptb, nT=nT, sub_=sub_: e.activation(out=nT[:, :, sub_ * 128:(sub_ + 1) * 128], in_=ptb.rearrange("p (k t) -> p k t", k=8), func=AF.Copy),
                         reads=[pk], writes=[dk])
                    nTkeys.append(dk)
                return nT, nTk, nTkeys

            nbs_next = a_s1(0)
            for i in range(NB):
                nT, nTk, nTkeys = a_s2(nbs_next)
                if stop == 2 and substop == 2 and i == 0:
                    T.finish_early()
                LS = []
                for sub in range(4):
                    ti = i * 4 + sub
                    p, pk = psr.next()
                    mm(p[:, 0:416], [(nT[:, k, sub * 128:(sub + 1) * 128], win[:, k, 1024:1440]) for k in range(8)], reads=winkeys + [nTkeys[sub]], writes=[pk])
                    LS.append({"ti": ti, "tok": slice(ti * 128, (ti + 1) * 128), "p": p, "pk": pk})
                for L_ in LS:
                    p, pk = L_["p"], L_["pk"]
                    s2, s2k = ss2.next()
                    jk, jkk = pools["junk"].next()
                    T.op("act", lambda e, p=p, s2=s2, jk=jk: e.activation(out=jk[:, 0:256], in_=p[:, 0:256], func=AF.Square, accum_out=s2[:, 0:1]), reads=[pk], writes=[s2k, jkk])
                    T.op("act", lambda e, p=p, s2=s2, jk=jk: e.activation(out=jk[:, 256:384], in_=p[:, 256:384], func=AF.Square, accum_out=s2[:, 1:2]), reads=[pk], writes=[s2k, jkk])
                    L_["s2"], L_["s2k"] = s2, s2k
                for L_ in LS:
                    s2, s2k = L_["s2"], L_["s2k"]
                    T.op("dve", lambda e, s2=s2: e.tensor_scalar(out=s2[:, 2:3], in0=s2[:, 0:1], scalar1=1.0 / 256, scalar2=EPS, op0=ALU.mult, op1=ALU.add), reads=[s2k], writes=[s2k])
                    T.op("dve", lambda e, s2=s2: e.tensor_scalar(out=s2[:, 3:4], in0=s2[:, 1:2], scalar1=1.0 / 128, scalar2=EPS, op0=ALU.mult, op1=ALU.add), reads=[s2k], writes=[s2k])
                for L_ in LS:
                    s2, s2k = L_["s2"], L_["s2k"]
                    T.op("pool", lambda e, s2=s2: e.tensor_tensor(out=s2[:, 4:6], in0=s2[:, 2:4], in1=mh[:, 0:2], op=ALU.pow), reads=[s2k, "mh"], writes=[s2k])
                for L_ in LS:
                    p, pk, s2, s2k = L_["p"], L_["pk"], L_["s2"], L_["s2k"]
                    ln_, lk = latn.next()
                    T.op("dve", lambda e, p=p, s2=s2, ln_=ln_: e.scalar_tensor_tensor(out=ln_[:, 0:256], in0=p[:, 0:256], scalar=s2[:, 4:5], in1=gq_bc[:], op0=ALU.mult, op1=ALU.mult),
                         reads=[pk, s2k, "gq_bc"], writes=[lk])
                    T.op("dve", lambda e, p=p, s2=s2, ln_=ln_: e.scalar_tensor_tensor(out=ln_[:, 256:384], in0=p[:, 256:384], scalar=s2[:, 5:6], in1=gkv_bc[:], op0=ALU.mult, op1=ALU.mult),
                         reads=[pk, s2k, "gkv_bc"], writes=[lk])
                    L_["ln"], L_["lk"] = ln_, lk
                for L_ in LS:
                    p, pk, ti = L_["p"], L_["pk"], L_["ti"]
                    r_, rk = rt.next()
                    cs = cosT[:, ti, :]; sn = sinT[:, ti, :]
                    T.op("dve", lambda e, p=p, r_=r_, cs=cs: e.tensor_tensor(out=r_[:, 0, :], in0=p[:, 384:400], in1=cs, op=ALU.mult), reads=[pk, "cosT"], writes=[rk])
                    T.op("dve", lambda e, p=p, r_=r_, sn=sn: e.tensor_tensor(out=r_[:, 1, :], in0=p[:, 400:416], in1=sn, op=ALU.mult), reads=[pk, "sinT"], writes=[rk])
                    T.op("dve", lambda e, p=p, r_=r_, cs=cs: e.tensor_tensor(out=r_[:, 2, :], in0=p[:, 400:416], in1=cs, op=ALU.mult), reads=[pk, "cosT"], writes=[rk])
                    T.op("dve", lambda e, p=p, r_=r_, sn=sn: e.tensor_tensor(out=r_[:, 3, :], in0=p[:, 384:400], in1=sn, op=ALU.mult), reads=[pk, "sinT"], writes=[rk])
                    L_["r"], L_["rk"] = r_, rk
                for L_ in LS:
                    r_, rk, ln_, lk = L_["r"], L_["rk"], L_["ln"], L_["lk"]
                    T.op("pool", lambda e, r_=r_, ln_=ln_: e.tensor_tensor(out=ln_[:, 448:464], in0=r_[:, 0, :], in1=r_[:, 1, :], op=ALU.subtract), reads=[rk], writes=[lk])
                    T.op("pool", lambda e, r_=r_, ln_=ln_: e.tensor_tensor(out=ln_[:, 464:480], in0=r_[:, 2, :], in1=r_[:, 3, :], op=ALU.add), reads=[rk], writes=[lk])
                for L_ in LS:
                    ln_, lk = L_["ln"], L_["lk"]
                    pt, ptk = pools["ps"].next()
                    ptb = pt[:].bitcast(BF16)
                    T.group("pe", [
                        lambda e, ptb=ptb, ln_=ln_: e.transpose(out=ptb[:, 0:128], in_=ln_[:, 0:128], identity=idb[:]),
                        lambda e, ptb=ptb, ln_=ln_: e.transpose(out=ptb[:, 128:256], in_=ln_[:, 128:256], identity=idb[:]),
                        lambda e, ptb=ptb, ln_=ln_: e.transpose(out=ptb[:, 256:384], in_=ln_[:, 256:384], identity=idb[:]),
                        lambda e, ptb=ptb, ln_=ln_: e.transpose(out=ptb[0:96, 384:512], in_=ln_[:, 384:480], identity=idb[:]),
                    ], reads=[lk, "idb"], writes=[ptk])
                    tok, ti = L_["tok"], L_["ti"]
                    T.op("act", lambda e, ptb=ptb, tok=tok: e.activation(out=cqnT[:, :, tok], in_=ptb[:, 0:256].rearrange("p (k t) -> p k t", k=2), func=AF.Copy),
                         reads=[ptk], writes=["cqnT%d" % ti])
                    T.op("act", lambda e, ptb=ptb, tok=tok: e.activation(out=ckvnT[:, tok], in_=ptb[:, 256:384], func=AF.Copy), reads=[ptk], writes=["ckvnT%d" % ti])
                    T.op("act", lambda e, ptb=ptb, tok=tok: e.activation(out=kropeT[64:96, tok], in_=ptb[64:96, 384:512], func=AF.Copy), reads=[ptk], writes=["kropeT%d" % ti])

                if i + 1 < NB:
                    nbs_next = a_s1(i + 1)
                if stop == 2 and substop == 3 and i == 0:
                    T.finish_early()
                for c in range(4):
                    p, pk = psr.next()
                    mm(p[:, :], [(win[:, k, c * 128:(c + 1) * 128], nT[:, k, :]) for k in range(8)], reads=winkeys + nTkeys, writes=[pk])
                    xk = "xl%d" % c
                    T.op("pool", lambda e, c=c: e.tensor_copy(out=xl[c][:, 0:3], in_=xl[c][:, 512:515]), reads=[xk], writes=[xk])
                    T.op("act", lambda e, c=c, p=p: e.activation(out=xl[c][:, 3:515], in_=p[:, :], func=AF.Copy), reads=[pk, xk], writes=[xk])
                for c in range(4):
                    xk = "xl%d" % c
                    T.op("dve", lambda e, c=c: e.tensor_scalar(out=xa[c][:], in0=xl[c][:, 0:512], scalar1=cws[:, c, 0:1], scalar2=cbs[:, c:c + 1], op0=ALU.mult, op1=ALU.add),
                         reads=[xk, "cws", "cbs"], writes=["xa%d" % c])
                    for j in range(1, 4):
                        T.op("dve", lambda e, c=c, j=j: e.scalar_tensor_tensor(out=xa[c][:], in0=xl[c][:, j:j + 512], scalar=cws[:, c, j:j + 1], in1=xa[c][:], op0=ALU.mult, op1=ALU.add),
                             reads=[xk, "cws", "xa%d" % c], writes=["xa%d" % c])
                for c in range(4):
                    T.op("pool", lambda e, c=c: e.tensor_copy(out=xab[c][:], in_=xa[c][:]), reads=["xa%d" % c], writes=["xab%d" % c])
                for c in range(4):
                    p, pk = psr.next()
                    mm(p[:, :], [(win[:, k, 512 + c * 128:512 + (c + 1) * 128], nT[:, k, :]) for k in range(8)], reads=winkeys + nTkeys, writes=[pk])
                    T.op("act", lambda e, c=c, p=p: e.activation(out=gl[c][:], in_=p[:, :], func=AF.Gelu_apprx_tanh), reads=[pk], writes=["gl%d" % c])
                for c in range(4):
                    p, pk = psr.next()
                    mm(p[:, :], [(wabd[:, c, :], xab[c][:])], reads=["wabd", "xab%d" % c], writes=[pk])
                    T.op("act", lambda e, c=c, p=p: e.activation(out=rr[c][:], in_=p[:, :], func=AF.Sigmoid, bias=bas[:, c:c + 1]), reads=[pk, "bas"], writes=["rr%d" % c])
                    p, pk = psr.next()
                    mm(p[:, :], [(wxbd[:, c, :], xab[c][:])], reads=["wxbd", "xab%d" % c], writes=[pk])
                    T.op("act", lambda e, c=c, p=p: e.activation(out=ig[c][:], in_=p[:, :], func=AF.Sigmoid, bias=bxs[:, c:c + 1]), reads=[pk, "bxs"], writes=["ig%d" % c])
                for c in range(4):
                    T.op("act", lambda e, c=c: e.activation(out=rr[c][:], in_=rr[c][:], func=AF.Exp, scale=c1s[:, c:c + 1]), reads=["rr%d" % c, "c1s"], writes=["rr%d" % c])
                if stop == 2 and substop == 4 and i == 0:
                    T.finish_early()
                ycb, yck = ycT.next()

                def ma_q(h):
                    p, pk = psr.next()
                    mm(p[:, :], [(win[:, k, 1440 + h * 128:1440 + (h + 1) * 128], nT[:, k, :]) for k in range(8)], reads=winkeys + nTkeys, writes=[pk])
                    qm, qmk = qmT.next()
                    T.op("dve", lambda e, qm=qm, p=p: e.tensor_copy(out=qm[:], in_=p[:, :]), reads=[pk], writes=[qmk])
                    return qm, qmk

                def ma_s(h, qm, qmk):
                    pts = []
                    for mc in range(2):
                        p2, p2k = psr.next()
                        mm(p2[:, :], [(kmemT[:, h, mc * 128:(mc + 1) * 128], qm[:])], reads=[qmk], writes=[p2k])
                        pT, pTk = pTm.next()
                        T.op("act", lambda e, pT=pT, p2=p2: e.activation(out=pT[:], in_=p2[:, :], func=AF.Exp, scale=1.0 / math.sqrt(128.0)), reads=[p2k], writes=[pTk])
                        pts.append((pT, pTk))
                    return pts

                def ma_o(h, pts):
                    pn, pnk = psr.next()
                    mm(pn[:, :], [(vmem[:, mc, h * 128:(h + 1) * 128], pts[mc][0][:]) for mc in range(2)], reads=[pts[0][1], pts[1][1]], writes=[pnk])
                    pd, pdk = psr.next()
                    mm(pd[:, :], [(onesb[:], pts[mc][0][:]) for mc in range(2)], reads=[pts[0][1], pts[1][1], "onesb"], writes=[pdk])
                    rd, rdk = rden.next()
                    T.op("dve", lambda e, rd=rd, pd=pd: e.reciprocal(out=rd[:], in_=pd[:, :]), reads=[pdk], writes=[rdk])
                    T.op("dve", lambda e, rd=rd, pn=pn, ycb=ycb, h=h: e.tensor_tensor(out=ycb[:, h, :], in0=pn[:, :], in1=rd[:], op=ALU.mult), reads=[pnk, rdk], writes=[yck])

                q_ = {0: ma_q(0)}
                q_[1] = ma_q(1)
                s_ = {0: ma_s(0, *q_[0])}
                for h in range(4):
                    if h + 2 < 4:
                        q_[h + 2] = ma_q(h + 2)
                    if h + 1 < 4:
                        s_[h + 1] = ma_s(h + 1, *q_[h + 1])
                    ma_o(h, s_[h])
                T.dma("sp", yc_v[:, :, i * 512:(i + 1) * 512], ycb[:], reads=[yck], writes=["yc_s%d" % i])
                if stop == 2 and substop == 5 and i == 0:
                    T.finish_early()
                yab, yak = yaT.next()
                for c in range(4):
                    T.op("pool", lambda e, c=c: e.tensor_tensor(out=sq[c][:], in0=rr[c][:], in1=rr[c][:], op=ALU.mult), reads=["rr%d" % c], writes=["sq%d" % c])
                for c in range(4):
                    T.op("act", lambda e, c=c: e.activation(out=sq[c][:], in_=sq[c][:], func=AF.Sqrt, scale=-1.0, bias=1.0), reads=["sq%d" % c], writes=["sq%d" % c])
                for c in range(4):
                    T.op("pool", lambda e, c=c: e.tensor_tensor(out=ig[c][:], in0=ig[c][:], in1=xa[c][:], op=ALU.mult), reads=["ig%d" % c, "xa%d" % c], writes=["ig%d" % c])
                    T.op("pool", lambda e, c=c: e.tensor_tensor(out=ig[c][:], in0=ig[c][:], in1=sq[c][:], op=ALU.mult), reads=["ig%d" % c, "sq%d" % c], writes=["ig%d" % c])
                    T.op("dve", lambda e, c=c: e.tensor_tensor_scan(out=hs[c][:], data0=rr[c][:], data1=ig[c][:], initial=hprev[:, c:c + 1], op0=ALU.mult, op1=ALU.add),
                         reads=["rr%d" % c, "ig%d" % c, "hprev", "sq%d" % c], writes=["sq%d" % c])
                    T.op("dve", lambda e, c=c: e.tensor_copy(out=hprev[:, c:c + 1], in_=hs[c][:, 511:512]), reads=["sq%d" % c], writes=["hprev"])
                    T.op("pool", lambda e, c=c, yab=yab: e.tensor_tensor(out=yab[:, c, :], in0=hs[c][:], in1=gl[c][:], op=ALU.mult), reads=["sq%d" % c, "gl%d" % c], writes=[yak])
                T.dma("sp", ya_v[:, :, i * 512:(i + 1) * 512], yab[:], reads=[yak], writes=["ya_s%d" % i])
            T.barrier()
            if stop == 2:
                T.finish_early()

        stCW = ExitStack()
        wg = stCW.enter_context(nc.sbuf_tensor("wg", [128, 8, 3072], BF16, side="right"))
        wbr = stCW.enter_context(nc.sbuf_tensor("wbr", [128, 3, 4, D], BF16, side="right"))
        wo = stCW.enter_context(nc.sbuf_tensor("wo", [128, 8, D], BF16, side="right"))
        with ExitStack() as st:
            wuq = SB(st, "wuq", [128, 2, 768], BF16)
            wukv = SB(st, "wukv", [128, 1024], BF16)
            for k in range(2):
                T.dma("pool", wuq[:, k, :], w_uq[k * 128:(k + 1) * 128, :], writes=["wuq"])
            T.dma("pool", wukv[:], w_ukv, writes=["wukv"])
            msk = SB(st, "msk", [128, 4, 512], BF16)
            for r in range(4):
                T.dma("pool", msk[:, r, :], cmask[r], writes=["msk"])
            prefetch_c_weights = True
            KT = [SB(st, "KT%d" % i, [128, S], BF16) for i in range(2)]
            QT = [SB(st, "QT%d" % i, [128, S], BF16) for i in range(2)]
            for i in range(2):
                T.op("pool", lambda e, i=i: e.memset(KT[i][:], 0.0), writes=["KT%d" % i])
                T.op("pool", lambda e, i=i: e.memset(QT[i][:], 0.0), writes=["QT%d" % i])
            VH = [SB(st, "VH%d" % i, [128, NT, 128], BF16) for i in range(2)]
            for i in range(2):
                T.op("pool", lambda e, i=i: e.memset(VH[i][:], 1.0), writes=["VH%d" % i])
            for k in range(8):
                for b3 in range(3):
                    T.dma("pool", wg[:, k, b3 * 1024:(b3 + 1) * 1024], w_in[k * 128:(k + 1) * 128, 1952 + b3 * 1024:1952 + (b3 + 1) * 1024], writes=["wg%d_%d" % (k, b3)])
            for b3 in range(3):
                for kc in range(4):
                    T.dma("pool", wbr[:, b3, kc, :], w_br[b3, kc * 128:(kc + 1) * 128, :], writes=["wbr%d_%d" % (b3, kc)])
            for k in range(8):
                T.dma("pool", wo[:, k, :], w_o[k * 128:(k + 1) * 128, :], writes=["wo%d" % k])
            NQB = 1 if "q1" in EXP else 2
            qtok = Rot([SB(st, "qtok%d" % i, [128, 4, 96], BF16) for i in range(NQB)], "qtok", shared=True)
            rt = Rot([SB(st, "brt%d" % i, [128, 4, 4, 16], F32) for i in range(NQB)], "brt", shared=True)
            pTb = Rot([SB(st, "pTb%d" % i, [128, 512], BF16) for i in range(6)], "pTb")
            rdb = Rot([SB(st, "rdb%d" % i, [128, 512], F32) for i in range(2)], "rdb")
            ybT = Rot([SB(st, "ybT%d" % i, [64, 512], BF16) for i in range(2)], "ybT")
            psr = Rot(PS[0:4] + PS[6:8], "psb")
            pso = Rot(PS[4:6], "pso")
            scale = 1.0 / math.sqrt(96.0)
            allck = ["ckvnT%d" % t for t in range(NT)]
            allcq = ["cqnT%d" % t for t in range(NT)]
            allkr = ["kropeT%d" % t for t in range(NT)]
            def prep(h):
                b = h % 2
                KTh, QTh, VHh = KT[b], QT[b], VH[b]
                kk, qk, vk = "KT%d" % b, "QT%d" % b, "VH%d" % b
                for i in range(NB):
                    p, pk = psr.next()
                    mm(p[0:64, :], [(wukv[:, h * 128:h * 128 + 64], ckvnT[:, i * 512:(i + 1) * 512])], reads=["wukv"] + allck[i * 4:(i + 1) * 4], writes=[pk])
                    T.op("act", lambda e, p=p, i=i, KTh=KTh: e.activation(out=KTh[0:64, i * 512:(i + 1) * 512], in_=p[0:64, :], func=AF.Copy), reads=[pk], writes=[kk])
                T.op("act", lambda e, KTh=KTh: e.activation(out=KTh[64:96, :], in_=kropeT[64:96, :], func=AF.Copy), reads=allkr, writes=[kk])
                for g in range(4):
                    p, pk = psr.next()
                    T.group("pe", [(lambda e, p=p, j=j, g=g, h=h: e.matmul(p[:, j * 64:(j + 1) * 64], lhsT=ckvnT[:, (g * 8 + j) * 128:(g * 8 + j + 1) * 128],
                                                                        rhs=wukv[:, h * 128 + 64:h * 128 + 128], start=True, stop=True)) for j in range(8)],
                            reads=["wukv"] + allck[g * 8:(g + 1) * 8], writes=[pk])
                    T.op("dve", lambda e, p=p, g=g, VHh=VHh: e.tensor_copy(out=VHh[:, g * 8:(g + 1) * 8, 0:64], in_=p[:, :].rearrange("p (j d) -> p j d", j=8)), reads=[pk], writes=[vk])
                for g in range(NB):
                    p, pk = psr.next()
                    fns = []
                    for j in range(4):
                        ti = g * 4 + j
                        for k in range(2):
                            fns.append(lambda e, p=p, j=j, k=k, ti=ti, h=h: e.matmul(p[:, j * 96:(j + 1) * 96], lhsT=cqnT[:, k, ti * 128:(ti + 1) * 128],
                                                                                  rhs=wuq[:, k, h * 96:(h + 1) * 96], start=(k == 0), stop=(k == 1)))
                    T.group("pe", fns, reads=["wuq"] + allcq[g * 4:(g + 1) * 4], writes=[pk])
                    pv = p[:, 0:384].rearrange("p (j d) -> p j d", j=4)
                    qt, qtk = qtok.next()
                    T.op("act", lambda e, pv=pv, qt=qt: e.activation(out=qt[:, :, 0:64], in_=pv[:, :, 0:64], func=AF.Copy), reads=[pk], writes=[qtk])
                    r_, rk = rt.next()
                    cs = cosT[:, g * 4:(g + 1) * 4, :]; sn = sinT[:, g * 4:(g + 1) * 4, :]
                    T.op("dve", lambda e, pv=pv, r_=r_, cs=cs: e.tensor_tensor(out=r_[:, 0, :, :], in0=pv[:, :, 64:80], in1=cs, op=ALU.mult), reads=[pk, "cosT"], writes=[rk])
                    T.op("dve", lambda e, pv=pv, r_=r_, sn=sn: e.tensor_tensor(out=r_[:, 1, :, :], in0=pv[:, :, 80:96], in1=sn, op=ALU.mult), reads=[pk, "sinT"], writes=[rk])
                    T.op("dve", lambda e, pv=pv, r_=r_, cs=cs: e.tensor_tensor(out=r_[:, 2, :, :], in0=pv[:, :, 80:96], in1=cs, op=ALU.mult), reads=[pk, "cosT"], writes=[rk])
                    T.op("dve", lambda e, pv=pv, r_=r_, sn=sn: e.tensor_tensor(out=r_[:, 3, :, :], in0=pv[:, :, 64:80], in1=sn, op=ALU.mult), reads=[pk, "sinT"], writes=[rk])
                    T.op("dve", lambda e, r_=r_, qt=qt: e.tensor_tensor(out=qt[:, :, 64:80], in0=r_[:, 0, :, :], in1=r_[:, 1, :, :], op=ALU.subtract), reads=[rk], writes=[qtk])
                    T.op("dve", lambda e, r_=r_, qt=qt: e.tensor_tensor(out=qt[:, :, 80:96], in0=r_[:, 2, :, :], in1=r_[:, 3, :, :], op=ALU.add), reads=[rk], writes=[qtk])
                    pt, ptk = psr.next()
                    ptb = pt[:].bitcast(BF16)
                    T.group("pe", [(lambda e, ptb=ptb, qt=qt, j=j: e.transpose(out=ptb[0:96, j * 128:(j + 1) * 128], in_=qt[:, j, :], identity=idb[:])) for j in range(4)],
                            reads=[qtk, "idb"], writes=[ptk])
                    T.op("dve", lambda e, ptb=ptb, g=g, QTh=QTh: e.tensor_copy(out=QTh[0:96, g * 512:(g + 1) * 512], in_=ptb[0:96, 0:512]), reads=[ptk], writes=[qk])

            LOOK = 4

            def attn(h):
                b = h % 2
                KTh, QTh, VHh = KT[b], QT[b], VH[b]
                kk, qk, vk = "KT%d" % b, "QT%d" % b, "VH%d" % b
                items = [(i, kt) for i in range(NB) for kt in range(4 * i + 4)]
                N = len(items)
                S1 = {}
                acc = {}

                def stage1(n):
                    i, kt = items[n]
                    p, pk = psr.next()
                    mm(p[:, :], [(KTh[:, kt * 128:(kt + 1) * 128], QTh[:, i * 512:(i + 1) * 512])], reads=[kk, qk], writes=[pk])
                    pT, pTk = pTb.next()
                    T.op("act", lambda e, pT=pT, p=p: e.activation(out=pT[:], in_=p[:, :], func=AF.Exp, scale=scale), reads=[pk], writes=[pTk])
                    if kt >= 4 * i:
                        r = kt - 4 * i
                        T.op("dve", lambda e, pT=pT, r=r: e.tensor_tensor(out=pT[:], in0=pT[:], in1=msk[:, r, :], op=ALU.mult), reads=[pTk, "msk"], writes=[pTk])
                    S1[n] = (pT, pTk)

                def stage2(n):
                    i, kt = items[n]
                    nk = 4 * i + 4
                    if kt == 0:
                        acc[i] = pso.next()
                    po, pok = acc[i]
                    pT, pTk = S1.pop(n)
                    T.group("pe", [lambda e, po=po, pT=pT, kt=kt, nk=nk, VHh=VHh: e.matmul(po[:, :], lhsT=VHh[:, kt, :], rhs=pT[:], start=(kt == 0), stop=(kt == nk - 1))],
                            reads=[pTk, vk], writes=[pok])
                    if kt == nk - 1:
                        rd, rdk = rdb.next()
                        T.op("dve", lambda e, rd=rd, po=po: e.reciprocal(out=rd[64:128, :], in_=po[64:128, :]), reads=[pok], writes=[rdk])
                        yb, ybk = ybT.next()
                        T.op("dve", lambda e, rd=rd, po=po, yb=yb: e.tensor_tensor(out=yb[:], in0=po[0:64, :], in1=rd[64:128, :], op=ALU.mult), reads=[pok, rdk], writes=[ybk])
                        T.dma("sp", yb_s[h * 64:(h + 1) * 64, i * 512:(i + 1) * 512], yb[:], reads=[ybk], writes=["yb_s_%d_%d" % (h, i)])

                for n in range(min(LOOK, N)):
                    stage1(n)
                for n in range(N):
                    if n + LOOK < N:
                        stage1(n + LOOK)
                    stage2(n)

            prep(0)
            for h in range(8):
                if h + 1 < 8:
                    prep(h + 1)
                attn(h)
            T.barrier()
            if stop == 3:
                T.finish_early()

        stAB.close()
        comb = SB(top, "comb", [128, NT, NE], F32)
        lg = SB(top, "lg", [128, NT, 36], F32)
        with ExitStack() as st:
            load_g(st, "g_mix", "c")
            load_g(st, "g_ffn", "c")
            pools = {
                "xt": Rot([SB(st, "cxt%d" % i, [128, D], F32) for i in range(4)], "cxt"),
                "junk": Rot([SB(st, "cjk", [128, D], BF16)], "cjk"),
                "ss": Rot([SB(st, "css%d" % i, [128, 4], F32) for i in range(8)], "css"),
                "nb": Rot([SB(st, "cnb%d" % i, [128, D], BF16) for i in range(2)], "cnb"),
                "ps": Rot(PS[0:2], "psn"),
            }
            psr = Rot(PS[2:8], "psc")
            wgkeys = ["wg%d_%d" % (k, b3) for k in range(8) for b3 in range(3)]
            wgr = SB(st, "wgr", [128, 8, 36], F32)
            T.dma("sp", wgr[:], w_gr.rearrange("(k p) c -> p k c", p=128), writes=["wgr"])
            TC = 256
            NSC = TC // 128
            NBC = S // TC
            nTp = Rot([SB(st, "cnT%d" % i, [128, 8, TC], BF16) for i in range(1)], "cnT")
            yT = [Rot([SB(st, "yT%d_%d" % (b3, i), [128, 4, TC], BF16) for i in range(2)], "yT%d" % b3) for b3 in range(3)]
            ysrc = [ya_s.rearrange("(c p) t -> p c t", p=128), yb_s.rearrange("(c p) t -> p c t", p=128), yc_s.rearrange("(c p) t -> p c t", p=128)]
            gs = Rot([SB(st, "gs%d" % i, [128, TC], F32) for i in range(3)], "gs")
            tb = Rot([SB(st, "tb%d" % i, [128, TC], F32) for i in range(6)], "tb")
            mT = Rot([SB(st, "mT%d" % i, [128, 8, TC], BF16) for i in range(1)], "mT")
            hT = Rot([SB(st, "hT%d" % i, [128, D], F32) for i in range(2)], "hT")
            n2f = Rot([SB(st, "n2f%d" % i, [128, D], F32) for i in range(2)], "n2f")
            n2Tf = Rot([SB(st, "n2Tf%d" % i, [128, 8, 128], F32) for i in range(1)], "n2Tf")
            n2Tb = Rot([SB(st, "n2Tb%d" % i, [128, 8, TC], BF16) for i in range(1)], "n2Tb")
            n2T_v = n2T_s.rearrange("(k p) t -> p k t", p=128)

            def c_s1(i):
                st_ = {"i": i, "xts": [], "nbs": []}
                for sub in range(NSC):
                    r0 = i * TC + sub * 128
                    xt, xk = pools["xt"].next()
                    T.dma("sp", xt[:], x[r0:r0 + 128, :], writes=[xk])
                    jk, jkk = pools["junk"].next()
                    ss, sk = pools["ss"].next()
                    T.op("act", lambda e, jk=jk, xt=xt, ss=ss: e.activation(out=jk[:], in_=xt[:], func=AF.Square, accum_out=ss[:, 0:1]), reads=[xk], writes=[sk, jkk])
                    T.op("dve", lambda e, ss=ss: e.tensor_scalar(out=ss[:, 1:2], in0=ss[:, 0:1], scalar1=1.0 / D, scalar2=EPS, op0=ALU.mult, op1=ALU.add), reads=[sk], writes=[sk])
                    T.op("pool", lambda e, ss=ss: e.tensor_tensor(out=ss[:, 2:3], in0=ss[:, 1:2], in1=mh[:, 0:1], op=ALU.pow), reads=[sk, "mh"], writes=[sk])
                    nb, nk = pools["nb"].next()
                    T.op("dve", lambda e, nb=nb, xt=xt, ss=ss, gb=gbc["g_mix"]: e.scalar_tensor_tensor(out=nb[:], in0=xt[:], scalar=ss[:, 2:3], in1=gb[:], op0=ALU.mult, op1=ALU.mult),
                         reads=[xk, sk, "bc_g_mix"], writes=[nk])
                    st_["xts"].append((xt, xk))
                    st_["nbs"].append((nb, nk))
                ys = []
                for b3 in range(3):
                    y_, yk = yT[b3].next()
                    T.dma("sp", y_[:], ysrc[b3][:, :, i * TC:(i + 1) * TC], writes=[yk])
                    ys.append((y_, yk))
                st_["ys"] = ys
                return st_

            def c_s2(st_):
                nT, nTk = nTp.next()
                nTkeys = []
                for sub in range(NSC):
                    nb, nk = st_["nbs"][sub]
                    pt, pk = pools["ps"].next()
                    ptb = pt[:].bitcast(BF16)
                    T.group("pe", [(lambda e, ptb=ptb, nb=nb, k=k: e.transpose(out=ptb[:, k * 128:(k + 1) * 128], in_=nb[:, k * 128:(k + 1) * 128], identity=idb[:])) for k in range(8)],
                            reads=[nk, "idb"], writes=[pk])
                    dk = "%s_s%d" % (nTk, sub)
                    T.op("act", lambda e, ptb=ptb, nT=nT, sub=sub: e.activation(out=nT[:, :, sub * 128:(sub + 1) * 128], in_=ptb.rearrange("p (k t) -> p k t", k=8), func=AF.Copy),
                         reads=[pk], writes=[dk])
                    nTkeys.append(dk)
                ys = st_["ys"]
                m_, mk = mT.next()
                for c in range(8):
                    tbs = []
                    for b3 in range(3):
                        p, pk = psr.next()
                        mm(p[:, 0:TC], [(wg[:, k, b3 * 1024 + c * 128:b3 * 1024 + (c + 1) * 128], nT[:, k, :]) for k in range(8)], reads=wgkeys + nTkeys, writes=[pk])
                        g_, gk = gs.next()
                        T.op("act", lambda e, g_=g_, p=p: e.activation(out=g_[:], in_=p[:, 0:TC], func=AF.Sigmoid), reads=[pk], writes=[gk])
                        p2, p2k = psr.next()
                        mm(p2[:, 0:TC], [(wbr[:, b3, kc, c * 128:(c + 1) * 128], ys[b3][0][:, kc, :]) for kc in range(4)], reads=["wbr", ys[b3][1]], writes=[p2k])
                        t_, tk = tb.next()
                        T.op("dve", lambda e, t_=t_, p2=p2, g_=g_: e.tensor_tensor(out=t_[:], in0=p2[:, 0:TC], in1=g_[:], op=ALU.mult), reads=[p2k, gk], writes=[tk])
                        tbs.append((t_, tk))
                    T.op("pool", lambda e, a=tbs[0][0], b_=tbs[1][0]: e.tensor_tensor(out=a[:], in0=a[:], in1=b_[:], op=ALU.add), reads=[tbs[0][1], tbs[1][1]], writes=[tbs[0][1]])
                    T.op("pool", lambda e, a=tbs[0][0], b_=tbs[2][0], m_=m_, c=c: e.tensor_tensor(out=m_[:, c, :], in0=a[:], in1=b_[:], op=ALU.add),
                         reads=[tbs[0][1], tbs[2][1]], writes=["%s_c%d" % (mk, c)])
                st_["m"] = (m_, ["%s_c%d" % (mk, c) for c in range(8)])

            def c_s3(st_):
                i = st_["i"]
                m_, mkeys = st_["m"]
                st_["nfs"] = []
                for sub in range(NSC):
                    ti = i * NSC + sub
                    xt, xk = st_["xts"][sub]
                    h_, hk = hT.next()
                    for half in range(2):
                        p, pk = psr.next()
                        mm(p[:, :], [(m_[:, kc, sub * 128:(sub + 1) * 128], wo[:, kc, half * 512:(half + 1) * 512]) for kc in range(8)], reads=mkeys + ["wo"], writes=[pk])
                        T.op("dve", lambda e, h_=h_, p=p, xt=xt, half=half: e.tensor_tensor(out=h_[:, half * 512:(half + 1) * 512], in0=p[:, :], in1=xt[:, half * 512:(half + 1) * 512], op=ALU.add),
                             reads=[pk, xk], writes=[hk])
                    T.dma("sp", h_s[ti * 128:(ti + 1) * 128, :], h_[:], reads=[hk], writes=["h_s%d" % ti])
                    jk, jkk = pools["junk"].next()
                    ss, sk = pools["ss"].next()
                    T.op("act", lambda e, jk=jk, h_=h_, ss=ss: e.activation(out=jk[:], in_=h_[:], func=AF.Square, accum_out=ss[:, 0:1]), reads=[hk], writes=[sk, jkk])
                    T.op("dve", lambda e, ss=ss: e.tensor_scalar(out=ss[:, 1:2], in0=ss[:, 0:1], scalar1=1.0 / D, scalar2=EPS, op0=ALU.mult, op1=ALU.add), reads=[sk], writes=[sk])
                    T.op("pool", lambda e, ss=ss: e.tensor_tensor(out=ss[:, 2:3], in0=ss[:, 1:2], in1=mh[:, 0:1], op=ALU.pow), reads=[sk, "mh"], writes=[sk])
                    nf, nfk = n2f.next()
                    T.op("dve", lambda e, nf=nf, h_=h_, ss=ss: e.scalar_tensor_tensor(out=nf[:], in0=h_[:], scalar=ss[:, 2:3], in1=gbc["g_ffn"][:], op0=ALU.mult, op1=ALU.mult),
                         reads=[hk, sk, "bc_g_ffn"], writes=[nfk])
                    st_["nfs"].append((nf, nfk))

            def c_s4(st_):
                i = st_["i"]
                n2b, n2bk = n2Tb.next()
                for sub in range(NSC):
                    ti = i * NSC + sub
                    nf, nfk = st_["nfs"][sub]
                    ntf, ntfk = n2Tf.next()
                    for hh in range(2):
                        p, pk = psr.next()
                        T.group("pe", [(lambda e, p=p, nf=nf, hh=hh, k=k: e.transpose(out=p[:, k * 128:(k + 1) * 128], in_=nf[:, (hh * 4 + k) * 128:(hh * 4 + k + 1) * 128], identity=idf[:])) for k in range(4)],
                                reads=[nfk, "idf"], writes=[pk])
                        T.op("act", lambda e, p=p, ntf=ntf, hh=hh: e.activation(out=ntf[:, hh * 4:(hh + 1) * 4, :], in_=p[:, :].rearrange("p (k t) -> p k t", k=4), func=AF.Copy),
                             reads=[pk], writes=["%s_%d" % (ntfk, hh)])
                        T.op("dve", lambda e, p=p, n2b=n2b, hh=hh, sub=sub: e.tensor_copy(out=n2b[:, hh * 4:(hh + 1) * 4, sub * 128:(sub + 1) * 128], in_=p[:, :].rearrange("p (k t) -> p k t", k=4)),
                             reads=[pk], writes=["%s_%d_%d" % (n2bk, sub, hh)])
                    p, pk = psr.next()
                    mm(p[:, 0:36], [(ntf[:, k, :], wgr[:, k, :]) for k in range(8)], reads=["%s_0" % ntfk, "%s_1" % ntfk, "wgr"], writes=[pk])
                    T.op("dve", lambda e, p=p, ti=ti: e.tensor_tensor(out=lg[:, ti, :], in0=p[:, 0:36], in1=bgr_bc[:], op=ALU.add), reads=[pk, "bgr_bc"], writes=["lg%d" % ti])
                T.dma("sp", n2T_v[:, :, i * TC:(i + 1) * TC], n2b[:], reads=["%s_%d_%d" % (n2bk, s_, hh) for s_ in range(NSC) for hh in range(2)], writes=["n2T_s%d" % i])

            cur_c = c_s1(0)
            prev_c = None
            for t in range(NBC):
                c_s2(cur_c)
                nxt_c = c_s1(t + 1) if t + 1 < NBC else None
                if prev_c is not None:
                    c_s4(prev_c)
                c_s3(cur_c)
                prev_c = cur_c
                cur_c = nxt_c
            c_s4(prev_c)
            T.barrier()
            if stop == 4:
                T.finish_early()

        stCW.close()
        with ExitStack() as st:
            lgk = ["lg%d" % t for t in range(NT)]
            R = lambda nm, shp: SB(st, nm, shp, F32)
            gm = R("r_gm", [128, NT]); ge = R("r_ge", [128, NT, 4]); gsum = R("r_gsum", [128, NT]); gw = R("r_gw", [128, NT])
            mg = R("r_mg", [128, NT, 4]); eg = R("r_eg", [128, NT, 8]); tmp8 = R("r_tmp8", [128, NT, 8])
            m1 = R("r_m1", [128, NT]); m2 = R("r_m2", [128, NT]); sel = R("r_sel", [128, NT, 8]); pe_ = R("r_pe", [128, NT, 8]); psum_ = R("r_ps", [128, NT])
            glv = lg[:, :, 0:4]
            elv = lg[:, :, 4:36].rearrange("p t (g e) -> p t g e", g=4)

            def bc(ap2, n):
                return ap2.unsqueeze(2).to_broadcast([128, NT, n])
            T.op("dve", lambda e: e.tensor_reduce(out=gm[:], in_=glv, axis=AX.X, op=ALU.max), reads=lgk, writes=["gm"])
            T.op("dve", lambda e: e.tensor_tensor(out=ge[:], in0=glv, in1=bc(gm[:], 4), op=ALU.subtract), reads=lgk + ["gm"], writes=["ge"])
            T.op("dve", lambda e: e.tensor_tensor(out=mg[:], in0=glv, in1=bc(gm[:], 4), op=ALU.is_equal), reads=lgk + ["gm"], writes=["mg"])
            T.op("act", lambda e: e.activation(out=ge[:], in_=ge[:], func=AF.Exp), reads=["ge"], writes=["ge"])
            T.op("dve", lambda e: e.tensor_reduce(out=gsum[:], in_=ge[:], axis=AX.X, op=ALU.add), reads=["ge"], writes=["gsum"])
            T.op("dve", lambda e: e.reciprocal(out=gw[:], in_=gsum[:]), reads=["gsum"], writes=["gw"])
            T.op("dve", lambda e: e.tensor_tensor(out=eg[:], in0=elv[:, :, 0, :], in1=bc(mg[:, :, 0], 8), op=ALU.mult), reads=lgk + ["mg"], writes=["eg"])
            for g in range(1, 4):
                T.op("dve", lambda e, g=g: e.tensor_tensor(out=tmp8[:], in0=elv[:, :, g, :], in1=bc(mg[:, :, g], 8), op=ALU.mult), reads=lgk + ["mg"], writes=["tmp8"])
                T.op("dve", lambda e: e.tensor_tensor(out=eg[:], in0=eg[:], in1=tmp8[:], op=ALU.add), reads=["eg", "tmp8"], writes=["eg"])
            T.op("dve", lambda e: e.tensor_reduce(out=m1[:], in_=eg[:], axis=AX.X, op=ALU.max), reads=["eg"], writes=["m1"])
            T.op("dve", lambda e: e.tensor_tensor(out=tmp8[:], in0=eg[:], in1=bc(m1[:], 8), op=ALU.is_equal), reads=["eg", "m1"], writes=["tmp8"])
            T.op("dve", lambda e: e.scalar_tensor_tensor(out=tmp8[:], in0=tmp8[:], scalar=-1.0e30, in1=eg[:], op0=ALU.mult, op1=ALU.add), reads=["tmp8", "eg"], writes=["tmp8"])
            T.op("dve", lambda e: e.tensor_reduce(out=m2[:], in_=tmp8[:], axis=AX.X, op=ALU.max), reads=["tmp8"], writes=["m2"])
            T.op("dve", lambda e: e.tensor_tensor(out=sel[:], in0=eg[:], in1=bc(m2[:], 8), op=ALU.is_ge), reads=["eg", "m2"], writes=["sel"])
            T.op("dve", lambda e: e.tensor_tensor(out=pe_[:], in0=eg[:], in1=bc(m1[:], 8), op=ALU.subtract), reads=["eg", "m1"], writes=["pe_"])
            T.op("act", lambda e: e.activation(out=pe_[:], in_=pe_[:], func=AF.Exp), reads=["pe_"], writes=["pe_"])
            T.op("dve", lambda e: e.tensor_tensor(out=pe_[:], in0=pe_[:], in1=sel[:], op=ALU.mult), reads=["pe_", "sel"], writes=["pe_"])
            T.op("dve", lambda e: e.tensor_reduce(out=psum_[:], in_=pe_[:], axis=AX.X, op=ALU.add), reads=["pe_"], writes=["psum_"])
            T.op("dve", lambda e: e.reciprocal(out=psum_[:], in_=psum_[:]), reads=["psum_"], writes=["psum_"])
            T.op("dve", lambda e: e.tensor_tensor(out=psum_[:], in0=psum_[:], in1=gw[:], op=ALU.mult), reads=["psum_", "gw"], writes=["psum_"])
            T.op("dve", lambda e: e.tensor_tensor(out=pe_[:], in0=pe_[:], in1=bc(psum_[:], 8), op=ALU.mult), reads=["pe_", "psum_"], writes=["pe_"])
            cv = comb[:].rearrange("p t (g e) -> p t g e", g=4)
            for g in range(4):
                T.op("dve", lambda e, g=g: e.tensor_tensor(out=cv[:, :, g, :], in0=pe_[:], in1=bc(mg[:, :, g], 8), op=ALU.mult), reads=["pe_", "mg"], writes=["comb"])
            if dbg:
                T.dma("sp", comb_s, comb[:], reads=["comb"], writes=["comb_s"])
            T.barrier()
            if stop == 5:
                T.finish_early()

        with ExitStack() as st:
            load_g(st, "g_fin", "d")
            TH = S // NHALF
            NS = TH // 128
            NBH = TH // 512
            n2T = SB(st, "n2T", [128, 8, TH], BF16)
            acc = SB(st, "acc", [128, NS, D], F32)
            stg = [SB(st, "stg%d" % i, [128, 8 * 256], F32) for i in range(3)]
            wgb = [SB(st, "wgb%d" % i, [128, 8, 256], BF16) for i in range(2)]
            wub = [SB(st, "wub%d" % i, [128, 8, 256], BF16) for i in range(2)]
            wdb = [SB(st, "wdb%d" % i, [128, 2, D], BF16) for i in range(2)]
            sg = Rot([SB(st, "sg%d" % i, [128, 512], F32) for i in range(2)], "sg")
            hid = Rot([SB(st, "hid%d" % i, [128, 2, 512], BF16) for i in range(2)], "hid")
            ot = Rot([SB(st, "ot%d" % i, [128, D], F32) for i in range(1)], "ot")
            jkp = Rot([SB(st, "djk", [128, D], BF16)], "djk")
            ssp = Rot([SB(st, "dss%d" % i, [128, 4], F32) for i in range(4)], "dss")
            psr = Rot(PS[0:8], "psd")
            n2T_v = n2T_s.rearrange("(k p) t -> p k t", p=128)
            for hf in range(NHALF):
                t0 = hf * TH
                for k in range(8):
                    T.dma("sp", n2T[:, k, :], n2T_v[:, k, t0:t0 + TH], writes=["n2T_k%d" % k])
                n2keys = ["n2T_k%d" % k for k in range(8)]
                def d_weights(ex):
                    b = ex % 2
                    T.dma("sp", stg[0][:].rearrange("p (k c) -> p k c", k=8), w_eg[ex].rearrange("(k p) c -> p k c", p=128), writes=["stg0"])
                    T.op("pool", lambda e, b=b: e.tensor_copy(out=wgb[b][:], in_=stg[0][:].rearrange("p (k c) -> p k c", k=8)), reads=["stg0"], writes=["wgb%d" % b])
                    T.dma("sp", stg[1][:].rearrange("p (k c) -> p k c", k=8), w_eu[ex].rearrange("(k p) c -> p k c", p=128), writes=["stg1"])
                    T.op("pool", lambda e, b=b: e.tensor_copy(out=wub[b][:], in_=stg[1][:].rearrange("p (k c) -> p k c", k=8)), reads=["stg1"], writes=["wub%d" % b])
                    T.dma("sp", stg[2][:].rearrange("p (k c) -> p k c", k=2), w_ed[ex].rearrange("(k p) c -> p k c", p=128), writes=["stg2"])
                    T.op("pool", lambda e, b=b: e.tensor_copy(out=wdb[b][:], in_=stg[2][:].rearrange("p (k c) -> p k c", k=2)), reads=["stg2"], writes=["wdb%d" % b])
                    if ex == 0:
                        for s_ in range(NS):
                            T.dma("sp", acc[:, s_, :], h_s[t0 + s_ * 128:t0 + (s_ + 1) * 128, :], writes=["acc%d_0" % s_, "acc%d_1" % s_])

                def d_up(ex, t):
                    b = ex % 2
                    hd, hdk = hid.next()
                    for oc in range(2):
                        pg, pgk = psr.next()
                        mm(pg[:, :], [(wgb[b][:, k, oc * 128:(oc + 1) * 128], n2T[:, k, t * 512:(t + 1) * 512]) for k in range(8)], reads=["wgb%d" % b] + n2keys, writes=[pgk])
                        pu, puk = psr.next()
                        mm(pu[:, :], [(wub[b][:, k, oc * 128:(oc + 1) * 128], n2T[:, k, t * 512:(t + 1) * 512]) for k in range(8)], reads=["wub%d" % b] + n2keys, writes=[puk])
                        s1, s1k = sg.next()
                        T.op("act", lambda e, s1=s1, pg=pg: e.activation(out=s1[:], in_=pg[:, :], func=AF.Silu), reads=[pgk], writes=[s1k])
                        T.op("dve", lambda e, s1=s1, pu=pu, hd=hd, oc=oc: e.tensor_tensor(out=hd[:, oc, :], in0=pu[:, :], in1=s1[:], op=ALU.mult), reads=[puk, s1k], writes=["%s_%d" % (hdk, oc)])
                    return hd, hdk

                def d_down(ex, t, hd, hdk):
                    b = ex % 2
                    for sub in range(4):
                        s_ = t * 4 + sub
                        ti = hf * NS + s_
                        for half in range(2):
                            pd, pdk = psr.next()
                            mm(pd[:, :], [(hd[:, jc, sub * 128:(sub + 1) * 128], wdb[b][:, jc, half * 512:(half + 1) * 512]) for jc in range(2)],
                               reads=["%s_0" % hdk, "%s_1" % hdk, "wdb%d" % b], writes=[pdk])
                            ak = "acc%d_%d" % (s_, half)
                            T.op("dve", lambda e, pd=pd, s_=s_, half=half, ti=ti, ex=ex: e.scalar_tensor_tensor(
                                out=acc[:, s_, half * 512:(half + 1) * 512], in0=pd[:, :], scalar=comb[:, ti, ex:ex + 1],
                                in1=acc[:, s_, half * 512:(half + 1) * 512], op0=ALU.mult, op1=ALU.add), reads=[pdk, "comb", ak], writes=[ak])

                items_d = [(ex, t) for ex in range(NE) for t in range(NBH)]
                d_weights(0)
                nxt_d = d_up(*items_d[0])
                for n, (ex, t) in enumerate(items_d):
                    cur_d = nxt_d
                    if n + 1 < len(items_d):
                        ex2, t2 = items_d[n + 1]
                        if t2 == 0:
                            d_weights(ex2)
                        nxt_d = d_up(ex2, t2)
                    d_down(ex, t, *cur_d)
                for s_ in range(NS):
                    aks = ["acc%d_0" % s_, "acc%d_1" % s_]
                    jk, jkk = jkp.next()
                    ss, sk = ssp.next()
                    T.op("act", lambda e, jk=jk, s_=s_, ss=ss: e.activation(out=jk[:], in_=acc[:, s_, :], func=AF.Square, accum_out=ss[:, 0:1]), reads=aks, writes=[sk, jkk])
                    T.op("dve", lambda e, ss=ss: e.tensor_scalar(out=ss[:, 1:2], in0=ss[:, 0:1], scalar1=1.0 / D, scalar2=EPS, op0=ALU.mult, op1=ALU.add), reads=[sk], writes=[sk])
                    T.op("pool", lambda e, ss=ss: e.tensor_tensor(out=ss[:, 2:3], in0=ss[:, 1:2], in1=mh[:, 0:1], op=ALU.pow), reads=[sk, "mh"], writes=[sk])
                    o_, ok = ot.next()
                    T.op("dve", lambda e, o_=o_, s_=s_, ss=ss: e.scalar_tensor_tensor(out=o_[:], in0=acc[:, s_, :], scalar=ss[:, 2:3], in1=gbc["g_fin"][:], op0=ALU.mult, op1=ALU.mult),
                         reads=aks + [sk, "bc_g_fin"], writes=[ok])
                    T.dma("sp", out[t0 + s_ * 128:t0 + (s_ + 1) * 128, :], o_[:], reads=[ok], writes=["out%d" % (t0 // 128 + s_)])

        with nc.Block() as block:
            T.finish(block)
    return nc


def _host_inputs(inputs):
    f = lambda a: np.ascontiguousarray(np.asarray(a))
    x = f(inputs["x"]); mem = f(inputs["mem"]); positions = f(inputs["positions"])
    B = x.shape[0]
    shared = {
        "g_mix": f(inputs["g_mix"])[0], "g_mem": f(inputs["g_mem"])[0], "g_ffn": f(inputs["g_ffn"])[0], "g_fin": f(inputs["g_final"]),
        "g_q": f(inputs["g_q"])[0], "g_kv": f(inputs["g_kv"])[0],
        "w_in": f(inputs["w_in"])[0],
        "cw": f(f(inputs["conv_w"])[0].T.reshape(4, 128, 4).transpose(1, 0, 2)),
        "cb": f(f(inputs["conv_b"])[0].reshape(4, 128).T),
        "lba": f(f(inputs["lru_ba"])[0].reshape(4, 128).T),
        "lbx": f(f(inputs["lru_bx"])[0].reshape(4, 128).T),
        "llam": f(f(inputs["lru_lambda"])[0].reshape(4, 128).T),
        "lwa": f(inputs["lru_wa"])[0], "lwx": f(inputs["lru_wx"])[0],
        "w_uq": f(inputs["w_uq"])[0], "w_ukv": f(inputs["w_ukv"])[0], "w_mkv": f(inputs["w_mem_kv"])[0],
        "w_br": f(inputs["w_branch"])[0], "w_o": f(inputs["w_o"])[0],
        "w_gr": f(np.concatenate([f(inputs["w_group"])[0], f(inputs["w_router"])[0]], axis=1)),
        "b_gr": f(np.concatenate([f(inputs["b_group"])[0], f(inputs["b_router"])[0]], axis=0)),
        "w_eg": f(inputs["w_e_gate"])[0], "w_eu": f(inputs["w_e_up"])[0], "w_ed": f(inputs["w_e_down"])[0],
    }
    ident = np.eye(128, dtype=np.float32)
    kk = np.arange(128)[:, None]; qq = np.arange(512)[None, :]
    cmask = np.stack([((128 * r + kk) <= qq).astype(np.float32) for r in range(4)], axis=0)
    invf = np.broadcast_to((10000.0 ** (-np.arange(0, 32, 2, dtype=np.float32) / 32.0)).astype(np.float32)[None, :], (128, 16)).copy()
    shared.update({"ident": ident, "cmask": cmask, "invf": invf})
    shared = {k: np.ascontiguousarray(v, dtype=np.float32) for k, v in shared.items()}
    maps = []
    for b in range(B):
        m = dict(shared)
        m["x"] = x[b]
        m["mem"] = mem[b]
        m["pos"] = np.ascontiguousarray(positions[b].reshape(NT, 128).T.astype(np.int32))
        maps.append(m)
    return maps


_NC_CACHE = {}


def kernel(**inputs):
    maps = _host_inputs(inputs)
    if "nc" not in _NC_CACHE:
        _NC_CACHE["nc"] = build_nc(False)
    nc = _NC_CACHE["nc"]
    res = run_bass_kernel_spmd(nc, maps, core_ids=list(range(len(maps))))
    return np.stack([np.asarray(r["out"], dtype=np.float32) for r in res.results], axis=0)
```

```python
import math
import numpy as np
from contextlib import ExitStack
import concourse.bass as bass
import concourse.mybir as mybir
from concourse.bass_utils import run_bass_kernel_spmd

F32 = mybir.dt.float32
BF16 = mybir.dt.bfloat16
I32 = mybir.dt.int32
AF = mybir.ActivationFunctionType
ALU = mybir.AluOpType
AX = mybir.AxisListType

S = 4096
D = 1024
NT = S // 128
NB = S // 512
EPS = 1e-6
DIN = 5024
NE = 32
NHALF = 2


class Trk:
    ENG = ("pe", "act", "dve", "pool", "sp")
    SELF = True
    STEP = 0
    FINAL = None

    def __init__(self, nc, stack, n_dma_sems=12):
        self.nc = nc
        self.prog = {e: [] for e in self.ENG}
        self.sem = {e: stack.enter_context(nc.semaphore("sem_" + e)) for e in self.ENG}
        self.nops = {e: 0 for e in self.ENG}
        self.waited = {e: set() for e in self.ENG}
        self.known_c = {e: {e2: 0 for e2 in self.ENG} for e in self.ENG}
        self.known_d = {e: {} for e in self.ENG}
        self.dsem = {}
        self.drr = {}
        self.dcnt = {}
        self.dobj = {}
        for q in ("sp", "pool", "act"):
            self.dsem[q] = [stack.enter_context(nc.semaphore("dsem_%s_%d" % (q, i))) for i in range(n_dma_sems)]
            self.drr[q] = 0
            for s in self.dsem[q]:
                self.dcnt[id(s)] = 0
                self.dobj[id(s)] = s
        self.tiles = {}
        self.mute = False

    def _st(self, k):
        st = self.tiles.get(k)
        if st is None:
            st = {"w": None, "r": []}
            self.tiles[k] = st
        return st

    def _deps(self, reads, writes):
        evs = []
        for k in reads:
            st = self._st(k)
            if st["w"] is not None:
                evs.append(st["w"])
            if k.startswith("ps"):
                evs.extend(st["r"])
        for k in writes:
            st = self._st(k)
            if st["w"] is not None:
                evs.append(st["w"])
            evs.extend(st["r"])
        return evs

    def _emit_waits(self, e, evs, is_dma=False):
        cmax = {}
        dmax = {}
        for ev in evs:
            if ev[0] == "c":
                _, e2, idx = ev
                if e2 == e and (e == "pe" or (not is_dma and not self.SELF)):
                    continue
                if cmax.get(e2, 0) < idx:
                    cmax[e2] = idx
            else:
                _, sid, v = ev
                if dmax.get(sid, 0) < v:
                    dmax[sid] = v
        for e2, idx in cmax.items():
            if self.known_c[e][e2] >= idx:
                continue
            k0 = self.known_c[e][e2]
            if self.STEP and e2 != e:
                for v in range(k0 + self.STEP, idx, self.STEP):
                    self.waited[e2].add(v)
                    self.prog[e].append(("wc", e2, v))
            self.known_c[e][e2] = idx
            self.waited[e2].add(idx)
            self.prog[e].append(("wc", e2, idx))
        for sid, v in dmax.items():
            if self.known_d[e].get(sid, 0) >= v:
                continue
            self.known_d[e][sid] = v
            self.prog[e].append(("wd", sid, v))

    def _mark(self, ev, reads, writes):
        for k in reads:
            r = self._st(k)["r"]
            r.append(ev)
            if len(r) > 24:
                best = {}
                for x in r:
                    key = (x[0], x[1])
                    if key not in best or best[key][2] < x[2]:
                        best[key] = x
                r[:] = list(best.values())
        for k in writes:
            st = self._st(k)
            st["w"] = ev
            st["r"] = []

    def op(self, e, fn, reads=(), writes=()):
        return self.group(e, [fn], reads, writes)

    def group(self, e, fns, reads=(), writes=()):
        if self.mute:
            return None
        self._emit_waits(e, self._deps(reads, writes))
        self.nops[e] += 1
        idx = self.nops[e]
        self.prog[e].append(("op", list(fns), idx))
        ev = ("c", e, idx)
        self._mark(ev, reads, writes)
        return ev

    def dma(self, q, out, in_, reads=(), writes=(), **kw):
        if self.mute:
            return None
        pool = self.dsem[q]
        s = pool[self.drr[q] % len(pool)]
        self.drr[q] += 1
        sid = id(s)
        evs = self._deps(reads, writes)
        if self.dcnt[sid] > 0:
            evs.append(("d", sid, self.dcnt[sid]))
        self._emit_waits(q, evs, is_dma=True)
        self.dcnt[sid] += 16
        ev = ("d", sid, self.dcnt[sid])
        self.prog[q].append(("dma", s, out, in_, kw))
        self._mark(ev, reads, writes)
        return ev

    def barrier(self, engines=None):
        if self.mute:
            return
        evs = [("c", e2, self.nops[e2]) for e2 in self.ENG if self.nops[e2] > 0]
        evs += [("d", sid, v) for sid, v in self.dcnt.items() if v > 0]
        for e in (engines or self.ENG):
            self._emit_waits(e, evs)
        self.tiles = {}

    def finish_early(self):
        self.barrier(self.FINAL)
        self.mute = True

    def finish(self, block):
        self.mute = False
        self.barrier(self.FINAL)
        rank = {e: {idx: i + 1 for i, idx in enumerate(sorted(self.waited[e]))} for e in self.ENG}
        t = self

        def run(e, en):
            for it in t.prog[e]:
                if it[0] == "wc":
                    en.wait_ge(t.sem[it[1]], rank[it[1]][it[2]])
                elif it[0] == "wd":
                    en.wait_ge(t.dobj[it[1]], it[2])
                elif it[0] == "dma":
                    en.dma_start(out=it[2], in_=it[3], **it[4]).then_inc(it[1], 16)
                else:
                    fns, idx = it[1], it[2]
                    for i, fn in enumerate(fns):
                        r = fn(en)
                        if i == len(fns) - 1 and idx in rank[e]:
                            r.then_inc(t.sem[e], 1)

        @block.sync
        def _(en):
            run("sp", en)

        @block.tensor
        def _(en):
            run("pe", en)

        @block.scalar
        def _(en):
            run("act", en)

        @block.vector
        def _(en):
            run("dve", en)

        @block.gpsimd
        def _(en):
            run("pool", en)


class Rot:
    def __init__(self, bufs, name, shared=False):
        self.bufs = bufs
        self.name = name
        self.i = 0
        self.shared = shared

    def next(self):
        j = self.i % len(self.bufs)
        self.i += 1
        return self.bufs[j], "%s#%d" % (self.name, 0 if self.shared else j)


def build_nc(dbg=False, stop=99, substop=99):
    EXP = ""
    nc = bass.Bass("TRN2", target_bir_lowering=False)

    def din(name, shape, dt=F32):
        return nc.dram_tensor(name, list(shape), dt, kind="ExternalInput").ap()

    def dscr(name, shape, dt):
        return nc.dram_tensor(name, list(shape), dt, kind=("ExternalOutput" if dbg else "Internal")).ap()

    x = din("x", [S, D])
    mem = din("mem", [256, D])
    pos = din("pos", [128, NT], I32)
    g_mix = din("g_mix", [D]); g_mem = din("g_mem", [D]); g_ffn = din("g_ffn", [D]); g_fin = din("g_fin", [D])
    g_q = din("g_q", [256]); g_kv = din("g_kv", [128])
    w_in = din("w_in", [D, DIN])
    cw = din("cw", [128, 4, 4]); cb = din("cb", [128, 4]); lba = din("lba", [128, 4]); lbx = din("lbx", [128, 4]); llam = din("llam", [128, 4])
    lwa = din("lwa", [8, 64, 64]); lwx = din("lwx", [8, 64, 64])
    w_uq = din("w_uq", [256, 768]); w_ukv = din("w_ukv", [128, 1024]); w_mkv = din("w_mkv", [D, D])
    w_br = din("w_br", [3, 512, D]); w_o = din("w_o", [D, D])
    w_gr = din("w_gr", [D, 36]); b_gr = din("b_gr", [36])
    w_eg = din("w_eg", [NE, D, 256]); w_eu = din("w_eu", [NE, D, 256]); w_ed = din("w_ed", [NE, 256, D])
    ident = din("ident", [128, 128]); cmask = din("cmask", [4, 128, 512]); invf = din("invf", [128, 16])
    out = nc.dram_tensor("out", [S, D], F32, kind="ExternalOutput").ap()

    ya_s = dscr("ya_s", [512, S], BF16); yb_s = dscr("yb_s", [512, S], BF16); yc_s = dscr("yc_s", [512, S], BF16)
    h_s = dscr("h_s", [S, D], F32); n2T_s = dscr("n2T_s", [D, S], BF16)
    comb_s = dscr("comb_s", [128, NT, NE], F32) if dbg else None

    with ExitStack() as top:
        T = Trk(nc, top)

        def SB(st, name, shape, dt):
            return st.enter_context(nc.sbuf_tensor(name, list(shape), dt))

        PS = [top.enter_context(nc.psum_tensor("ps%d" % i, [128, 512], F32)) for i in range(8)]

        def mm(out_ap, pairs, reads, writes):
            n = len(pairs)
            fns = []
            for i, (l, r) in enumerate(pairs):
                fns.append(lambda e, l=l, r=r, i=i: e.matmul(out_ap, lhsT=l, rhs=r, start=(i == 0), stop=(i == n - 1)))
            return T.group("pe", fns, reads, writes)

        idf = SB(top, "idf", [128, 128], F32)
        idb = SB(top, "idb", [128, 128], BF16)
        onesb = SB(top, "onesb", [128, 128], BF16)
        mh = SB(top, "mh", [128, 2], F32)
        T.dma("sp", idf[:], ident, writes=["idf"])
        T.dma("pool", idb[:], ident, writes=["idb"])
        T.op("dve", lambda e: e.memset(onesb[:], 1.0), writes=["onesb"])
        T.op("dve", lambda e: e.memset(mh[:], -0.5), writes=["mh"])
        gbc = {}
        gsrc = {"g_mix": g_mix, "g_mem": g_mem, "g_ffn": g_ffn, "g_fin": g_fin}

        def load_g(st_, nm, tag):
            t_ = SB(st_, "bc_%s_%s" % (nm, tag), [128, D], F32)
            T.dma("sp", t_[:], gsrc[nm].partition_broadcast(128), writes=["bc_" + nm])
            gbc[nm] = t_
        gq_bc = SB(top, "gq_bc", [128, 256], F32); T.dma("sp", gq_bc[:], g_q.partition_broadcast(128), writes=["gq_bc"])
        gkv_bc = SB(top, "gkv_bc", [128, 128], F32); T.dma("sp", gkv_bc[:], g_kv.partition_broadcast(128), writes=["gkv_bc"])
        bgr_bc = SB(top, "bgr_bc", [128, 36], F32); T.dma("sp", bgr_bc[:], b_gr.partition_broadcast(128), writes=["bgr_bc"])
        stAB = ExitStack()
        cosT = SB(stAB, "cosT", [128, NT, 16], F32)
        sinT = SB(stAB, "sinT", [128, NT, 16], F32)
        cqnT = SB(stAB, "cqnT", [128, 2, S], BF16)
        ckvnT = SB(stAB, "ckvnT", [128, S], BF16)
        kropeT = SB(stAB, "kropeT", [96, S], BF16)

        with ExitStack() as st:
            posi = SB(st, "posi", [128, NT], I32)
            posf = SB(st, "posf", [128, NT], F32)
            ivf = SB(st, "ivf", [128, 16], F32)
            ang = SB(st, "ang", [128, NT, 16], F32)
            kf = SB(st, "kf", [128, NT, 16], F32)
            ki = SB(st, "ki", [128, NT, 16], I32)
            T.dma("sp", posi[:], pos, writes=["posi"])
            T.dma("sp", ivf[:], invf, writes=["ivf"])
            T.op("dve", lambda e: e.tensor_copy(out=posf[:], in_=posi[:]), reads=["posi"], writes=["posf"])
            T.op("dve", lambda e: e.tensor_tensor(out=ang[:], in0=posf[:].unsqueeze(2).to_broadcast([128, NT, 16]),
                                                  in1=ivf[:].unsqueeze(1).to_broadcast([128, NT, 16]), op=ALU.mult),
                 reads=["posf", "ivf"], writes=["ang"])
            TWO_PI = 2.0 * math.pi
            for shift, dst, nm in ((0.0, sinT, "sinT"), (math.pi / 2.0, cosT, "cosT")):
                T.op("dve", lambda e, shift=shift: e.tensor_scalar(out=kf[:], in0=ang[:], scalar1=shift, scalar2=1.0 / TWO_PI, op0=ALU.add, op1=ALU.mult),
                     reads=["ang"], writes=["kf"])
                T.op("dve", lambda e: e.tensor_copy(out=ki[:], in_=kf[:]), reads=["kf"], writes=["ki"])
                T.op("dve", lambda e: e.tensor_copy(out=kf[:], in_=ki[:]), reads=["ki"], writes=["kf"])
                T.op("dve", lambda e, shift=shift: e.tensor_scalar(out=kf[:], in0=kf[:], scalar1=-TWO_PI, scalar2=shift, op0=ALU.mult, op1=ALU.add),
                     reads=["kf"], writes=["kf"])
                T.op("dve", lambda e: e.tensor_tensor(out=kf[:], in0=kf[:], in1=ang[:], op=ALU.add), reads=["kf", "ang"], writes=["kf"])
                T.op("dve", lambda e: e.tensor_scalar(out=kf[:], in0=kf[:], scalar1=math.pi, scalar2=-math.pi, op0=ALU.min, op1=ALU.max),
                     reads=["kf"], writes=["kf"])
                T.op("act", lambda e, dst=dst: e.activation(out=dst[:], in_=kf[:], func=AF.Sin), reads=["kf"], writes=[nm])
            T.barrier()
            if stop == 0:
                T.finish_early()

        def norm_T(st_pools, src_rows, gb, gkey, dstT, dst_cols, dkey, keep_x=None):
            xt, xk = st_pools["xt"].next() if keep_x is None else keep_x
            T.dma("sp", xt[:], src_rows, writes=[xk])
            jk, jkk = st_pools["junk"].next()
            ss, sk = st_pools["ss"].next()
            T.op("act", lambda e: e.activation(out=jk[:], in_=xt[:], func=AF.Square, accum_out=ss[:, 0:1]), reads=[xk], writes=[sk, jkk])
            T.op("dve", lambda e: e.tensor_scalar(out=ss[:, 1:2], in0=ss[:, 0:1], scalar1=1.0 / D, scalar2=EPS, op0=ALU.mult, op1=ALU.add), reads=[sk], writes=[sk])
            T.op("pool", lambda e: e.tensor_tensor(out=ss[:, 2:3], in0=ss[:, 1:2], in1=mh[:, 0:1], op=ALU.pow), reads=[sk, "mh"], writes=[sk])
            nb, nk = st_pools["nb"].next()
            T.op("dve", lambda e: e.scalar_tensor_tensor(out=nb[:], in0=xt[:], scalar=ss[:, 2:3], in1=gb[:], op0=ALU.mult, op1=ALU.mult),
                 reads=[xk, sk, gkey], writes=[nk])
            pt, pk = st_pools["ps"].next()
            ptb = pt[:].bitcast(BF16)
            T.group("pe", [(lambda e, k=k: e.transpose(out=ptb[:, k * 128:(k + 1) * 128], in_=nb[:, k * 128:(k + 1) * 128], identity=idb[:])) for k in range(8)],
                    reads=[nk, "idb"], writes=[pk])
            T.op("act", lambda e: e.activation(out=dstT[:, :, dst_cols], in_=ptb.rearrange("p (k t) -> p k t", k=8), func=AF.Copy),
                 reads=[pk], writes=[dkey])
            return xt, xk

        kmemT = SB(stAB, "kmemT", [128, 4, 256], BF16)
        vmem = SB(stAB, "vmem", [128, 2, 512], BF16)
        with ExitStack() as st:
            load_g(st, "g_mem", "a0")
            pools = {
                "xt": Rot([SB(st, "a0xt%d" % i, [128, D], F32) for i in range(2)], "a0xt"),
                "junk": Rot([SB(st, "a0jk", [128, D], BF16)], "a0jk"),
                "ss": Rot([SB(st, "a0ss%d" % i, [128, 4], F32) for i in range(2)], "a0ss"),
                "nb": Rot([SB(st, "a0nb%d" % i, [128, D], BF16) for i in range(2)], "a0nb"),
                "ps": Rot(PS[0:4], "ps"),
            }
            psr = Rot(PS[4:8], "psb")
            memT = SB(st, "memT", [128, 8, 256], BF16)
            wmk = SB(st, "wmk", [128, 8, D], BF16)
            for k in range(8):
                T.dma("pool", wmk[:, k, :], w_mkv[k * 128:(k + 1) * 128, :], writes=["wmk%d" % k])
            for mt in range(2):
                norm_T(pools, mem[mt * 128:(mt + 1) * 128, :], gbc["g_mem"], "bc_g_mem", memT, slice(mt * 128, (mt + 1) * 128), "memT%d" % mt)
            wk = ["wmk%d" % k for k in range(8)]
            for h in range(4):
                p, pk = psr.next()
                mm(p[:, 0:256], [(wmk[:, k, h * 128:(h + 1) * 128], memT[:, k, :]) for k in range(8)], reads=wk + ["memT0", "memT1"], writes=[pk])
                T.op("act", lambda e, p=p, h=h: e.activation(out=kmemT[:, h, :], in_=p[:, 0:256], func=AF.Copy), reads=[pk], writes=["kmemT"])
            for mt in range(2):
                p, pk = psr.next()
                mm(p[:, :], [(memT[:, k, mt * 128:(mt + 1) * 128], wmk[:, k, 512:1024]) for k in range(8)], reads=wk + ["memT%d" % mt], writes=[pk])
                T.op("act", lambda e, p=p, mt=mt: e.activation(out=vmem[:, mt, :], in_=p[:, :], func=AF.Copy), reads=[pk], writes=["vmem"])
            T.barrier()
            if stop == 1:
                T.finish_early()

        with ExitStack() as st:
            load_g(st, "g_mix", "a")
            pools = {
                "xt": Rot([SB(st, "axt%d" % i, [128, D], F32) for i in range(4)], "axt"),
                "junk": Rot([SB(st, "ajk", [128, D], BF16)], "ajk"),
                "ss": Rot([SB(st, "ass%d" % i, [128, 4], F32) for i in range(8)], "ass"),
                "nb": Rot([SB(st, "anb%d" % i, [128, D], BF16) for i in range(4)], "anb"),
                "ps": Rot(PS[0:2], "psn"),
            }
            psr = Rot(PS[2:8], "psa")
            NA = 1952
            win = SB(st, "win", [128, 8, NA], BF16)
            for k in range(8):
                for (c0, c1) in ((0, 1024), (1024, NA)):
                    T.dma("pool", win[:, k, c0:c1], w_in[k * 128:(k + 1) * 128, c0:c1], writes=["win%d_%d" % (k, c0)])
            winkeys = ["win%d_%d" % (k, c0) for k in range(8) for c0 in (0, 1024)]
            wabd = SB(st, "wabd", [128, 4, 128], BF16)
            wxbd = SB(st, "wxbd", [128, 4, 128], BF16)
            T.op("dve", lambda e: e.memset(wabd[:], 0.0), writes=["wabd"])
            T.op("dve", lambda e: e.memset(wxbd[:], 0.0), writes=["wxbd"])
            for c in range(4):
                for j in range(2):
                    T.dma("pool", wabd[j * 64:(j + 1) * 64, c, j * 64:(j + 1) * 64], lwa[2 * c + j], reads=["wabd"], writes=["wabd"])
                    T.dma("pool", wxbd[j * 64:(j + 1) * 64, c, j * 64:(j + 1) * 64], lwx[2 * c + j], reads=["wxbd"], writes=["wxbd"])
            cws = SB(st, "cws", [128, 4, 4], F32); T.dma("sp", cws[:], cw, writes=["cws"])
            cbs = SB(st, "cbs", [128, 4], F32); T.dma("sp", cbs[:], cb, writes=["cbs"])
            bas = SB(st, "bas", [128, 4], F32); T.dma("sp", bas[:], lba, writes=["bas"])
            bxs = SB(st, "bxs", [128, 4], F32); T.dma("sp", bxs[:], lbx, writes=["bxs"])
            lam = SB(st, "lam", [128, 4], F32); T.dma("sp", lam[:], llam, writes=["lam"])
            sp1 = SB(st, "sp1", [128, 4], F32); sp2 = SB(st, "sp2", [128, 4], F32); c1s = SB(st, "c1s", [128, 4], F32)
            T.op("dve", lambda e: e.tensor_scalar(out=sp1[:], in0=lam[:], scalar1=-1.0, scalar2=None, op0=ALU.mult), reads=["lam"], writes=["sp1"])
            T.op("dve", lambda e: e.tensor_tensor(out=sp1[:], in0=sp1[:], in1=lam[:], op=ALU.max), reads=["lam", "sp1"], writes=["sp1"])
            T.op("act", lambda e: e.activation(out=sp1[:], in_=sp1[:], func=AF.Exp, scale=-1.0), reads=["sp1"], writes=["sp1"])
            T.op("act", lambda e: e.activation(out=sp1[:], in_=sp1[:], func=AF.Ln, bias=1.0, scale=1.0), reads=["sp1"], writes=["sp1"])
            T.op("dve", lambda e: e.tensor_scalar(out=sp2[:], in0=lam[:], scalar1=-1.0, scalar2=0.0, op0=ALU.mult, op1=ALU.max), reads=["lam"], writes=["sp2"])
            T.op("dve", lambda e: e.tensor_tensor(out=c1s[:], in0=sp1[:], in1=sp2[:], op=ALU.add), reads=["sp1", "sp2"], writes=["c1s"])
            T.op("dve", lambda e: e.tensor_scalar(out=c1s[:], in0=c1s[:], scalar1=-8.0, scalar2=None, op0=ALU.mult), reads=["c1s"], writes=["c1s"])

            if stop == 2 and substop == 1:
                T.finish_early()
            nTp = Rot([SB(st, "anT%d" % i, [128, 8, 512], BF16) for i in range(1)], "anT")
            xl = [SB(st, "xl%d" % c, [128, 515], F32) for c in range(4)]
            hprev = SB(st, "hprev", [128, 4], F32)
            T.op("dve", lambda e: e.memset(hprev[:], 0.0), writes=["hprev"])
            for c in range(4):
                T.op("dve", lambda e, c=c: e.memset(xl[c][:], 0.0), writes=["xl%d" % c])
            xa = [SB(st, "xa%d" % c, [128, 512], F32) for c in range(4)]
            xab = [SB(st, "xab%d" % c, [128, 512], BF16) for c in range(4)]
            rr = [SB(st, "rr%d" % c, [128, 512], F32) for c in range(4)]
            ig = [SB(st, "ig%d" % c, [128, 512], F32) for c in range(4)]
            gl = [SB(st, "gl%d" % c, [128, 512], BF16) for c in range(4)]
            sq = [SB(st, "sq%d" % c, [128, 512], F32) for c in range(4)]
            hs = sq
            yaT = Rot([SB(st, "yaT%d" % i, [128, 4, 512], BF16) for i in range(1)], "yaT")
            ycT = Rot([SB(st, "ycT%d" % i, [128, 4, 512], BF16) for i in range(1)], "ycT")
            qmT = Rot([SB(st, "qmT%d" % i, [128, 512], BF16) for i in range(3)], "qmT")
            pTm = Rot([SB(st, "pTm%d" % i, [128, 512], BF16) for i in range(6)], "pTm")
            rden = Rot([SB(st, "rdm%d" % i, [128, 512], F32) for i in range(2)], "rdm")
            latn = Rot([SB(st, "latn%d" % i, [128, 480], BF16) for i in range(4)], "latn")
            for i in range(4):
                T.op("pool", lambda e, i=i: e.memset(latn.bufs[i][:], 0.0), writes=["latn#%d" % i])
            ss2 = Rot([SB(st, "ss2_%d" % i, [128, 8], F32) for i in range(4)], "ss2")
            rt = Rot([SB(st, "rt%d" % i, [128, 4, 16], F32) for i in range(4)], "rt")
            ya_v = ya_s.rearrange("(c p) t -> p c t", p=128)
            yc_v = yc_s.rearrange("(c p) t -> p c t", p=128)

            if stop == 2 and substop == 11:
                T.finish_early()
            def a_s1(i):
                nbs = []
                for sub_ in range(4):
                    r0 = i * 512 + sub_ * 128
                    xt, xk = pools["xt"].next()
                    T.dma("sp", xt[:], x[r0:r0 + 128, :], writes=[xk])
                    jk, jkk = pools["junk"].next()
                    ss, sk = pools["ss"].next()
                    T.op("act", lambda e, jk=jk, xt=xt, ss=ss: e.activation(out=jk[:], in_=xt[:], func=AF.Square, accum_out=ss[:, 0:1]), reads=[xk], writes=[sk, jkk])
                    T.op("dve", lambda e, ss=ss: e.tensor_scalar(out=ss[:, 1:2], in0=ss[:, 0:1], scalar1=1.0 / D, scalar2=EPS, op0=ALU.mult, op1=ALU.add), reads=[sk], writes=[sk])
                    T.op("pool", lambda e, ss=ss: e.tensor_tensor(out=ss[:, 2:3], in0=ss[:, 1:2], in1=mh[:, 0:1], op=ALU.pow), reads=[sk, "mh"], writes=[sk])
                    nb, nk = pools["nb"].next()
                    T.op("dve", lambda e, nb=nb, xt=xt, ss=ss, gb=gbc["g_mix"]: e.scalar_tensor_tensor(out=nb[:], in0=xt[:], scalar=ss[:, 2:3], in1=gb[:], op0=ALU.mult, op1=ALU.mult),
                         reads=[xk, sk, "bc_g_mix"], writes=[nk])
                    nbs.append((nb, nk))
                return nbs

            def a_s2(nbs):
                nT, nTk = nTp.next()
                nTkeys = []
                for sub_ in range(4):
                    nb, nk = nbs[sub_]
                    pt, pk = pools["ps"].next()
                    ptb = pt[:].bitcast(BF16)
                    T.group("pe", [(lambda e, ptb=ptb, nb=nb, k=k: e.transpose(out=ptb[:, k * 128:(k + 1) * 128], in_=nb[:, k * 128:(k + 1) * 128], identity=idb[:])) for k in range(8)],
                            reads=[nk, "idb"], writes=[pk])
                    dk = "%s_s%d" % (nTk, sub_)
                    T.op("act", lambda e, ptb=ptb, nT=nT, sub_=sub_: e.activation(out=nT[:, :, sub_ * 128:(sub_ + 1) * 128], in_=ptb.rearrange("p (k t) -> p k t", k=8), func=AF.Copy),
                         reads=[pk], writes=[dk])
                    nTkeys.append(dk)
                return nT, nTk, nTkeys

            nbs_next = a_s1(0)
            for i in range(NB):
                nT, nTk, nTkeys = a_s2(nbs_next)
                if stop == 2 and substop == 2 and i == 0:
                    T.finish_early()
                LS = []
                for sub in range(4):
                    ti = i * 4 + sub
                    p, pk = psr.next()
                    mm(p[:, 0:416], [(nT[:, k, sub * 128:(sub + 1) * 128], win[:, k, 1024:1440]) for k in range(8)], reads=winkeys + [nTkeys[sub]], writes=[pk])
                    LS.append({"ti": ti, "tok": slice(ti * 128, (ti + 1) * 128), "p": p, "pk": pk})
                for L_ in LS:
                    p, pk = L_["p"], L_["pk"]
                    s2, s2k = ss2.next()
                    jk, jkk = pools["junk"].next()
                    T.op("act", lambda e, p=p, s2=s2, jk=jk: e.activation(out=jk[:, 0:256], in_=p[:, 0:256], func=AF.Square, accum_out=s2[:, 0:1]), reads=[pk], writes=[s2k, jkk])
                    T.op("act", lambda e, p=p, s2=s2, jk=jk: e.activation(out=jk[:, 256:384], in_=p[:, 256:384], func=AF.Square, accum_out=s2[:, 1:2]), reads=[pk], writes=[s2k, jkk])
                    L_["s2"], L_["s2k"] = s2, s2k
                for L_ in LS:
                    s2, s2k = L_["s2"], L_["s2k"]
                    T.op("dve", lambda e, s2=s2: e.tensor_scalar(out=s2[:, 2:3], in0=s2[:, 0:1], scalar1=1.0 / 256, scalar2=EPS, op0=ALU.mult, op1=ALU.add), reads=[s2k], writes=[s2k])
                    T.op("dve", lambda e, s2=s2: e.tensor_scalar(out=s2[:, 3:4], in0=s2[:, 1:2], scalar1=1.0 / 128, scalar2=EPS, op0=ALU.mult, op1=ALU.add), reads=[s2k], writes=[s2k])
                for L_ in LS:
                    s2, s2k = L_["s2"], L_["s2k"]
                    T.op("pool", lambda e, s2=s2: e.tensor_tensor(out=s2[:, 4:6], in0=s2[:, 2:4], in1=mh[:, 0:2], op=ALU.pow), reads=[s2k, "mh"], writes=[s2k])
                for L_ in LS:
                    p, pk, s2, s2k = L_["p"], L_["pk"], L_["s2"], L_["s2k"]
                    ln_, lk = latn.next()
                    T.op("dve", lambda e, p=p, s2=s2, ln_=ln_: e.scalar_tensor_tensor(out=ln_[:, 0:256], in0=p[:, 0:256], scalar=s2[:, 4:5], in1=gq_bc[:], op0=ALU.mult, op1=ALU.mult),
                         reads=[pk, s2k, "gq_bc"], writes=[lk])
                    T.op("dve", lambda e, p=p, s2=s2, ln_=ln_: e.scalar_tensor_tensor(out=ln_[:, 256:384], in0=p[:, 256:384], scalar=s2[:, 5:6], in1=gkv_bc[:], op0=ALU.mult, op1=ALU.mult),
                         reads=[pk, s2k, "gkv_bc"], writes=[lk])
                    L_["ln"], L_["lk"] = ln_, lk
                for L_ in LS:
                    p, pk, ti = L_["p"], L_["pk"], L_["ti"]
                    r_, rk = rt.next()
                    cs = cosT[:, ti, :]; sn = sinT[:, ti, :]
                    T.op("dve", lambda e, p=p, r_=r_, cs=cs: e.tensor_tensor(out=r_[:, 0, :], in0=p[:, 384:400], in1=cs, op=ALU.mult), reads=[pk, "cosT"], writes=[rk])
                    T.op("dve", lambda e, p=p, r_=r_, sn=sn: e.tensor_tensor(out=r_[:, 1, :], in0=p[:, 400:416], in1=sn, op=ALU.mult), reads=[pk, "sinT"], writes=[rk])
                    T.op("dve", lambda e, p=p, r_=r_, cs=cs: e.tensor_tensor(out=r_[:, 2, :], in0=p[:, 400:416], in1=cs, op=ALU.mult), reads=[pk, "cosT"], writes=[rk])
                    T.op("dve", lambda e, p=p, r_=r_, sn=sn: e.tensor_tensor(out=r_[:, 3, :], in0=p[:, 384:400], in1=sn, op=ALU.mult), reads=[pk, "sinT"], writes=[rk])
                    L_["r"], L_["rk"] = r_, rk
                for L_ in LS:
                    r_, rk, ln_, lk = L_["r"], L_["rk"], L_["ln"], L_["lk"]
                    T.op("pool", lambda e, r_=r_, ln_=ln_: e.tensor_tensor(out=ln_[:, 448:464], in0=r_[:, 0, :], in1=r_[:, 1, :], op=ALU.subtract), reads=[rk], writes=[lk])
                    T.op("pool", lambda e, r_=r_, ln_=ln_: e.tensor_tensor(out=ln_[:, 464:480], in0=r_[:, 2, :], in1=r_[:, 3, :], op=ALU.add), reads=[rk], writes=[lk])
                if i + 1 < NB:
                    nbs_next = a_s1(i + 1)
                if stop == 2 and substop == 3 and i == 0:
                    T.finish_early()
                for c in range(4):
                    p, pk = psr.next()
                    mm(p[:, :], [(win[:, k, c * 128:(c + 1) * 128], nT[:, k, :]) for k in range(8)], reads=winkeys + nTkeys, writes=[pk])
                    xk = "xl%d" % c
                    T.op("pool", lambda e, c=c: e.tensor_copy(out=xl[c][:, 0:3], in_=xl[c][:, 512:515]), reads=[xk], writes=[xk])
                    T.op("act", lambda e, c=c, p=p: e.activation(out=xl[c][:, 3:515], in_=p[:, :], func=AF.Copy), reads=[pk, xk], writes=[xk])
                for c in range(4):
                    xk = "xl%d" % c
                    T.op("dve", lambda e, c=c: e.tensor_scalar(out=xa[c][:], in0=xl[c][:, 0:512], scalar1=cws[:, c, 0:1], scalar2=cbs[:, c:c + 1], op0=ALU.mult, op1=ALU.add),
                         reads=[xk, "cws", "cbs"], writes=["xa%d" % c])
                    for j in range(1, 4):
                        T.op("dve", lambda e, c=c, j=j: e.scalar_tensor_tensor(out=xa[c][:], in0=xl[c][:, j:j + 512], scalar=cws[:, c, j:j + 1], in1=xa[c][:], op0=ALU.mult, op1=ALU.add),
                             reads=[xk, "cws", "xa%d" % c], writes=["xa%d" % c])
                for c in range(4):
                    T.op("pool", lambda e, c=c: e.tensor_copy(out=xab[c][:], in_=xa[c][:]), reads=["xa%d" % c], writes=["xab%d" % c])
                for c in range(4):
                    p, pk = psr.next()
                    mm(p[:, :], [(win[:, k, 512 + c * 128:512 + (c + 1) * 128], nT[:, k, :]) for k in range(8)], reads=winkeys + nTkeys, writes=[pk])
                    T.op("act", lambda e, c=c, p=p: e.activation(out=gl[c][:], in_=p[:, :], func=AF.Gelu_apprx_tanh), reads=[pk], writes=["gl%d" % c])
                for L_ in LS:
                    ln_, lk = L_["ln"], L_["lk"]
                    pt, ptk = pools["ps"].next()
                    ptb = pt[:].bitcast(BF16)
                    T.group("pe", [
                        lambda e, ptb=ptb, ln_=ln_: e.transpose(out=ptb[:, 0:128], in_=ln_[:, 0:128], identity=idb[:]),
                        lambda e, ptb=ptb, ln_=ln_: e.transpose(out=ptb[:, 128:256], in_=ln_[:, 128:256], identity=idb[:]),
                        lambda e, ptb=ptb, ln_=ln_: e.transpose(out=ptb[:, 256:384], in_=ln_[:, 256:384], identity=idb[:]),
                        lambda e, ptb=ptb, ln_=ln_: e.transpose(out=ptb[0:96, 384:512], in_=ln_[:, 384:480], identity=idb[:]),
                    ], reads=[lk, "idb"], writes=[ptk])
                    tok, ti = L_["tok"], L_["ti"]
                    T.op("act", lambda e, ptb=ptb, tok=tok: e.activation(out=cqnT[:, :, tok], in_=ptb[:, 0:256].rearrange("p (k t) -> p k t", k=2), func=AF.Copy),
                         reads=[ptk], writes=["cqnT%d" % ti])
                    T.op("act", lambda e, ptb=ptb, tok=tok: e.activation(out=ckvnT[:, tok], in_=ptb[:, 256:384], func=AF.Copy), reads=[ptk], writes=["ckvnT%d" % ti])
                    T.op("act", lambda e, ptb=ptb, tok=tok: e.activation(out=kropeT[64:96, tok], in_=ptb[64:96, 384:512], func=AF.Copy), reads=[ptk], writes=["kropeT%d" % ti])
                for c in range(4):
                    p, pk = psr.next()
                    mm(p[:, :], [(wabd[:, c, :], xab[c][:])], reads=["wabd", "xab%d" % c], writes=[pk])
                    T.op("act", lambda e, c=c, p=p: e.activation(out=rr[c][:], in_=p[:, :], func=AF.Sigmoid, bias=bas[:, c:c + 1]), reads=[pk, "bas"], writes=["rr%d" % c])
                    p, pk = psr.next()
                    mm(p[:, :], [(wxbd[:, c, :], xab[c][:])], reads=["wxbd", "xab%d" % c], writes=[pk])
                    T.op("act", lambda e, c=c, p=p: e.activation(out=ig[c][:], in_=p[:, :], func=AF.Sigmoid, bias=bxs[:, c:c + 1]), reads=[pk, "bxs"], writes=["ig%d" % c])
                for c in range(4):
                    T.op("act", lambda e, c=c: e.activation(out=rr[c][:], in_=rr[c][:], func=AF.Exp, scale=c1s[:, c:c + 1]), reads=["rr%d" % c, "c1s"], writes=["rr%d" % c])
                if stop == 2 and substop == 4 and i == 0:
                    T.finish_early()
                ycb, yck = ycT.next()

                def ma_q(h):
                    p, pk = psr.next()
                    mm(p[:, :], [(win[:, k, 1440 + h * 128:1440 + (h + 1) * 128], nT[:, k, :]) for k in range(8)], reads=winkeys + nTkeys, writes=[pk])
                    qm, qmk = qmT.next()
                    T.op("dve", lambda e, qm=qm, p=p: e.tensor_copy(out=qm[:], in_=p[:, :]), reads=[pk], writes=[qmk])
                    return qm, qmk

                def ma_s(h, qm, qmk):
                    pts = []
                    for mc in range(2):
                        p2, p2k = psr.next()
                        mm(p2[:, :], [(kmemT[:, h, mc * 128:(mc + 1) * 128], qm[:])], reads=[qmk], writes=[p2k])
                        pT, pTk = pTm.next()
                        T.op("act", lambda e, pT=pT, p2=p2: e.activation(out=pT[:], in_=p2[:, :], func=AF.Exp, scale=1.0 / math.sqrt(128.0)), reads=[p2k], writes=[pTk])
                        pts.append((pT, pTk))
                    return pts

                def ma_o(h, pts):
                    pn, pnk = psr.next()
                    mm(pn[:, :], [(vmem[:, mc, h * 128:(h + 1) * 128], pts[mc][0][:]) for mc in range(2)], reads=[pts[0][1], pts[1][1]], writes=[pnk])
                    pd, pdk = psr.next()
                    mm(pd[:, :], [(onesb[:], pts[mc][0][:]) for mc in range(2)], reads=[pts[0][1], pts[1][1], "onesb"], writes=[pdk])
                    rd, rdk = rden.next()
                    T.op("dve", lambda e, rd=rd, pd=pd: e.reciprocal(out=rd[:], in_=pd[:, :]), reads=[pdk], writes=[rdk])
                    T.op("dve", lambda e, rd=rd, pn=pn, ycb=ycb, h=h: e.tensor_tensor(out=ycb[:, h, :], in0=pn[:, :], in1=rd[:], op=ALU.mult), reads=[pnk, rdk], writes=[yck])

                q_ = {0: ma_q(0)}
                q_[1] = ma_q(1)
                s_ = {0: ma_s(0, *q_[0])}
                for h in range(4):
                    if h + 2 < 4:
                        q_[h + 2] = ma_q(h + 2)
                    if h + 1 < 4:
                        s_[h + 1] = ma_s(h + 1, *q_[h + 1])
                    ma_o(h, s_[h])
                T.dma("sp", yc_v[:, :, i * 512:(i + 1) * 512], ycb[:], reads=[yck], writes=["yc_s%d" % i])
                if stop == 2 and substop == 5 and i == 0:
                    T.finish_early()
                yab, yak = yaT.next()
                for c in range(4):
                    T.op("pool", lambda e, c=c: e.tensor_tensor(out=sq[c][:], in0=rr[c][:], in1=rr[c][:], op=ALU.mult), reads=["rr%d" % c], writes=["sq%d" % c])
                for c in range(4):
                    T.op("act", lambda e, c=c: e.activation(out=sq[c][:], in_=sq[c][:], func=AF.Sqrt, scale=-1.0, bias=1.0), reads=["sq%d" % c], writes=["sq%d" % c])
                for c in range(4):
                    T.op("pool", lambda e, c=c: e.tensor_tensor(out=ig[c][:], in0=ig[c][:], in1=xa[c][:], op=ALU.mult), reads=["ig%d" % c, "xa%d" % c], writes=["ig%d" % c])
                    T.op("pool", lambda e, c=c: e.tensor_tensor(out=ig[c][:], in0=ig[c][:], in1=sq[c][:], op=ALU.mult), reads=["ig%d" % c, "sq%d" % c], writes=["ig%d" % c])
                    T.op("dve", lambda e, c=c: e.tensor_tensor_scan(out=hs[c][:], data0=rr[c][:], data1=ig[c][:], initial=hprev[:, c:c + 1], op0=ALU.mult, op1=ALU.add),
                         reads=["rr%d" % c, "ig%d" % c, "hprev", "sq%d" % c], writes=["sq%d" % c])
                    T.op("dve", lambda e, c=c: e.tensor_copy(out=hprev[:, c:c + 1], in_=hs[c][:, 511:512]), reads=["sq%d" % c], writes=["hprev"])
                    T.op("pool", lambda e, c=c, yab=yab: e.tensor_tensor(out=yab[:, c, :], in0=hs[c][:], in1=gl[c][:], op=ALU.mult), reads=["sq%d" % c, "gl%d" % c], writes=[yak])
                T.dma("sp", ya_v[:, :, i * 512:(i + 1) * 512], yab[:], reads=[yak], writes=["ya_s%d" % i])
            T.barrier()
            if stop == 2:
                T.finish_early()

        stCW = ExitStack()
        wg = stCW.enter_context(nc.sbuf_tensor("wg", [128, 8, 3072], BF16, side="right"))
        wbr = stCW.enter_context(nc.sbuf_tensor("wbr", [128, 3, 4, D], BF16, side="right"))
        wo = stCW.enter_context(nc.sbuf_tensor("wo", [128, 8, D], BF16, side="right"))
        with ExitStack() as st:
            wuq = SB(st, "wuq", [128, 2, 768], BF16)
            wukv = SB(st, "wukv", [128, 1024], BF16)
            for k in range(2):
                T.dma("pool", wuq[:, k, :], w_uq[k * 128:(k + 1) * 128, :], writes=["wuq"])
            T.dma("pool", wukv[:], w_ukv, writes=["wukv"])
            msk = SB(st, "msk", [128, 4, 512], BF16)
            for r in range(4):
                T.dma("pool", msk[:, r, :], cmask[r], writes=["msk"])
            prefetch_c_weights = True
            KT = [SB(st, "KT%d" % i, [128, S], BF16) for i in range(2)]
            QT = [SB(st, "QT%d" % i, [128, S], BF16) for i in range(2)]
            for i in range(2):
                T.op("pool", lambda e, i=i: e.memset(KT[i][:], 0.0), writes=["KT%d" % i])
                T.op("pool", lambda e, i=i: e.memset(QT[i][:], 0.0), writes=["QT%d" % i])
            VH = [SB(st, "VH%d" % i, [128, NT, 128], BF16) for i in range(2)]
            for i in range(2):
                T.op("pool", lambda e, i=i: e.memset(VH[i][:], 1.0), writes=["VH%d" % i])
            for k in range(8):
                for b3 in range(3):
                    T.dma("pool", wg[:, k, b3 * 1024:(b3 + 1) * 1024], w_in[k * 128:(k + 1) * 128, 1952 + b3 * 1024:1952 + (b3 + 1) * 1024], writes=["wg%d_%d" % (k, b3)])
            for b3 in range(3):
                for kc in range(4):
                    T.dma("pool", wbr[:, b3, kc, :], w_br[b3, kc * 128:(kc + 1) * 128, :], writes=["wbr%d_%d" % (b3, kc)])
            for k in range(8):
                T.dma("pool", wo[:, k, :], w_o[k * 128:(k + 1) * 128, :], writes=["wo%d" % k])
            NQB = 1 if "q1" in EXP else 2
            qtok = Rot([SB(st, "qtok%d" % i, [128, 4, 96], BF16) for i in range(NQB)], "qtok", shared=True)
            rt = Rot([SB(st, "brt%d" % i, [128, 4, 4, 16], F32) for i in range(NQB)], "brt", shared=True)
            pTb = Rot([SB(st, "pTb%d" % i, [128, 512], BF16) for i in range(6)], "pTb")
            rdb = Rot([SB(st, "rdb%d" % i, [128, 512], F32) for i in range(2)], "rdb")
            ybT = Rot([SB(st, "ybT%d" % i, [64, 512], BF16) for i in range(2)], "ybT")
            psr = Rot(PS[0:4] + PS[6:8], "psb")
            pso = Rot(PS[4:6], "pso")
            scale = 1.0 / math.sqrt(96.0)
            allck = ["ckvnT%d" % t for t in range(NT)]
            allcq = ["cqnT%d" % t for t in range(NT)]
            allkr = ["kropeT%d" % t for t in range(NT)]
            def prep(h):
                b = h % 2
                KTh, QTh, VHh = KT[b], QT[b], VH[b]
                kk, qk, vk = "KT%d" % b, "QT%d" % b, "VH%d" % b
                for i in range(NB):
                    p, pk = psr.next()
                    mm(p[0:64, :], [(wukv[:, h * 128:h * 128 + 64], ckvnT[:, i * 512:(i + 1) * 512])], reads=["wukv"] + allck[i * 4:(i + 1) * 4], writes=[pk])
                    T.op("act", lambda e, p=p, i=i, KTh=KTh: e.activation(out=KTh[0:64, i * 512:(i + 1) * 512], in_=p[0:64, :], func=AF.Copy), reads=[pk], writes=[kk])
                T.op("act", lambda e, KTh=KTh: e.activation(out=KTh[64:96, :], in_=kropeT[64:96, :], func=AF.Copy), reads=allkr, writes=[kk])
                for g in range(4):
                    p, pk = psr.next()
                    T.group("pe", [(lambda e, p=p, j=j, g=g, h=h: e.matmul(p[:, j * 64:(j + 1) * 64], lhsT=ckvnT[:, (g * 8 + j) * 128:(g * 8 + j + 1) * 128],
                                                                        rhs=wukv[:, h * 128 + 64:h * 128 + 128], start=True, stop=True)) for j in range(8)],
                            reads=["wukv"] + allck[g * 8:(g + 1) * 8], writes=[pk])
                    T.op("dve", lambda e, p=p, g=g, VHh=VHh: e.tensor_copy(out=VHh[:, g * 8:(g + 1) * 8, 0:64], in_=p[:, :].rearrange("p (j d) -> p j d", j=8)), reads=[pk], writes=[vk])
                for g in range(NB):
                    p, pk = psr.next()
                    fns = []
                    for j in range(4):
                        ti = g * 4 + j
                        for k in range(2):
                            fns.append(lambda e, p=p, j=j, k=k, ti=ti, h=h: e.matmul(p[:, j * 96:(j + 1) * 96], lhsT=cqnT[:, k, ti * 128:(ti + 1) * 128],
                                                                                  rhs=wuq[:, k, h * 96:(h + 1) * 96], start=(k == 0), stop=(k == 1)))
                    T.group("pe", fns, reads=["wuq"] + allcq[g * 4:(g + 1) * 4], writes=[pk])
                    pv = p[:, 0:384].rearrange("p (j d) -> p j d", j=4)
                    qt, qtk = qtok.next()
                    T.op("act", lambda e, pv=pv, qt=qt: e.activation(out=qt[:, :, 0:64], in_=pv[:, :, 0:64], func=AF.Copy), reads=[pk], writes=[qtk])
                    r_, rk = rt.next()
                    cs = cosT[:, g * 4:(g + 1) * 4, :]; sn = sinT[:, g * 4:(g + 1) * 4, :]
                    T.op("dve", lambda e, pv=pv, r_=r_, cs=cs: e.tensor_tensor(out=r_[:, 0, :, :], in0=pv[:, :, 64:80], in1=cs, op=ALU.mult), reads=[pk, "cosT"], writes=[rk])
                    T.op("dve", lambda e, pv=pv, r_=r_, sn=sn: e.tensor_tensor(out=r_[:, 1, :, :], in0=pv[:, :, 80:96], in1=sn, op=ALU.mult), reads=[pk, "sinT"], writes=[rk])
                    T.op("dve", lambda e, pv=pv, r_=r_, cs=cs: e.tensor_tensor(out=r_[:, 2, :, :], in0=pv[:, :, 80:96], in1=cs, op=ALU.mult), reads=[pk, "cosT"], writes=[rk])
                    T.op("dve", lambda e, pv=pv, r_=r_, sn=sn: e.tensor_tensor(out=r_[:, 3, :, :], in0=pv[:, :, 64:80], in1=sn, op=ALU.mult), reads=[pk, "sinT"], writes=[rk])
                    T.op("dve", lambda e, r_=r_, qt=qt: e.tensor_tensor(out=qt[:, :, 64:80], in0=r_[:, 0, :, :], in1=r_[:, 1, :, :], op=ALU.subtract), reads=[rk], writes=[qtk])
                    T.op("dve", lambda e, r_=r_, qt=qt: e.tensor_tensor(out=qt[:, :, 80:96], in0=r_[:, 2, :, :], in1=r_[:, 3, :, :], op=ALU.add), reads=[rk], writes=[qtk])
                    pt, ptk = psr.next()
                    ptb = pt[:].bitcast(BF16)
                    T.group("pe", [(lambda e, ptb=ptb, qt=qt, j=j: e.transpose(out=ptb[0:96, j * 128:(j + 1) * 128], in_=qt[:, j, :], identity=idb[:])) for j in range(4)],
                            reads=[qtk, "idb"], writes=[ptk])
                    T.op("dve", lambda e, ptb=ptb, g=g, QTh=QTh: e.tensor_copy(out=QTh[0:96, g * 512:(g + 1) * 512], in_=ptb[0:96, 0:512]), reads=[ptk], writes=[qk])

            LOOK = 4

            def attn(h):
                b = h % 2
                KTh, QTh, VHh = KT[b], QT[b], VH[b]
                kk, qk, vk = "KT%d" % b, "QT%d" % b, "VH%d" % b
                items = [(i, kt) for i in range(NB) for kt in range(4 * i + 4)]
                N = len(items)
                S1 = {}
                acc = {}

                def stage1(n):
                    i, kt = items[n]
                    p, pk = psr.next()
                    mm(p[:, :], [(KTh[:, kt * 128:(kt + 1) * 128], QTh[:, i * 512:(i + 1) * 512])], reads=[kk, qk], writes=[pk])
                    pT, pTk = pTb.next()
                    T.op("act", lambda e, pT=pT, p=p: e.activation(out=pT[:], in_=p[:, :], func=AF.Exp, scale=scale), reads=[pk], writes=[pTk])
                    if kt >= 4 * i:
                        r = kt - 4 * i
                        T.op("dve", lambda e, pT=pT, r=r: e.tensor_tensor(out=pT[:], in0=pT[:], in1=msk[:, r, :], op=ALU.mult), reads=[pTk, "msk"], writes=[pTk])
                    S1[n] = (pT, pTk)

                def stage2(n):
                    i, kt = items[n]
                    nk = 4 * i + 4
                    if kt == 0:
                        acc[i] = pso.next()
                    po, pok = acc[i]
                    pT, pTk = S1.pop(n)
                    T.group("pe", [lambda e, po=po, pT=pT, kt=kt, nk=nk, VHh=VHh: e.matmul(po[:, :], lhsT=VHh[:, kt, :], rhs=pT[:], start=(kt == 0), stop=(kt == nk - 1))],
                            reads=[pTk, vk], writes=[pok])
                    if kt == nk - 1:
                        rd, rdk = rdb.next()
                        T.op("dve", lambda e, rd=rd, po=po: e.reciprocal(out=rd[64:128, :], in_=po[64:128, :]), reads=[pok], writes=[rdk])
                        yb, ybk = ybT.next()
                        T.op("dve", lambda e, rd=rd, po=po, yb=yb: e.tensor_tensor(out=yb[:], in0=po[0:64, :], in1=rd[64:128, :], op=ALU.mult), reads=[pok, rdk], writes=[ybk])
                        T.dma("sp", yb_s[h * 64:(h + 1) * 64, i * 512:(i + 1) * 512], yb[:], reads=[ybk], writes=["yb_s_%d_%d" % (h, i)])

                for n in range(min(LOOK, N)):
                    stage1(n)
                for n in range(N):
                    if n + LOOK < N:
                        stage1(n + LOOK)
                    stage2(n)

            prep(0)
            for h in range(8):
                if h + 1 < 8:
                    prep(h + 1)
                attn(h)
            T.barrier()
            if stop == 3:
                T.finish_early()

        stAB.close()
        comb = SB(top, "comb", [128, NT, NE], F32)
        lg = SB(top, "lg", [128, NT, 36], F32)
        with ExitStack() as st:
            load_g(st, "g_mix", "c")
            load_g(st, "g_ffn", "c")
            pools = {
                "xt": Rot([SB(st, "cxt%d" % i, [128, D], F32) for i in range(4)], "cxt"),
                "junk": Rot([SB(st, "cjk", [128, D], BF16)], "cjk"),
                "ss": Rot([SB(st, "css%d" % i, [128, 4], F32) for i in range(8)], "css"),
                "nb": Rot([SB(st, "cnb%d" % i, [128, D], BF16) for i in range(2)], "cnb"),
                "ps": Rot(PS[0:2], "psn"),
            }
            psr = Rot(PS[2:8], "psc")
            wgkeys = ["wg%d_%d" % (k, b3) for k in range(8) for b3 in range(3)]
            wgr = SB(st, "wgr", [128, 8, 36], F32)
            T.dma("sp", wgr[:], w_gr.rearrange("(k p) c -> p k c", p=128), writes=["wgr"])
            TC = 256
            NSC = TC // 128
            NBC = S // TC
            nTp = Rot([SB(st, "cnT%d" % i, [128, 8, TC], BF16) for i in range(1)], "cnT")
            yT = [Rot([SB(st, "yT%d_%d" % (b3, i), [128, 4, TC], BF16) for i in range(2)], "yT%d" % b3) for b3 in range(3)]
            ysrc = [ya_s.rearrange("(c p) t -> p c t", p=128), yb_s.rearrange("(c p) t -> p c t", p=128), yc_s.rearrange("(c p) t -> p c t", p=128)]
            gs = Rot([SB(st, "gs%d" % i, [128, TC], F32) for i in range(3)], "gs")
            tb = Rot([SB(st, "tb%d" % i, [128, TC], F32) for i in range(6)], "tb")
            mT = Rot([SB(st, "mT%d" % i, [128, 8, TC], BF16) for i in range(1)], "mT")
            hT = Rot([SB(st, "hT%d" % i, [128, D], F32) for i in range(2)], "hT")
            n2f = Rot([SB(st, "n2f%d" % i, [128, D], F32) for i in range(2)], "n2f")
            n2Tf = Rot([SB(st, "n2Tf%d" % i, [128, 8, 128], F32) for i in range(1)], "n2Tf")
            n2Tb = Rot([SB(st, "n2Tb%d" % i, [128, 8, TC], BF16) for i in range(1)], "n2Tb")
            n2T_v = n2T_s.rearrange("(k p) t -> p k t", p=128)

            def c_s1(i):
                st_ = {"i": i, "xts": [], "nbs": []}
                for sub in range(NSC):
                    r0 = i * TC + sub * 128
                    xt, xk = pools["xt"].next()
                    T.dma("sp", xt[:], x[r0:r0 + 128, :], writes=[xk])
                    jk, jkk = pools["junk"].next()
                    ss, sk = pools["ss"].next()
                    T.op("act", lambda e, jk=jk, xt=xt, ss=ss: e.activation(out=jk[:], in_=xt[:], func=AF.Square, accum_out=ss[:, 0:1]), reads=[xk], writes=[sk, jkk])
                    T.op("dve", lambda e, ss=ss: e.tensor_scalar(out=ss[:, 1:2], in0=ss[:, 0:1], scalar1=1.0 / D, scalar2=EPS, op0=ALU.mult, op1=ALU.add), reads=[sk], writes=[sk])
                    T.op("pool", lambda e, ss=ss: e.tensor_tensor(out=ss[:, 2:3], in0=ss[:, 1:2], in1=mh[:, 0:1], op=ALU.pow), reads=[sk, "mh"], writes=[sk])
                    nb, nk = pools["nb"].next()
                    T.op("dve", lambda e, nb=nb, xt=xt, ss=ss, gb=gbc["g_mix"]: e.scalar_tensor_tensor(out=nb[:], in0=xt[:], scalar=ss[:, 2:3], in1=gb[:], op0=ALU.mult, op1=ALU.mult),
                         reads=[xk, sk, "bc_g_mix"], writes=[nk])
                    st_["xts"].append((xt, xk))
                    st_["nbs"].append((nb, nk))
                ys = []
                for b3 in range(3):
                    y_, yk = yT[b3].next()
                    T.dma("sp", y_[:], ysrc[b3][:, :, i * TC:(i + 1) * TC], writes=[yk])
                    ys.append((y_, yk))
                st_["ys"] = ys
                return st_

            def c_s2(st_):
                nT, nTk = nTp.next()
                nTkeys = []
                for sub in range(NSC):
                    nb, nk = st_["nbs"][sub]
                    pt, pk = pools["ps"].next()
                    ptb = pt[:].bitcast(BF16)
                    T.group("pe", [(lambda e, ptb=ptb, nb=nb, k=k: e.transpose(out=ptb[:, k * 128:(k + 1) * 128], in_=nb[:, k * 128:(k + 1) * 128], identity=idb[:])) for k in range(8)],
                            reads=[nk, "idb"], writes=[pk])
                    dk = "%s_s%d" % (nTk, sub)
                    T.op("act", lambda e, ptb=ptb, nT=nT, sub=sub: e.activation(out=nT[:, :, sub * 128:(sub + 1) * 128], in_=ptb.rearrange("p (k t) -> p k t", k=8), func=AF.Copy),
                         reads=[pk], writes=[dk])
                    nTkeys.append(dk)
                ys = st_["ys"]
                m_, mk = mT.next()
                for c in range(8):
                    tbs = []
                    for b3 in range(3):
                        p, pk = psr.next()
                        mm(p[:, 0:TC], [(wg[:, k, b3 * 1024 + c * 128:b3 * 1024 + (c + 1) * 128], nT[:, k, :]) for k in range(8)], reads=wgkeys + nTkeys, writes=[pk])
                        g_, gk = gs.next()
                        T.op("act", lambda e, g_=g_, p=p: e.activation(out=g_[:], in_=p[:, 0:TC], func=AF.Sigmoid), reads=[pk], writes=[gk])
                        p2, p2k = psr.next()
                        mm(p2[:, 0:TC], [(wbr[:, b3, kc, c * 128:(c + 1) * 128], ys[b3][0][:, kc, :]) for kc in range(4)], reads=["wbr", ys[b3][1]], writes=[p2k])
                        t_, tk = tb.next()
                        T.op("dve", lambda e, t_=t_, p2=p2, g_=g_: e.tensor_tensor(out=t_[:], in0=p2[:, 0:TC], in1=g_[:], op=ALU.mult), reads=[p2k, gk], writes=[tk])
                        tbs.append((t_, tk))
                    T.op("pool", lambda e, a=tbs[0][0], b_=tbs[1][0]: e.tensor_tensor(out=a[:], in0=a[:], in1=b_[:], op=ALU.add), reads=[tbs[0][1], tbs[1][1]], writes=[tbs[0][1]])
                    T.op("pool", lambda e, a=tbs[0][0], b_=tbs[2][0], m_=m_, c=c: e.tensor_tensor(out=m_[:, c, :], in0=a[:], in1=b_[:], op=ALU.add),
                         reads=[tbs[0][1], tbs[2][1]], writes=["%s_c%d" % (mk, c)])
                st_["m"] = (m_, ["%s_c%d" % (mk, c) for c in range(8)])

            def c_s3(st_):
                i = st_["i"]
                m_, mkeys = st_["m"]
                st_["nfs"] = []
                for sub in range(NSC):
                    ti = i * NSC + sub
                    xt, xk = st_["xts"][sub]
                    h_, hk = hT.next()
                    for half in range(2):
                        p, pk = psr.next()
                        mm(p[:, :], [(m_[:, kc, sub * 128:(sub + 1) * 128], wo[:, kc, half * 512:(half + 1) * 512]) for kc in range(8)], reads=mkeys + ["wo"], writes=[pk])
                        T.op("dve", lambda e, h_=h_, p=p, xt=xt, half=half: e.tensor_tensor(out=h_[:, half * 512:(half + 1) * 512], in0=p[:, :], in1=xt[:, half * 512:(half + 1) * 512], op=ALU.add),
                             reads=[pk, xk], writes=[hk])
                    T.dma("sp", h_s[ti * 128:(ti + 1) * 128, :], h_[:], reads=[hk], writes=["h_s%d" % ti])
                    jk, jkk = pools["junk"].next()
                    ss, sk = pools["ss"].next()
                    T.op("act", lambda e, jk=jk, h_=h_, ss=ss: e.activation(out=jk[:], in_=h_[:], func=AF.Square, accum_out=ss[:, 0:1]), reads=[hk], writes=[sk, jkk])
                    T.op("dve", lambda e, ss=ss: e.tensor_scalar(out=ss[:, 1:2], in0=ss[:, 0:1], scalar1=1.0 / D, scalar2=EPS, op0=ALU.mult, op1=ALU.add), reads=[sk], writes=[sk])
                    T.op("pool", lambda e, ss=ss: e.tensor_tensor(out=ss[:, 2:3], in0=ss[:, 1:2], in1=mh[:, 0:1], op=ALU.pow), reads=[sk, "mh"], writes=[sk])
                    nf, nfk = n2f.next()
                    T.op("dve", lambda e, nf=nf, h_=h_, ss=ss: e.scalar_tensor_tensor(out=nf[:], in0=h_[:], scalar=ss[:, 2:3], in1=gbc["g_ffn"][:], op0=ALU.mult, op1=ALU.mult),
                         reads=[hk, sk, "bc_g_ffn"], writes=[nfk])
                    st_["nfs"].append((nf, nfk))

            def c_s4(st_):
                i = st_["i"]
                n2b, n2bk = n2Tb.next()
                for sub in range(NSC):
                    ti = i * NSC + sub
                    nf, nfk = st_["nfs"][sub]
                    ntf, ntfk = n2Tf.next()
                    for hh in range(2):
                        p, pk = psr.next()
                        T.group("pe", [(lambda e, p=p, nf=nf, hh=hh, k=k: e.transpose(out=p[:, k * 128:(k + 1) * 128], in_=nf[:, (hh * 4 + k) * 128:(hh * 4 + k + 1) * 128], identity=idf[:])) for k in range(4)],
                                reads=[nfk, "idf"], writes=[pk])
                        T.op("act", lambda e, p=p, ntf=ntf, hh=hh: e.activation(out=ntf[:, hh * 4:(hh + 1) * 4, :], in_=p[:, :].rearrange("p (k t) -> p k t", k=4), func=AF.Copy),
                             reads=[pk], writes=["%s_%d" % (ntfk, hh)])
                        T.op("dve", lambda e, p=p, n2b=n2b, hh=hh, sub=sub: e.tensor_copy(out=n2b[:, hh * 4:(hh + 1) * 4, sub * 128:(sub + 1) * 128], in_=p[:, :].rearrange("p (k t) -> p k t", k=4)),
                             reads=[pk], writes=["%s_%d_%d" % (n2bk, sub, hh)])
                    p, pk = psr.next()
                    mm(p[:, 0:36], [(ntf[:, k, :], wgr[:, k, :]) for k in range(8)], reads=["%s_0" % ntfk, "%s_1" % ntfk, "wgr"], writes=[pk])
                    T.op("dve", lambda e, p=p, ti=ti: e.tensor_tensor(out=lg[:, ti, :], in0=p[:, 0:36], in1=bgr_bc[:], op=ALU.add), reads=[pk, "bgr_bc"], writes=["lg%d" % ti])
                T.dma("sp", n2T_v[:, :, i * TC:(i + 1) * TC], n2b[:], reads=["%s_%d_%d" % (n2bk, s_, hh) for s_ in range(NSC) for hh in range(2)], writes=["n2T_s%d" % i])

            cur_c = c_s1(0)
            prev_c = None
            for t in range(NBC):
                c_s2(cur_c)
                nxt_c = c_s1(t + 1) if t + 1 < NBC else None
                if prev_c is not None:
                    c_s4(prev_c)
                c_s3(cur_c)
                prev_c = cur_c
                cur_c = nxt_c
            c_s4(prev_c)
            T.barrier()
            if stop == 4:
                T.finish_early()

        stCW.close()
        with ExitStack() as st:
            lgk = ["lg%d" % t for t in range(NT)]
            R = lambda nm, shp: SB(st, nm, shp, F32)
            gm = R("r_gm", [128, NT]); ge = R("r_ge", [128, NT, 4]); gsum = R("r_gsum", [128, NT]); gw = R("r_gw", [128, NT])
            mg = R("r_mg", [128, NT, 4]); eg = R("r_eg", [128, NT, 8]); tmp8 = R("r_tmp8", [128, NT, 8])
            m1 = R("r_m1", [128, NT]); m2 = R("r_m2", [128, NT]); sel = R("r_sel", [128, NT, 8]); pe_ = R("r_pe", [128, NT, 8]); psum_ = R("r_ps", [128, NT])
            glv = lg[:, :, 0:4]
            elv = lg[:, :, 4:36].rearrange("p t (g e) -> p t g e", g=4)

            def bc(ap2, n):
                return ap2.unsqueeze(2).to_broadcast([128, NT, n])
            T.op("dve", lambda e: e.tensor_reduce(out=gm[:], in_=glv, axis=AX.X, op=ALU.max), reads=lgk, writes=["gm"])
            T.op("dve", lambda e: e.tensor_tensor(out=ge[:], in0=glv, in1=bc(gm[:], 4), op=ALU.subtract), reads=lgk + ["gm"], writes=["ge"])
            T.op("dve", lambda e: e.tensor_tensor(out=mg[:], in0=glv, in1=bc(gm[:], 4), op=ALU.is_equal), reads=lgk + ["gm"], writes=["mg"])
            T.op("act", lambda e: e.activation(out=ge[:], in_=ge[:], func=AF.Exp), reads=["ge"], writes=["ge"])
            T.op("dve", lambda e: e.tensor_reduce(out=gsum[:], in_=ge[:], axis=AX.X, op=ALU.add), reads=["ge"], writes=["gsum"])
            T.op("dve", lambda e: e.reciprocal(out=gw[:], in_=gsum[:]), reads=["gsum"], writes=["gw"])
            T.op("dve", lambda e: e.tensor_tensor(out=eg[:], in0=elv[:, :, 0, :], in1=bc(mg[:, :, 0], 8), op=ALU.mult), reads=lgk + ["mg"], writes=["eg"])
            for g in range(1, 4):
                T.op("dve", lambda e, g=g: e.tensor_tensor(out=tmp8[:], in0=elv[:, :, g, :], in1=bc(mg[:, :, g], 8), op=ALU.mult), reads=lgk + ["mg"], writes=["tmp8"])
                T.op("dve", lambda e: e.tensor_tensor(out=eg[:], in0=eg[:], in1=tmp8[:], op=ALU.add), reads=["eg", "tmp8"], writes=["eg"])
            T.op("dve", lambda e: e.tensor_reduce(out=m1[:], in_=eg[:], axis=AX.X, op=ALU.max), reads=["eg"], writes=["m1"])
            T.op("dve", lambda e: e.tensor_tensor(out=tmp8[:], in0=eg[:], in1=bc(m1[:], 8), op=ALU.is_equal), reads=["eg", "m1"], writes=["tmp8"])
            T.op("dve", lambda e: e.scalar_tensor_tensor(out=tmp8[:], in0=tmp8[:], scalar=-1.0e30, in1=eg[:], op0=ALU.mult, op1=ALU.add), reads=["tmp8", "eg"], writes=["tmp8"])
            T.op("dve", lambda e: e.tensor_reduce(out=m2[:], in_=tmp8[:], axis=AX.X, op=ALU.max), reads=["tmp8"], writes=["m2"])
            T.op("dve", lambda e: e.tensor_tensor(out=sel[:], in0=eg[:], in1=bc(m2[:], 8), op=ALU.is_ge), reads=["eg", "m2"], writes=["sel"])
            T.op("dve", lambda e: e.tensor_tensor(out=pe_[:], in0=eg[:], in1=bc(m1[:], 8), op=ALU.subtract), reads=["eg", "m1"], writes=["pe_"])
            T.op("act", lambda e: e.activation(out=pe_[:], in_=pe_[:], func=AF.Exp), reads=["pe_"], writes=["pe_"])
            T.op("dve", lambda e: e.tensor_tensor(out=pe_[:], in0=pe_[:], in1=sel[:], op=ALU.mult), reads=["pe_", "sel"], writes=["pe_"])
            T.op("dve", lambda e: e.tensor_reduce(out=psum_[:], in_=pe_[:], axis=AX.X, op=ALU.add), reads=["pe_"], writes=["psum_"])
            T.op("dve", lambda e: e.reciprocal(out=psum_[:], in_=psum_[:]), reads=["psum_"], writes=["psum_"])
            T.op("dve", lambda e: e.tensor_tensor(out=psum_[:], in0=psum_[:], in1=gw[:], op=ALU.mult), reads=["psum_", "gw"], writes=["psum_"])
            T.op("dve", lambda e: e.tensor_tensor(out=pe_[:], in0=pe_[:], in1=bc(psum_[:], 8), op=ALU.mult), reads=["pe_", "psum_"], writes=["pe_"])
            cv = comb[:].rearrange("p t (g e) -> p t g e", g=4)
            for g in range(4):
                T.op("dve", lambda e, g=g: e.tensor_tensor(out=cv[:, :, g, :], in0=pe_[:], in1=bc(mg[:, :, g], 8), op=ALU.mult), reads=["pe_", "mg"], writes=["comb"])
            if dbg:
                T.dma("sp", comb_s, comb[:], reads=["comb"], writes=["comb_s"])
            T.barrier()
            if stop == 5:
                T.finish_early()

        with ExitStack() as st:
            load_g(st, "g_fin", "d")
            TH = S // NHALF
            NS = TH // 128
            NBH = TH // 512
            n2T = SB(st, "n2T", [128, 8, TH], BF16)
            acc = SB(st, "acc", [128, NS, D], F32)
            stg = [SB(st, "stg%d" % i, [128, 8 * 256], F32) for i in range(3)]
            wgb = [SB(st, "wgb%d" % i, [128, 8, 256], BF16) for i in range(2)]
            wub = [SB(st, "wub%d" % i, [128, 8, 256], BF16) for i in range(2)]
            wdb = [SB(st, "wdb%d" % i, [128, 2, D], BF16) for i in range(2)]
            sg = Rot([SB(st, "sg%d" % i, [128, 512], F32) for i in range(2)], "sg")
            hid = Rot([SB(st, "hid%d" % i, [128, 2, 512], BF16) for i in range(2)], "hid")
            ot = Rot([SB(st, "ot%d" % i, [128, D], F32) for i in range(1)], "ot")
            jkp = Rot([SB(st, "djk", [128, D], BF16)], "djk")
            ssp = Rot([SB(st, "dss%d" % i, [128, 4], F32) for i in range(4)], "dss")
            psr = Rot(PS[0:8], "psd")
            n2T_v = n2T_s.rearrange("(k p) t -> p k t", p=128)
            for hf in range(NHALF):
                t0 = hf * TH
                for k in range(8):
                    T.dma("sp", n2T[:, k, :], n2T_v[:, k, t0:t0 + TH], writes=["n2T_k%d" % k])
                n2keys = ["n2T_k%d" % k for k in range(8)]
                def d_weights(ex):
                    b = ex % 2
                    T.dma("sp", stg[0][:].rearrange("p (k c) -> p k c", k=8), w_eg[ex].rearrange("(k p) c -> p k c", p=128), writes=["stg0"])
                    T.op("pool", lambda e, b=b: e.tensor_copy(out=wgb[b][:], in_=stg[0][:].rearrange("p (k c) -> p k c", k=8)), reads=["stg0"], writes=["wgb%d" % b])
                    T.dma("sp", stg[1][:].rearrange("p (k c) -> p k c", k=8), w_eu[ex].rearrange("(k p) c -> p k c", p=128), writes=["stg1"])
                    T.op("pool", lambda e, b=b: e.tensor_copy(out=wub[b][:], in_=stg[1][:].rearrange("p (k c) -> p k c", k=8)), reads=["stg1"], writes=["wub%d" % b])
                    T.dma("sp", stg[2][:].rearrange("p (k c) -> p k c", k=2), w_ed[ex].rearrange("(k p) c -> p k c", p=128), writes=["stg2"])
                    T.op("pool", lambda e, b=b: e.tensor_copy(out=wdb[b][:], in_=stg[2][:].rearrange("p (k c) -> p k c", k=2)), reads=["stg2"], writes=["wdb%d" % b])
                    if ex == 0:
                        for s_ in range(NS):
                            T.dma("sp", acc[:, s_, :], h_s[t0 + s_ * 128:t0 + (s_ + 1) * 128, :], writes=["acc%d_0" % s_, "acc%d_1" % s_])

                def d_up(ex, t):
                    b = ex % 2
                    hd, hdk = hid.next()
                    for oc in range(2):
                        pg, pgk = psr.next()
                        mm(pg[:, :], [(wgb[b][:, k, oc * 128:(oc + 1) * 128], n2T[:, k, t * 512:(t + 1) * 512]) for k in range(8)], reads=["wgb%d" % b] + n2keys, writes=[pgk])
                        pu, puk = psr.next()
                        mm(pu[:, :], [(wub[b][:, k, oc * 128:(oc + 1) * 128], n2T[:, k, t * 512:(t + 1) * 512]) for k in range(8)], reads=["wub%d" % b] + n2keys, writes=[puk])
                        s1, s1k = sg.next()
                        T.op("act", lambda e, s1=s1, pg=pg: e.activation(out=s1[:], in_=pg[:, :], func=AF.Silu), reads=[pgk], writes=[s1k])
                        T.op("dve", lambda e, s1=s1, pu=pu, hd=hd, oc=oc: e.tensor_tensor(out=hd[:, oc, :], in0=pu[:, :], in1=s1[:], op=ALU.mult), reads=[puk, s1k], writes=["%s_%d" % (hdk, oc)])
                    return hd, hdk

                def d_down(ex, t, hd, hdk):
                    b = ex % 2
                    for sub in range(4):
                        s_ = t * 4 + sub
                        ti = hf * NS + s_
                        for half in range(2):
                            pd, pdk = psr.next()
                            mm(pd[:, :], [(hd[:, jc, sub * 128:(sub + 1) * 128], wdb[b][:, jc, half * 512:(half + 1) * 512]) for jc in range(2)],
                               reads=["%s_0" % hdk, "%s_1" % hdk, "wdb%d" % b], writes=[pdk])
                            ak = "acc%d_%d" % (s_, half)
                            T.op("dve", lambda e, pd=pd, s_=s_, half=half, ti=ti, ex=ex: e.scalar_tensor_tensor(
                                out=acc[:, s_, half * 512:(half + 1) * 512], in0=pd[:, :], scalar=comb[:, ti, ex:ex + 1],
                                in1=acc[:, s_, half * 512:(half + 1) * 512], op0=ALU.mult, op1=ALU.add), reads=[pdk, "comb", ak], writes=[ak])

                items_d = [(ex, t) for ex in range(NE) for t in range(NBH)]
                d_weights(0)
                nxt_d = d_up(*items_d[0])
                for n, (ex, t) in enumerate(items_d):
                    cur_d = nxt_d
                    if n + 1 < len(items_d):
                        ex2, t2 = items_d[n + 1]
                        if t2 == 0:
                            d_weights(ex2)
                        nxt_d = d_up(ex2, t2)
                    d_down(ex, t, *cur_d)
                for s_ in range(NS):
                    aks = ["acc%d_0" % s_, "acc%d_1" % s_]
                    jk, jkk = jkp.next()
                    ss, sk = ssp.next()
                    T.op("act", lambda e, jk=jk, s_=s_, ss=ss: e.activation(out=jk[:], in_=acc[:, s_, :], func=AF.Square, accum_out=ss[:, 0:1]), reads=aks, writes=[sk, jkk])
                    T.op("dve", lambda e, ss=ss: e.tensor_scalar(out=ss[:, 1:2], in0=ss[:, 0:1], scalar1=1.0 / D, scalar2=EPS, op0=ALU.mult, op1=ALU.add), reads=[sk], writes=[sk])
                    T.op("pool", lambda e, ss=ss: e.tensor_tensor(out=ss[:, 2:3], in0=ss[:, 1:2], in1=mh[:, 0:1], op=ALU.pow), reads=[sk, "mh"], writes=[sk])
                    o_, ok = ot.next()
                    T.op("dve", lambda e, o_=o_, s_=s_, ss=ss: e.scalar_tensor_tensor(out=o_[:], in0=acc[:, s_, :], scalar=ss[:, 2:3], in1=gbc["g_fin"][:], op0=ALU.mult, op1=ALU.mult),
                         reads=aks + [sk, "bc_g_fin"], writes=[ok])
                    T.dma("sp", out[t0 + s_ * 128:t0 + (s_ + 1) * 128, :], o_[:], reads=[ok], writes=["out%d" % (t0 // 128 + s_)])

        with nc.Block() as block:
            T.finish(block)
    return nc


def _host_inputs(inputs):
    f = lambda a: np.ascontiguousarray(np.asarray(a))
    x = f(inputs["x"]); mem = f(inputs["mem"]); positions = f(inputs["positions"])
    B = x.shape[0]
    shared = {
        "g_mix": f(inputs["g_mix"])[0], "g_mem": f(inputs["g_mem"])[0], "g_ffn": f(inputs["g_ffn"])[0], "g_fin": f(inputs["g_final"]),
        "g_q": f(inputs["g_q"])[0], "g_kv": f(inputs["g_kv"])[0],
        "w_in": f(inputs["w_in"])[0],
        "cw": f(f(inputs["conv_w"])[0].T.reshape(4, 128, 4).transpose(1, 0, 2)),
        "cb": f(f(inputs["conv_b"])[0].reshape(4, 128).T),
        "lba": f(f(inputs["lru_ba"])[0].reshape(4, 128).T),
        "lbx": f(f(inputs["lru_bx"])[0].reshape(4, 128).T),
        "llam": f(f(inputs["lru_lambda"])[0].reshape(4, 128).T),
        "lwa": f(inputs["lru_wa"])[0], "lwx": f(inputs["lru_wx"])[0],
        "w_uq": f(inputs["w_uq"])[0], "w_ukv": f(inputs["w_ukv"])[0], "w_mkv": f(inputs["w_mem_kv"])[0],
        "w_br": f(inputs["w_branch"])[0], "w_o": f(inputs["w_o"])[0],
        "w_gr": f(np.concatenate([f(inputs["w_group"])[0], f(inputs["w_router"])[0]], axis=1)),
        "b_gr": f(np.concatenate([f(inputs["b_group"])[0], f(inputs["b_router"])[0]], axis=0)),
        "w_eg": f(inputs["w_e_gate"])[0], "w_eu": f(inputs["w_e_up"])[0], "w_ed": f(inputs["w_e_down"])[0],
    }
    ident = np.eye(128, dtype=np.float32)
    kk = np.arange(128)[:, None]; qq = np.arange(512)[None, :]
    cmask = np.stack([((128 * r + kk) <= qq).astype(np.float32) for r in range(4)], axis=0)
    invf = np.broadcast_to((10000.0 ** (-np.arange(0, 32, 2, dtype=np.float32) / 32.0)).astype(np.float32)[None, :], (128, 16)).copy()
    shared.update({"ident": ident, "cmask": cmask, "invf": invf})
    shared = {k: np.ascontiguousarray(v, dtype=np.float32) for k, v in shared.items()}
    maps = []
    for b in range(B):
        m = dict(shared)
        m["x"] = x[b]
        m["mem"] = mem[b]
        m["pos"] = np.ascontiguousarray(positions[b].reshape(NT, 128).T.astype(np.int32))
        maps.append(m)
    return maps


_NC_CACHE = {}


def kernel(**inputs):
    maps = _host_inputs(inputs)
    if "nc" not in _NC_CACHE:
        _NC_CACHE["nc"] = build_nc(False)
    nc = _NC_CACHE["nc"]
    res = run_bass_kernel_spmd(nc, maps, core_ids=list(range(len(maps))))
    return np.stack([np.asarray(r["out"], dtype=np.float32) for r in res.results], axis=0)
```

```python
import math
import numpy as np
from contextlib import ExitStack
import concourse.bass as bass
import concourse.mybir as mybir
from concourse.bass_utils import run_bass_kernel_spmd

F32 = mybir.dt.float32
BF16 = mybir.dt.bfloat16
I32 = mybir.dt.int32
AF = mybir.ActivationFunctionType
ALU = mybir.AluOpType
AX = mybir.AxisListType

S = 4096
D = 1024
NT = S // 128
NB = S // 512
EPS = 1e-6
DIN = 5024
NE = 32
NHALF = 2


class Trk:
    ENG = ("pe", "act", "dve", "pool", "sp")
    SELF = True
    STEP = 0
    FINAL = None

    def __init__(self, nc, stack, n_dma_sems=12):
        self.nc = nc
        self.prog = {e: [] for e in self.ENG}
        self.sem = {e: stack.enter_context(nc.semaphore("sem_" + e)) for e in self.ENG}
        self.nops = {e: 0 for e in self.ENG}
        self.waited = {e: set() for e in self.ENG}
        self.known_c = {e: {e2: 0 for e2 in self.ENG} for e in self.ENG}
        self.known_d = {e: {} for e in self.ENG}
        self.dsem = {}
        self.drr = {}
        self.dcnt = {}
        self.dobj = {}
        for q in ("sp", "pool", "act"):
            self.dsem[q] = [stack.enter_context(nc.semaphore("dsem_%s_%d" % (q, i))) for i in range(n_dma_sems)]
            self.drr[q] = 0
            for s in self.dsem[q]:
                self.dcnt[id(s)] = 0
                self.dobj[id(s)] = s
        self.tiles = {}
        self.mute = False

    def _st(self, k):
        st = self.tiles.get(k)
        if st is None:
            st = {"w": None, "r": []}
            self.tiles[k] = st
        return st

    def _deps(self, reads, writes):
        evs = []
        for k in reads:
            st = self._st(k)
            if st["w"] is not None:
                evs.append(st["w"])
            if k.startswith("ps"):
                evs.extend(st["r"])
        for k in writes:
            st = self._st(k)
            if st["w"] is not None:
                evs.append(st["w"])
            evs.extend(st["r"])
        return evs

    def _emit_waits(self, e, evs, is_dma=False):
        cmax = {}
        dmax = {}
        for ev in evs:
            if ev[0] == "c":
                _, e2, idx = ev
                if e2 == e and (e == "pe" or (not is_dma and not self.SELF)):
                    continue
                if cmax.get(e2, 0) < idx:
                    cmax[e2] = idx
            else:
                _, sid, v = ev
                if dmax.get(sid, 0) < v:
                    dmax[sid] = v
        for e2, idx in cmax.items():
            if self.known_c[e][e2] >= idx:
                continue
            k0 = self.known_c[e][e2]
            if self.STEP and e2 != e:
                for v in range(k0 + self.STEP, idx, self.STEP):
                    self.waited[e2].add(v)
                    self.prog[e].append(("wc", e2, v))
            self.known_c[e][e2] = idx
            self.waited[e2].add(idx)
            self.prog[e].append(("wc", e2, idx))
        for sid, v in dmax.items():
            if self.known_d[e].get(sid, 0) >= v:
                continue
            self.known_d[e][sid] = v
            self.prog[e].append(("wd", sid, v))

    def _mark(self, ev, reads, writes):
        for k in reads:
            r = self._st(k)["r"]
            r.append(ev)
            if len(r) > 24:
                best = {}
                for x in r:
                    key = (x[0], x[1])
                    if key not in best or best[key][2] < x[2]:
                        best[key] = x
                r[:] = list(best.values())
        for k in writes:
            st = self._st(k)
            st["w"] = ev
            st["r"] = []

    def op(self, e, fn, reads=(), writes=()):
        return self.group(e, [fn], reads, writes)

    def group(self, e, fns, reads=(), writes=()):
        if self.mute:
            return None
        self._emit_waits(e, self._deps(reads, writes))
        self.nops[e] += 1
        idx = self.nops[e]
        self.prog[e].append(("op", list(fns), idx))
        ev = ("c", e, idx)
        self._mark(ev, reads, writes)
        return ev

    def dma(self, q, out, in_, reads=(), writes=(), **kw):
        if self.mute:
            return None
        pool = self.dsem[q]
        s = pool[self.drr[q] % len(pool)]
        self.drr[q] += 1
        sid = id(s)
        evs = self._deps(reads, writes)
        if self.dcnt[sid] > 0:
            evs.append(("d", sid, self.dcnt[sid]))
        self._emit_waits(q, evs, is_dma=True)
        self.dcnt[sid] += 16
        ev = ("d", sid, self.dcnt[sid])
        self.prog[q].append(("dma", s, out, in_, kw))
        self._mark(ev, reads, writes)
        return ev

    def barrier(self, engines=None):
        if self.mute:
            return
        evs = [("c", e2, self.nops[e2]) for e2 in self.ENG if self.nops[e2] > 0]
        evs += [("d", sid, v) for sid, v in self.dcnt.items() if v > 0]
        for e in (engines or self.ENG):
            self._emit_waits(e, evs)
        self.tiles = {}

    def finish_early(self):
        self.barrier(self.FINAL)
        self.mute = True

    def finish(self, block):
        self.mute = False
        self.barrier(self.FINAL)
        rank = {e: {idx: i + 1 for i, idx in enumerate(sorted(self.waited[e]))} for e in self.ENG}
        t = self

        def run(e, en):
            for it in t.prog[e]:
                if it[0] == "wc":
                    en.wait_ge(t.sem[it[1]], rank[it[1]][it[2]])
                elif it[0] == "wd":
                    en.wait_ge(t.dobj[it[1]], it[2])
                elif it[0] == "dma":
                    en.dma_start(out=it[2], in_=it[3], **it[4]).then_inc(it[1], 16)
                else:
                    fns, idx = it[1], it[2]
                    for i, fn in enumerate(fns):
                        r = fn(en)
                        if i == len(fns) - 1 and idx in rank[e]:
                            r.then_inc(t.sem[e], 1)

        @block.sync
        def _(en):
            run("sp", en)

        @block.tensor
        def _(en):
            run("pe", en)

        @block.scalar
        def _(en):
            run("act", en)

        @block.vector
        def _(en):
            run("dve", en)

        @block.gpsimd
        def _(en):
            run("pool", en)


class Rot:
    def __init__(self, bufs, name, shared=False):
        self.bufs = bufs
        self.name = name
        self.i = 0
        self.shared = shared

    def next(self):
        j = self.i % len(self.bufs)
        self.i += 1
        return self.bufs[j], "%s#%d" % (self.name, 0 if self.shared else j)


def build_nc(dbg=False, stop=99, substop=99):
    EXP = ""
    nc = bass.Bass("TRN2", target_bir_lowering=False)

    def din(name, shape, dt=F32):
        return nc.dram_tensor(name, list(shape), dt, kind="ExternalInput").ap()

    def dscr(name, shape, dt):
        return nc.dram_tensor(name, list(shape), dt, kind=("ExternalOutput" if dbg else "Internal")).ap()

    x = din("x", [S, D])
    mem = din("mem", [256, D])
    pos = din("pos", [128, NT], I32)
    g_mix = din("g_mix", [D]); g_mem = din("g_mem", [D]); g_ffn = din("g_ffn", [D]); g_fin = din("g_fin", [D])
    g_q = din("g_q", [256]); g_kv = din("g_kv", [128])
    w_in = din("w_in", [D, DIN])
    cw = din("cw", [128, 4, 4]); cb = din("cb", [128, 4]); lba = din("lba", [128, 4]); lbx = din("lbx", [128, 4]); llam = din("llam", [128, 4])
    lwa = din("lwa", [8, 64, 64]); lwx = din("lwx", [8, 64, 64])
    w_uq = din("w_uq", [256, 768]); w_ukv = din("w_ukv", [128, 1024]); w_mkv = din("w_mkv", [D, D])
    w_br = din("w_br", [3, 512, D]); w_o = din("w_o", [D, D])
    w_gr = din("w_gr", [D, 36]); b_gr = din("b_gr", [36])
    w_eg = din("w_eg", [NE, D, 256]); w_eu = din("w_eu", [NE, D, 256]); w_ed = din("w_ed", [NE, 256, D])
    ident = din("ident", [128, 128]); cmask = din("cmask", [4, 128, 512]); invf = din("invf", [128, 16])
    out = nc.dram_tensor("out", [S, D], F32, kind="ExternalOutput").ap()

    ya_s = dscr("ya_s", [512, S], BF16); yb_s = dscr("yb_s", [512, S], BF16); yc_s = dscr("yc_s", [512, S], BF16)
    h_s = dscr("h_s", [S, D], F32); n2T_s = dscr("n2T_s", [D, S], BF16)
    comb_s = dscr("comb_s", [128, NT, NE], F32) if dbg else None

    with ExitStack() as top:
        T = Trk(nc, top)

        def SB(st, name, shape, dt):
            return st.enter_context(nc.sbuf_tensor(name, list(shape), dt))

        PS = [top.enter_context(nc.psum_tensor("ps%d" % i, [128, 512], F32)) for i in range(8)]

        def mm(out_ap, pairs, reads, writes):
            n = len(pairs)
            fns = []
            for i, (l, r) in enumerate(pairs):
                fns.append(lambda e, l=l, r=r, i=i: e.matmul(out_ap, lhsT=l, rhs=r, start=(i == 0), stop=(i == n - 1)))
            return T.group("pe", fns, reads, writes)

        idf = SB(top, "idf", [128, 128], F32)
        idb = SB(top, "idb", [128, 128], BF16)
        onesb = SB(top, "onesb", [128, 128], BF16)
        mh = SB(top, "mh", [128, 2], F32)
        T.dma("sp", idf[:], ident, writes=["idf"])
        T.dma("pool", idb[:], ident, writes=["idb"])
        T.op("dve", lambda e: e.memset(onesb[:], 1.0), writes=["onesb"])
        T.op("dve", lambda e: e.memset(mh[:], -0.5), writes=["mh"])
        gbc = {}
        gsrc = {"g_mix": g_mix, "g_mem": g_mem, "g_ffn": g_ffn, "g_fin": g_fin}

        def load_g(st_, nm, tag):
            t_ = SB(st_, "bc_%s_%s" % (nm, tag), [128, D], F32)
            T.dma("sp", t_[:], gsrc[nm].partition_broadcast(128), writes=["bc_" + nm])
            gbc[nm] = t_
        gq_bc = SB(top, "gq_bc", [128, 256], F32); T.dma("sp", gq_bc[:], g_q.partition_broadcast(128), writes=["gq_bc"])
        gkv_bc = SB(top, "gkv_bc", [128, 128], F32); T.dma("sp", gkv_bc[:], g_kv.partition_broadcast(128), writes=["gkv_bc"])
        bgr_bc = SB(top, "bgr_bc", [128, 36], F32); T.dma("sp", bgr_bc[:], b_gr.partition_broadcast(128), writes=["bgr_bc"])
        stAB = ExitStack()
        cosT = SB(stAB, "cosT", [128, NT, 16], F32)
        sinT = SB(stAB, "sinT", [128, NT, 16], F32)
        cqnT = SB(stAB, "cqnT", [128, 2, S], BF16)
        ckvnT = SB(stAB, "ckvnT", [128, S], BF16)
        kropeT = SB(stAB, "kropeT", [96, S], BF16)

        with ExitStack() as st:
            posi = SB(st, "posi", [128, NT], I32)
            posf = SB(st, "posf", [128, NT], F32)
            ivf = SB(st, "ivf", [128, 16], F32)
            ang = SB(st, "ang", [128, NT, 16], F32)
            kf = SB(st, "kf", [128, NT, 16], F32)
            ki = SB(st, "ki", [128, NT, 16], I32)
            T.dma("sp", posi[:], pos, writes=["posi"])
            T.dma("sp", ivf[:], invf, writes=["ivf"])
            T.op("dve", lambda e: e.tensor_copy(out=posf[:], in_=posi[:]), reads=["posi"], writes=["posf"])
            T.op("dve", lambda e: e.tensor_tensor(out=ang[:], in0=posf[:].unsqueeze(2).to_broadcast([128, NT, 16]),
                                                  in1=ivf[:].unsqueeze(1).to_broadcast([128, NT, 16]), op=ALU.mult),
                 reads=["posf", "ivf"], writes=["ang"])
            TWO_PI = 2.0 * math.pi
            for shift, dst, nm in ((0.0, sinT, "sinT"), (math.pi / 2.0, cosT, "cosT")):
                T.op("dve", lambda e, shift=shift: e.tensor_scalar(out=kf[:], in0=ang[:], scalar1=shift, scalar2=1.0 / TWO_PI, op0=ALU.add, op1=ALU.mult),
                     reads=["ang"], writes=["kf"])
                T.op("dve", lambda e: e.tensor_copy(out=ki[:], in_=kf[:]), reads=["kf"], writes=["ki"])
                T.op("dve", lambda e: e.tensor_copy(out=kf[:], in_=ki[:]), reads=["ki"], writes=["kf"])
                T.op("dve", lambda e, shift=shift: e.tensor_scalar(out=kf[:], in0=kf[:], scalar1=-TWO_PI, scalar2=shift, op0=ALU.mult, op1=ALU.add),
                     reads=["kf"], writes=["kf"])
                T.op("dve", lambda e: e.tensor_tensor(out=kf[:], in0=kf[:], in1=ang[:], op=ALU.add), reads=["kf", "ang"], writes=["kf"])
                T.op("dve", lambda e: e.tensor_scalar(out=kf[:], in0=kf[:], scalar1=math.pi, scalar2=-math.pi, op0=ALU.min, op1=ALU.max),
                     reads=["kf"], writes=["kf"])
                T.op("act", lambda e, dst=dst: e.activation(out=dst[:], in_=kf[:], func=AF.Sin), reads=["kf"], writes=[nm])
            T.barrier()
            if stop == 0:
                T.finish_early()

        def norm_T(st_pools, src_rows, gb, gkey, dstT, dst_cols, dkey, keep_x=None):
            xt, xk = st_pools["xt"].next() if keep_x is None else keep_x
            T.dma("sp", xt[:], src_rows, writes=[xk])
            jk, jkk = st_pools["junk"].next()
            ss, sk = st_pools["ss"].next()
            T.op("act", lambda e: e.activation(out=jk[:], in_=xt[:], func=AF.Square, accum_out=ss[:, 0:1]), reads=[xk], writes=[sk, jkk])
            T.op("dve", lambda e: e.tensor_scalar(out=ss[:, 1:2], in0=ss[:, 0:1], scalar1=1.0 / D, scalar2=EPS, op0=ALU.mult, op1=ALU.add), reads=[sk], writes=[sk])
            T.op("pool", lambda e: e.tensor_tensor(out=ss[:, 2:3], in0=ss[:, 1:2], in1=mh[:, 0:1], op=ALU.pow), reads=[sk, "mh"], writes=[sk])
            nb, nk = st_pools["nb"].next()
            T.op("dve", lambda e: e.scalar_tensor_tensor(out=nb[:], in0=xt[:], scalar=ss[:, 2:3], in1=gb[:], op0=ALU.mult, op1=ALU.mult),
                 reads=[xk, sk, gkey], writes=[nk])
            pt, pk = st_pools["ps"].next()
            ptb = pt[:].bitcast(BF16)
            T.group("pe", [(lambda e, k=k: e.transpose(out=ptb[:, k * 128:(k + 1) * 128], in_=nb[:, k * 128:(k + 1) * 128], identity=idb[:])) for k in range(8)],
                    reads=[nk, "idb"], writes=[pk])
            T.op("act", lambda e: e.activation(out=dstT[:, :, dst_cols], in_=ptb.rearrange("p (k t) -> p k t", k=8), func=AF.Copy),
                 reads=[pk], writes=[dkey])
            return xt, xk

        kmemT = SB(stAB, "kmemT", [128, 4, 256], BF16)
        vmem = SB(stAB, "vmem", [128, 2, 512], BF16)
        with ExitStack() as st:
            load_g(st, "g_mem", "a0")
            pools = {
                "xt": Rot([SB(st, "a0xt%d" % i, [128, D], F32) for i in range(2)], "a0xt"),
                "junk": Rot([SB(st, "a0jk", [128, D], BF16)], "a0jk"),
                "ss": Rot([SB(st, "a0ss%d" % i, [128, 4], F32) for i in range(2)], "a0ss"),
                "nb": Rot([SB(st, "a0nb%d" % i, [128, D], BF16) for i in range(2)], "a0nb"),
                "ps": Rot(PS[0:4], "ps"),
            }
            psr = Rot(PS[4:8], "psb")
            memT = SB(st, "memT", [128, 8, 256], BF16)
            wmk = SB(st, "wmk", [128, 8, D], BF16)
            for k in range(8):
                T.dma("pool", wmk[:, k, :], w_mkv[k * 128:(k + 1) * 128, :], writes=["wmk%d" % k])
            for mt in range(2):
                norm_T(pools, mem[mt * 128:(mt + 1) * 128, :], gbc["g_mem"], "bc_g_mem", memT, slice(mt * 128, (mt + 1) * 128), "memT%d" % mt)
            wk = ["wmk%d" % k for k in range(8)]
            for h in range(4):
                p, pk = psr.next()
                mm(p[:, 0:256], [(wmk[:, k, h * 128:(h + 1) * 128], memT[:, k, :]) for k in range(8)], reads=wk + ["memT0", "memT1"], writes=[pk])
                T.op("act", lambda e, p=p, h=h: e.activation(out=kmemT[:, h, :], in_=p[:, 0:256], func=AF.Copy), reads=[pk], writes=["kmemT"])
            for mt in range(2):
                p, pk = psr.next()
                mm(p[:, :], [(memT[:, k, mt * 128:(mt + 1) * 128], wmk[:, k, 512:1024]) for k in range(8)], reads=wk + ["memT%d" % mt], writes=[pk])
                T.op("act", lambda e, p=p, mt=mt: e.activation(out=vmem[:, mt, :], in_=p[:, :], func=AF.Copy), reads=[pk], writes=["vmem"])
            T.barrier()
            if stop == 1:
                T.finish_early()

        with ExitStack() as st:
            load_g(st, "g_mix", "a")
            pools = {
                "xt": Rot([SB(st, "axt%d" % i, [128, D], F32) for i in range(4)], "axt"),
                "junk": Rot([SB(st, "ajk", [128, D], BF16)], "ajk"),
                "ss": Rot([SB(st, "ass%d" % i, [128, 4], F32) for i in range(8)], "ass"),
                "nb": Rot([SB(st, "anb%d" % i, [128, D], BF16) for i in range(4)], "anb"),
                "ps": Rot(PS[0:2], "psn"),
            }
            psr = Rot(PS[2:8], "psa")
            NA = 1952
            win = SB(st, "win", [128, 8, NA], BF16)
            for k in range(8):
                for (c0, c1) in ((0, 1024), (1024, NA)):
                    T.dma("pool", win[:, k, c0:c1], w_in[k * 128:(k + 1) * 128, c0:c1], writes=["win%d_%d" % (k, c0)])
            winkeys = ["win%d_%d" % (k, c0) for k in range(8) for c0 in (0, 1024)]
            wabd = SB(st, "wabd", [128, 4, 128], BF16)
            wxbd = SB(st, "wxbd", [128, 4, 128], BF16)
            T.op("dve", lambda e: e.memset(wabd[:], 0.0), writes=["wabd"])
            T.op("dve", lambda e: e.memset(wxbd[:], 0.0), writes=["wxbd"])
            for c in range(4):
                for j in range(2):
                    T.dma("pool", wabd[j * 64:(j + 1) * 64, c, j * 64:(j + 1) * 64], lwa[2 * c + j], reads=["wabd"], writes=["wabd"])
                    T.dma("pool", wxbd[j * 64:(j + 1) * 64, c, j * 64:(j + 1) * 64], lwx[2 * c + j], reads=["wxbd"], writes=["wxbd"])
            cws = SB(st, "cws", [128, 4, 4], F32); T.dma("sp", cws[:], cw, writes=["cws"])
            cbs = SB(st, "cbs", [128, 4], F32); T.dma("sp", cbs[:], cb, writes=["cbs"])
            bas = SB(st, "bas", [128, 4], F32); T.dma("sp", bas[:], lba, writes=["bas"])
            bxs = SB(st, "bxs", [128, 4], F32); T.dma("sp", bxs[:], lbx, writes=["bxs"])
            lam = SB(st, "lam", [128, 4], F32); T.dma("sp", lam[:], llam, writes=["lam"])
            sp1 = SB(st, "sp1", [128, 4], F32); sp2 = SB(st, "sp2", [128, 4], F32); c1s = SB(st, "c1s", [128, 4], F32)
            T.op("dve", lambda e: e.tensor_scalar(out=sp1[:], in0=lam[:], scalar1=-1.0, scalar2=None, op0=ALU.mult), reads=["lam"], writes=["sp1"])
            T.op("dve", lambda e: e.tensor_tensor(out=sp1[:], in0=sp1[:], in1=lam[:], op=ALU.max), reads=["lam", "sp1"], writes=["sp1"])
            T.op("act", lambda e: e.activation(out=sp1[:], in_=sp1[:], func=AF.Exp, scale=-1.0), reads=["sp1"], writes=["sp1"])
            T.op("act", lambda e: e.activation(out=sp1[:], in_=sp1[:], func=AF.Ln, bias=1.0, scale=1.0), reads=["sp1"], writes=["sp1"])
            T.op("dve", lambda e: e.tensor_scalar(out=sp2[:], in0=lam[:], scalar1=-1.0, scalar2=0.0, op0=ALU.mult, op1=ALU.max), reads=["lam"], writes=["sp2"])
            T.op("dve", lambda e: e.tensor_tensor(out=c1s[:], in0=sp1[:], in1=sp2[:], op=ALU.add), reads=["sp1", "sp2"], writes=["c1s"])
            T.op("dve", lambda e: e.tensor_scalar(out=c1s[:], in0=c1s[:], scalar1=-8.0, scalar2=None, op0=ALU.mult), reads=["c1s"], writes=["c1s"])

            if stop == 2 and substop == 1:
                T.finish_early()
            nTp = Rot([SB(st, "anT%d" % i, [128, 8, 512], BF16) for i in range(1)], "anT")
            xl = [SB(st, "xl%d" % c, [128, 515], F32) for c in range(4)]
            hprev = SB(st, "hprev", [128, 4], F32)
            T.op("dve", lambda e: e.memset(hprev[:], 0.0), writes=["hprev"])
            for c in range(4):
                T.op("dve", lambda e, c=c: e.memset(xl[c][:], 0.0), writes=["xl%d" % c])
            xa = [SB(st, "xa%d" % c, [128, 512], F32) for c in range(4)]
            xab = [SB(st, "xab%d" % c, [128, 512], BF16) for c in range(4)]
            rr = [SB(st, "rr%d" % c, [128, 512], F32) for c in range(4)]
            ig = [SB(st, "ig%d" % c, [128, 512], F32) for c in range(4)]
            gl = [SB(st, "gl%d" % c, [128, 512], BF16) for c in range(4)]
            sq = [SB(st, "sq%d" % c, [128, 512], F32) for c in range(4)]
            hs = sq
            yaT = Rot([SB(st, "yaT%d" % i, [128, 4, 512], BF16) for i in range(1)], "yaT")
            ycT = Rot([SB(st, "ycT%d" % i, [128, 4, 512], BF16) for i in range(1)], "ycT")
            qmT = Rot([SB(st, "qmT%d" % i, [128, 512], BF16) for i in range(3)], "qmT")
            pTm = Rot([SB(st, "pTm%d" % i, [128, 512], BF16) for i in range(6)], "pTm")
            rden = Rot([SB(st, "rdm%d" % i, [128, 512], F32) for i in range(2)], "rdm")
            latn = Rot([SB(st, "latn%d" % i, [128, 480], BF16) for i in range(4)], "latn")
            for i in range(4):
                T.op("pool", lambda e, i=i: e.memset(latn.bufs[i][:], 0.0), writes=["latn#%d" % i])
            ss2 = Rot([SB(st, "ss2_%d" % i, [128, 8], F32) for i in range(4)], "ss2")
            rt = Rot([SB(st, "rt%d" % i, [128, 4, 16], F32) for i in range(4)], "rt")
            ya_v = ya_s.rearrange("(c p) t -> p c t", p=128)
            yc_v = yc_s.rearrange("(c p) t -> p c t", p=128)

            if stop == 2 and substop == 11:
                T.finish_early()
            def a_s1(i):
                nbs = []
                for sub_ in range(4):
                    r0 = i * 512 + sub_ * 128
                    xt, xk = pools["xt"].next()
                    T.dma("sp", xt[:], x[r0:r0 + 128, :], writes=[xk])
                    jk, jkk = pools["junk"].next()
                    ss, sk = pools["ss"].next()
                    T.op("act", lambda e, jk=jk, xt=xt, ss=ss: e.activation(out=jk[:], in_=xt[:], func=AF.Square, accum_out=ss[:, 0:1]), reads=[xk], writes=[sk, jkk])
                    T.op("dve", lambda e, ss=ss: e.tensor_scalar(out=ss[:, 1:2], in0=ss[:, 0:1], scalar1=1.0 / D, scalar2=EPS, op0=ALU.mult, op1=ALU.add), reads=[sk], writes=[sk])
                    T.op("pool", lambda e, ss=ss: e.tensor_tensor(out=ss[:, 2:3], in0=ss[:, 1:2], in1=mh[:, 0:1], op=ALU.pow), reads=[sk, "mh"], writes=[sk])
                    nb, nk = pools["nb"].next()
                    T.op("dve", lambda e, nb=nb, xt=xt, ss=ss, gb=gbc["g_mix"]: e.scalar_tensor_tensor(out=nb[:], in0=xt[:], scalar=ss[:, 2:3], in1=gb[:], op0=ALU.mult, op1=ALU.mult),
                         reads=[xk, sk, "bc_g_mix"], writes=[nk])
                    nbs.append((nb, nk))
                return nbs

            def a_s2(nbs):
                nT, nTk = nTp.next()
                nTkeys = []
                for sub_ in range(4):
                    nb, nk = nbs[sub_]
                    pt, pk = pools["ps"].next()
                    ptb = pt[:].bitcast(BF16)
                    T.group("pe", [(lambda e, ptb=ptb, nb=nb, k=k: e.transpose(out=ptb[:, k * 128:(k + 1) * 128], in_=nb[:, k * 128:(k + 1) * 128], identity=idb[:])) for k in range(8)],
                            reads=[nk, "idb"], writes=[pk])
                    dk = "%s_s%d" % (nTk, sub_)
                    T.op("act", lambda e, ptb=ptb, nT=nT, sub_=sub_: e.activation(out=nT[:, :, sub_ * 128:(sub_ + 1) * 128], in_=ptb.rearrange("p (k t) -> p k t", k=8), func=AF.Copy),
                         reads=[pk], writes=[dk])
                    nTkeys.append(dk)
                return nT, nTk, nTkeys

            nbs_next = a_s1(0)
            for i in range(NB):
                nT, nTk, nTkeys = a_s2(nbs_next)
                if stop == 2 and substop == 2 and i == 0:
                    T.finish_early()
                LS = []
                for sub in range(4):
                    ti = i * 4 + sub
                    p, pk = psr.next()
                    mm(p[:, 0:416], [(nT[:, k, sub * 128:(sub + 1) * 128], win[:, k, 1024:1440]) for k in range(8)], reads=winkeys + [nTkeys[sub]], writes=[pk])
                    LS.append({"ti": ti, "tok": slice(ti * 128, (ti + 1) * 128), "p": p, "pk": pk})
                for L_ in LS:
                    p, pk = L_["p"], L_["pk"]
                    s2, s2k = ss2.next()
                    jk, jkk = pools["junk"].next()
                    T.op("act", lambda e, p=p, s2=s2, jk=jk: e.activation(out=jk[:, 0:256], in_=p[:, 0:256], func=AF.Square, accum_out=s2[:, 0:1]), reads=[pk], writes=[s2k, jkk])
                    T.op("act", lambda e, p=p, s2=s2, jk=jk: e.activation(out=jk[:, 256:384], in_=p[:, 256:384], func=AF.Square, accum_out=s2[:, 1:2]), reads=[pk], writes=[s2k, jkk])
                    L_["s2"], L_["s2k"] = s2, s2k
                for L_ in LS:
                    s2, s2k = L_["s2"], L_["s2k"]
                    T.op("dve", lambda e, s2=s2: e.tensor_scalar(out=s2[:, 2:3], in0=s2[:, 0:1], scalar1=1.0 / 256, scalar2=EPS, op0=ALU.mult, op1=ALU.add), reads=[s2k], writes=[s2k])
                    T.op("dve", lambda e, s2=s2: e.tensor_scalar(out=s2[:, 3:4], in0=s2[:, 1:2], scalar1=1.0 / 128, scalar2=EPS, op0=ALU.mult, op1=ALU.add), reads=[s2k], writes=[s2k])
                for L_ in LS:
                    s2, s2k = L_["s2"], L_["s2k"]
                    T.op("pool", lambda e, s2=s2: e.tensor_tensor(out=s2[:, 4:6], in0=s2[:, 2:4], in1=mh[:, 0:2], op=ALU.pow), reads=[s2k, "mh"], writes=[s2k])
                for L_ in LS:
                    p, pk, s2, s2k = L_["p"], L_["pk"], L_["s2"], L_["s2k"]
                    ln_, lk = latn.next()
                    T.op("dve", lambda e, p=p, s2=s2, ln_=ln_: e.scalar_tensor_tensor(out=ln_[:, 0:256], in0=p[:, 0:256], scalar=s2[:, 4:5], in1=gq_bc[:], op0=ALU.mult, op1=ALU.mult),
                         reads=[pk, s2k, "gq_bc"], writes=[lk])
                    T.op("dve", lambda e, p=p, s2=s2, ln_=ln_: e.scalar_tensor_tensor(out=ln_[:, 256:384], in0=p[:, 256:384], scalar=s2[:, 5:6], in1=gkv_bc[:], op0=ALU.mult, op1=ALU.mult),
                         reads=[pk, s2k, "gkv_bc"], writes=[lk])
                    L_["ln"], L_["lk"] = ln_, lk
                for L_ in LS:
                    p, pk, ti = L_["p"], L_["pk"], L_["ti"]
                    r_, rk = rt.next()
                    cs = cosT[:, ti, :]; sn = sinT[:, ti, :]
                    T.op("dve", lambda e, p=p, r_=r_, cs=cs: e.tensor_tensor(out=r_[:, 0, :], in0=p[:, 384:400], in1=cs, op=ALU.mult), reads=[pk, "cosT"], writes=[rk])
                    T.op("dve", lambda e, p=p, r_=r_, sn=sn: e.tensor_tensor(out=r_[:, 1, :], in0=p[:, 400:416], in1=sn, op=ALU.mult), reads=[pk, "sinT"], writes=[rk])
                    T.op("dve", lambda e, p=p, r_=r_, cs=cs: e.tensor_tensor(out=r_[:, 2, :], in0=p[:, 400:416], in1=cs, op=ALU.mult), reads=[pk, "cosT"], writes=[rk])
                    T.op("dve", lambda e, p=p, r_=r_, sn=sn: e.tensor_tensor(out=r_[:, 3, :], in0=p[:, 384:400], in1=sn, op=ALU.mult), reads=[pk, "sinT"], writes=[rk])
                    L_["r"], L_["rk"] = r_, rk
                for L_ in LS:
                    r_, rk, ln_, lk = L_["r"], L_["rk"], L_["ln"], L_["lk"]
                    T.op("pool", lambda e, r_=r_, ln_=ln_: e.tensor_tensor(out=ln_[:, 448:464], in0=r_[:, 0, :], in1=r_[:, 1, :], op=ALU.subtract), reads=[rk], writes=[lk])
                    T.op("pool", lambda e, r_=r_, ln_=ln_: e.tensor_tensor(out=ln_[:, 464:480], in0=r_[:, 2, :], in1=r_[:, 3, :], op=ALU.add), reads=[rk], writes=[lk])
                if i + 1 < NB:
                    nbs_next = a_s1(i + 1)
                if stop == 2 and substop == 3 and i == 0:
                    T.finish_early()
                for c in range(4):
                    p, pk = psr.next()
                    mm(p[:, :], [(win[:, k, c * 128:(c + 1) * 128], nT[:, k, :]) for k in range(8)], reads=winkeys + nTkeys, writes=[pk])
                    xk = "xl%d" % c
                    T.op("pool", lambda e, c=c: e.tensor_copy(out=xl[c][:, 0:3], in_=xl[c][:, 512:515]), reads=[xk], writes=[xk])
                    T.op("act", lambda e, c=c, p=p: e.activation(out=xl[c][:, 3:515], in_=p[:, :], func=AF.Copy), reads=[pk, xk], writes=[xk])
                for c in range(4):
                    xk = "xl%d" % c
                    T.op("dve", lambda e, c=c: e.tensor_scalar(out=xa[c][:], in0=xl[c][:, 0:512], scalar1=cws[:, c, 0:1], scalar2=cbs[:, c:c + 1], op0=ALU.mult, op1=ALU.add),
                         reads=[xk, "cws", "cbs"], writes=["xa%d" % c])
                    for j in range(1, 4):
                        T.op("dve", lambda e, c=c, j=j: e.scalar_tensor_tensor(out=xa[c][:], in0=xl[c][:, j:j + 512], scalar=cws[:, c, j:j + 1], in1=xa[c][:], op0=ALU.mult, op1=ALU.add),
                             reads=[xk, "cws", "xa%d" % c], writes=["xa%d" % c])
                for c in range(4):
                    T.op("pool", lambda e, c=c: e.tensor_copy(out=xab[c][:], in_=xa[c][:]), reads=["xa%d" % c], writes=["xab%d" % c])
                for c in range(4):
                    p, pk = psr.next()
                    mm(p[:, :], [(win[:, k, 512 + c * 128:512 + (c + 1) * 128], nT[:, k, :]) for k in range(8)], reads=winkeys + nTkeys, writes=[pk])
                    T.op("act", lambda e, c=c, p=p: e.activation(out=gl[c][:], in_=p[:, :], func=AF.Gelu_apprx_tanh), reads=[pk], writes=["gl%d" % c])
                for L_ in LS:
                    ln_, lk = L_["ln"], L_["lk"]
                    pt, ptk = pools["ps"].next()
                    ptb = pt[:].bitcast(BF16)
                    T.group("pe", [
                        lambda e, ptb=ptb, ln_=ln_: e.transpose(out=ptb[:, 0:128], in_=ln_[:, 0:128], identity=idb[:]),
                        lambda e, ptb=ptb, ln_=ln_: e.transpose(out=ptb[:, 128:256], in_=ln_[:, 128:256], identity=idb[:]),
                        lambda e, ptb=ptb, ln_=ln_: e.transpose(out=ptb[:, 256:384], in_=ln_[:, 256:384], identity=idb[:]),
                        lambda e, ptb=ptb, ln_=ln_: e.transpose(out=ptb[0:96, 384:512], in_=ln_[:, 384:480], identity=idb[:]),
                    ], reads=[lk, "idb"], writes=[ptk])
                    tok, ti = L_["tok"], L_["ti"]
                    T.op("act", lambda e, ptb=ptb, tok=tok: e.activation(out=cqnT[:, :, tok], in_=ptb[:, 0:256].rearrange("p (k t) -> p k t", k=2), func=AF.Copy),
                         reads=[ptk], writes=["cqnT%d" % ti])
                    T.op("act", lambda e, ptb=ptb, tok=tok: e.activation(out=ckvnT[:, tok], in_=ptb[:, 256:384], func=AF.Copy), reads=[ptk], writes=["ckvnT%d" % ti])
                    T.op("act", lambda e, ptb=ptb, tok=tok: e.activation(out=kropeT[64:96, tok], in_=ptb[64:96, 384:512], func=AF.Copy), reads=[ptk], writes=["kropeT%d" % ti])
                for c in range(4):
                    p, pk = psr.next()
                    mm(p[:, :], [(wabd[:, c, :], xab[c][:])], reads=["wabd", "xab%d" % c], writes=[pk])
                    T.op("act", lambda e, c=c, p=p: e.activation(out=rr[c][:], in_=p[:, :], func=AF.Sigmoid, bias=bas[:, c:c + 1]), reads=[pk, "bas"], writes=["rr%d" % c])
                    p, pk = psr.next()
                    mm(p[:, :], [(wxbd[:, c, :], xab[c][:])], reads=["wxbd", "xab%d" % c], writes=[pk])
                    T.op("act", lambda e, c=c, p=p: e.activation(out=ig[c][:], in_=p[:, :], func=AF.Sigmoid, bias=bxs[:, c:c + 1]), reads=[pk, "bxs"], writes=["ig%d" % c])
                for c in range(4):
                    T.op("act", lambda e, c=c: e.activation(out=rr[c][:], in_=rr[c][:], func=AF.Exp, scale=c1s[:, c:c + 1]), reads=["rr%d" % c, "c1s"], writes=["rr%d" % c])
                if stop == 2 and substop == 4 and i == 0:
                    T.finish_early()
                ycb, yck = ycT.next()

                def ma_q(h):
                    p, pk = psr.next()
                    mm(p[:, :], [(win[:, k, 1440 + h * 128:1440 + (h + 1) * 128], nT[:, k, :]) for k in range(8)], reads=winkeys + nTkeys, writes=[pk])
                    qm, qmk = qmT.next()
                    T.op("dve", lambda e, qm=qm, p=p: e.tensor_copy(out=qm[:], in_=p[:, :]), reads=[pk], writes=[qmk])
                    return qm, qmk

                def ma_s(h, qm, qmk):
                    pts = []
                    for mc in range(2):
                        p2, p2k = psr.next()
                        mm(p2[:, :], [(kmemT[:, h, mc * 128:(mc + 1) * 128], qm[:])], reads=[qmk], writes=[p2k])
                        pT, pTk = pTm.next()
                        T.op("act", lambda e, pT=pT, p2=p2: e.activation(out=pT[:], in_=p2[:, :], func=AF.Exp, scale=1.0 / math.sqrt(128.0)), reads=[p2k], writes=[pTk])
                        pts.append((pT, pTk))
                    return pts

                def ma_o(h, pts):
                    pn, pnk = psr.next()
                    mm(pn[:, :], [(vmem[:, mc, h * 128:(h + 1) * 128], pts[mc][0][:]) for mc in range(2)], reads=[pts[0][1], pts[1][1]], writes=[pnk])
                    pd, pdk = psr.next()
                    mm(pd[:, :], [(onesb[:], pts[mc][0][:]) for mc in range(2)], reads=[pts[0][1], pts[1][1], "onesb"], writes=[pdk])
                    rd, rdk = rden.next()
                    T.op("dve", lambda e, rd=rd, pd=pd: e.reciprocal(out=rd[:], in_=pd[:, :]), reads=[pdk], writes=[rdk])
                    T.op("dve", lambda e, rd=rd, pn=pn, ycb=ycb, h=h: e.tensor_tensor(out=ycb[:, h, :], in0=pn[:, :], in1=rd[:], op=ALU.mult), reads=[pnk, rdk], writes=[yck])

                q_ = {0: ma_q(0)}
                q_[1] = ma_q(1)
                s_ = {0: ma_s(0, *q_[0])}
                for h in range(4):
                    if h + 2 < 4:
                        q_[h + 2] = ma_q(h + 2)
                    if h + 1 < 4:
                        s_[h + 1] = ma_s(h + 1, *q_[h + 1])
                    ma_o(h, s_[h])
                T.dma("sp", yc_v[:, :, i * 512:(i + 1) * 512], ycb[:], reads=[yck], writes=["yc_s%d" % i])
                if stop == 2 and substop == 5 and i == 0:
                    T.finish_early()
                yab, yak = yaT.next()
                for c in range(4):
                    T.op("pool", lambda e, c=c: e.tensor_tensor(out=sq[c][:], in0=rr[c][:], in1=rr[c][:], op=ALU.mult), reads=["rr%d" % c], writes=["sq%d" % c])
                for c in range(4):
                    T.op("act", lambda e, c=c: e.activation(out=sq[c][:], in_=sq[c][:], func=AF.Sqrt, scale=-1.0, bias=1.0), reads=["sq%d" % c], writes=["sq%d" % c])
                for c in range(4):
                    T.op("pool", lambda e, c=c: e.tensor_tensor(out=ig[c][:], in0=ig[c][:], in1=xa[c][:], op=ALU.mult), reads=["ig%d" % c, "xa%d" % c], writes=["ig%d" % c])
                    T.op("pool", lambda e, c=c: e.tensor_tensor(out=ig[c][:], in0=ig[c][:], in1=sq[c][:], op=ALU.mult), reads=["ig%d" % c, "sq%d" % c], writes=["ig%d" % c])
                    T.op("dve", lambda e, c=c: e.tensor_tensor_scan(out=hs[c][:], data0=rr[c][:], data1=ig[c][:], initial=hprev[:, c:c + 1], op0=ALU.mult, op1=ALU.add),
                         reads=["rr%d" % c, "ig%d" % c, "hprev", "sq%d" % c], writes=["sq%d" % c])
                    T.op("dve", lambda e, c=c: e.tensor_copy(out=hprev[:, c:c + 1], in_=hs[c][:, 511:512]), reads=["sq%d" % c], writes=["hprev"])
                    T.op("pool", lambda e, c=c, yab=yab: e.tensor_tensor(out=yab[:, c, :], in0=hs[c][:], in1=gl[c][:], op=ALU.mult), reads=["sq%d" % c, "gl%d" % c], writes=[yak])
                T.dma("sp", ya_v[:, :, i * 512:(i + 1) * 512], yab[:], reads=[yak], writes=["ya_s%d" % i])
            T.barrier()
            if stop == 2:
                T.finish_early()

        stCW = ExitStack()
        wg = stCW.enter_context(nc.sbuf_tensor("wg", [128, 8, 3072], BF16, side="right"))
        wbr = stCW.enter_context(nc.sbuf_tensor("wbr", [128, 3, 4, D], BF16, side="right"))
        wo = stCW.enter_context(nc.sbuf_tensor("wo", [128, 8, D], BF16, side="right"))
        with ExitStack() as st:
            wuq = SB(st, "wuq", [128, 2, 768], BF16)
            wukv = SB(st, "wukv", [128, 1024], BF16)
            for k in range(2):
                T.dma("pool", wuq[:, k, :], w_uq[k * 128:(k + 1) * 128, :], writes=["wuq"])
            T.dma("pool", wukv[:], w_ukv, writes=["wukv"])
            msk = SB(st, "msk", [128, 4, 512], BF16)
            for r in range(4):
                T.dma("pool", msk[:, r, :], cmask[r], writes=["msk"])
            prefetch_c_weights = True
            KT = [SB(st, "KT%d" % i, [128, S], BF16) for i in range(2)]
            QT = [SB(st, "QT%d" % i, [128, S], BF16) for i in range(2)]
            for i in range(2):
                T.op("pool", lambda e, i=i: e.memset(KT[i][:], 0.0), writes=["KT%d" % i])
                T.op("pool", lambda e, i=i: e.memset(QT[i][:], 0.0), writes=["QT%d" % i])
            VH = [SB(st, "VH%d" % i, [128, NT, 128], BF16) for i in range(2)]
            for i in range(2):
                T.op("pool", lambda e, i=i: e.memset(VH[i][:], 1.0), writes=["VH%d" % i])
            for k in range(8):
                for b3 in range(3):
                    T.dma("pool", wg[:, k, b3 * 1024:(b3 + 1) * 1024], w_in[k * 128:(k + 1) * 128, 1952 + b3 * 1024:1952 + (b3 + 1) * 1024], writes=["wg%d_%d" % (k, b3)])
            for b3 in range(3):
                for kc in range(4):
                    T.dma("pool", wbr[:, b3, kc, :], w_br[b3, kc * 128:(kc + 1) * 128, :], writes=["wbr%d_%d" % (b3, kc)])
            for k in range(8):
                T.dma("pool", wo[:, k, :], w_o[k * 128:(k + 1) * 128, :], writes=["wo%d" % k])
            NQB = 1 if "q1" in EXP else 2
            qtok = Rot([SB(st, "qtok%d" % i, [128, 4, 96], BF16) for i in range(NQB)], "qtok")
            rt = Rot([SB(st, "brt%d" % i, [128, 4, 4, 16], F32) for i in range(NQB)], "brt")
            pTb = Rot([SB(st, "pTb%d" % i, [128, 512], BF16) for i in range(6)], "pTb")
            rdb = Rot([SB(st, "rdb%d" % i, [128, 512], F32) for i in range(2)], "rdb")
            ybT = Rot([SB(st, "ybT%d" % i, [64, 512], BF16) for i in range(2)], "ybT")
            psr = Rot(PS[0:4] + PS[6:8], "psb")
            pso = Rot(PS[4:6], "pso")
            scale = 1.0 / math.sqrt(96.0)
            allck = ["ckvnT%d" % t for t in range(NT)]
            allcq = ["cqnT%d" % t for t in range(NT)]
            allkr = ["kropeT%d" % t for t in range(NT)]
            def prep(h):
                b = h % 2
                KTh, QTh, VHh = KT[b], QT[b], VH[b]
                kk, qk, vk = "KT%d" % b, "QT%d" % b, "VH%d" % b
                for i in range(NB):
                    p, pk = psr.next()
                    mm(p[0:64, :], [(wukv[:, h * 128:h * 128 + 64], ckvnT[:, i * 512:(i + 1) * 512])], reads=["wukv"] + allck[i * 4:(i + 1) * 4], writes=[pk])
                    T.op("act", lambda e, p=p, i=i, KTh=KTh: e.activation(out=KTh[0:64, i * 512:(i + 1) * 512], in_=p[0:64, :], func=AF.Copy), reads=[pk], writes=[kk])
                T.op("act", lambda e, KTh=KTh: e.activation(out=KTh[64:96, :], in_=kropeT[64:96, :], func=AF.Copy), reads=allkr, writes=[kk])
                for g in range(4):
                    p, pk = psr.next()
                    T.group("pe", [(lambda e, p=p, j=j, g=g, h=h: e.matmul(p[:, j * 64:(j + 1) * 64], lhsT=ckvnT[:, (g * 8 + j) * 128:(g * 8 + j + 1) * 128],
                                                                        rhs=wukv[:, h * 128 + 64:h * 128 + 128], start=True, stop=True)) for j in range(8)],
                            reads=["wukv"] + allck[g * 8:(g + 1) * 8], writes=[pk])
                    T.op("dve", lambda e, p=p, g=g, VHh=VHh: e.tensor_copy(out=VHh[:, g * 8:(g + 1) * 8, 0:64], in_=p[:, :].rearrange("p (j d) -> p j d", j=8)), reads=[pk], writes=[vk])
                for g in range(NB):
                    p, pk = psr.next()
                    fns = []
                    for j in range(4):
                        ti = g * 4 + j
                        for k in range(2):
                            fns.append(lambda e, p=p, j=j, k=k, ti=ti, h=h: e.matmul(p[:, j * 96:(j + 1) * 96], lhsT=cqnT[:, k, ti * 128:(ti + 1) * 128],
                                                                                  rhs=wuq[:, k, h * 96:(h + 1) * 96], start=(k == 0), stop=(k == 1)))
                    T.group("pe", fns, reads=["wuq"] + allcq[g * 4:(g + 1) * 4], writes=[pk])
                    pv = p[:, 0:384].rearrange("p (j d) -> p j d", j=4)
                    qt, qtk = qtok.next()
                    T.op("act", lambda e, pv=pv, qt=qt: e.activation(out=qt[:, :, 0:64], in_=pv[:, :, 0:64], func=AF.Copy), reads=[pk], writes=[qtk])
                    r_, rk = rt.next()
                    cs = cosT[:, g * 4:(g + 1) * 4, :]; sn = sinT[:, g * 4:(g + 1) * 4, :]
                    T.op("dve", lambda e, pv=pv, r_=r_, cs=cs: e.tensor_tensor(out=r_[:, 0, :, :], in0=pv[:, :, 64:80], in1=cs, op=ALU.mult), reads=[pk, "cosT"], writes=[rk])
                    T.op("dve", lambda e, pv=pv, r_=r_, sn=sn: e.tensor_tensor(out=r_[:, 1, :, :], in0=pv[:, :, 80:96], in1=sn, op=ALU.mult), reads=[pk, "sinT"], writes=[rk])
                    T.op("dve", lambda e, pv=pv, r_=r_, cs=cs: e.tensor_tensor(out=r_[:, 2, :, :], in0=pv[:, :, 80:96], in1=cs, op=ALU.mult), reads=[pk, "cosT"], writes=[rk])
                    T.op("dve", lambda e, pv=pv, r_=r_, sn=sn: e.tensor_tensor(out=r_[:, 3, :, :], in0=pv[:, :, 64:80], in1=sn, op=ALU.mult), reads=[pk, "sinT"], writes=[rk])
                    T.op("dve", lambda e, r_=r_, qt=qt: e.tensor_tensor(out=qt[:, :, 64:80], in0=r_[:, 0, :, :], in1=r_[:, 1, :, :], op=ALU.subtract), reads=[rk], writes=[qtk])
                    T.op("dve", lambda e, r_=r_, qt=qt: e.tensor_tensor(out=qt[:, :, 80:96], in0=r_[:, 2, :, :], in1=r_[:, 3, :, :], op=ALU.add), reads=[rk], writes=[qtk])
                    pt, ptk = psr.next()
                    ptb = pt[:].bitcast(BF16)
                    T.group("pe", [(lambda e, ptb=ptb, qt=qt, j=j: e.transpose(out=ptb[0:96, j * 128:(j + 1) * 128], in_=qt[:, j, :], identity=idb[:])) for j in range(4)],
                            reads=[qtk, "idb"], writes=[ptk])
                    T.op("dve", lambda e, ptb=ptb, g=g, QTh=QTh: e.tensor_copy(out=QTh[0:96, g * 512:(g + 1) * 512], in_=ptb[0:96, 0:512]), reads=[ptk], writes=[qk])

            LOOK = 4

            def attn(h):
                b = h % 2
                KTh, QTh, VHh = KT[b], QT[b], VH[b]
                kk, qk, vk = "KT%d" % b, "QT%d" % b, "VH%d" % b
                items = [(i, kt) for i in range(NB) for kt in range(4 * i + 4)]
                N = len(items)
                S1 = {}
                acc = {}

                def stage1(n):
                    i, kt = items[n]
                    p, pk = psr.next()
                    mm(p[:, :], [(KTh[:, kt * 128:(kt + 1) * 128], QTh[:, i * 512:(i + 1) * 512])], reads=[kk, qk], writes=[pk])
                    pT, pTk = pTb.next()
                    T.op("act", lambda e, pT=pT, p=p: e.activation(out=pT[:], in_=p[:, :], func=AF.Exp, scale=scale), reads=[pk], writes=[pTk])
                    if kt >= 4 * i:
                        r = kt - 4 * i
                        T.op("dve", lambda e, pT=pT, r=r: e.tensor_tensor(out=pT[:], in0=pT[:], in1=msk[:, r, :], op=ALU.mult), reads=[pTk, "msk"], writes=[pTk])
                    S1[n] = (pT, pTk)

                def stage2(n):
                    i, kt = items[n]
                    nk = 4 * i + 4
                    if kt == 0:
                        acc[i] = pso.next()
                    po, pok = acc[i]
                    pT, pTk = S1.pop(n)
                    T.group("pe", [lambda e, po=po, pT=pT, kt=kt, nk=nk, VHh=VHh: e.matmul(po[:, :], lhsT=VHh[:, kt, :], rhs=pT[:], start=(kt == 0), stop=(kt == nk - 1))],
                            reads=[pTk, vk], writes=[pok])
                    if kt == nk - 1:
                        rd, rdk = rdb.next()
                        T.op("dve", lambda e, rd=rd, po=po: e.reciprocal(out=rd[64:128, :], in_=po[64:128, :]), reads=[pok], writes=[rdk])
                        yb, ybk = ybT.next()
                        T.op("dve", lambda e, rd=rd, po=po, yb=yb: e.tensor_tensor(out=yb[:], in0=po[0:64, :], in1=rd[64:128, :], op=ALU.mult), reads=[pok, rdk], writes=[ybk])
                        T.dma("sp", yb_s[h * 64:(h + 1) * 64, i * 512:(i + 1) * 512], yb[:], reads=[ybk], writes=["yb_s_%d_%d" % (h, i)])

                for n in range(min(LOOK, N)):
                    stage1(n)
                for n in range(N):
                    if n + LOOK < N:
                        stage1(n + LOOK)
                    stage2(n)

            prep(0)
            for h in range(8):
                if h + 1 < 8:
                    prep(h + 1)
                attn(h)
            T.barrier()
            if stop == 3:
                T.finish_early()

        stAB.close()
        comb = SB(top, "comb", [128, NT, NE], F32)
        lg = SB(top, "lg", [128, NT, 36], F32)
        with ExitStack() as st:
            load_g(st, "g_mix", "c")
            load_g(st, "g_ffn", "c")
            pools = {
                "xt": Rot([SB(st, "cxt%d" % i, [128, D], F32) for i in range(4)], "cxt"),
                "junk": Rot([SB(st, "cjk", [128, D], BF16)], "cjk"),
                "ss": Rot([SB(st, "css%d" % i, [128, 4], F32) for i in range(8)], "css"),
                "nb": Rot([SB(st, "cnb%d" % i, [128, D], BF16) for i in range(2)], "cnb"),
                "ps": Rot(PS[0:2], "psn"),
            }
            psr = Rot(PS[2:8], "psc")
            wgkeys = ["wg%d_%d" % (k, b3) for k in range(8) for b3 in range(3)]
            wgr = SB(st, "wgr", [128, 8, 36], F32)
            T.dma("sp", wgr[:], w_gr.rearrange("(k p) c -> p k c", p=128), writes=["wgr"])
            TC = 256
            NSC = TC // 128
            NBC = S // TC
            nTp = Rot([SB(st, "cnT%d" % i, [128, 8, TC], BF16) for i in range(1)], "cnT")
            yT = [Rot([SB(st, "yT%d_%d" % (b3, i), [128, 4, TC], BF16) for i in range(2)], "yT%d" % b3) for b3 in range(3)]
            ysrc = [ya_s.rearrange("(c p) t -> p c t", p=128), yb_s.rearrange("(c p) t -> p c t", p=128), yc_s.rearrange("(c p) t -> p c t", p=128)]
            gs = Rot([SB(st, "gs%d" % i, [128, TC], F32) for i in range(3)], "gs")
            tb = Rot([SB(st, "tb%d" % i, [128, TC], F32) for i in range(6)], "tb")
            mT = Rot([SB(st, "mT%d" % i, [128, 8, TC], BF16) for i in range(1)], "mT")
            hT = Rot([SB(st, "hT%d" % i, [128, D], F32) for i in range(2)], "hT")
            n2f = Rot([SB(st, "n2f%d" % i, [128, D], F32) for i in range(2)], "n2f")
            n2Tf = Rot([SB(st, "n2Tf%d" % i, [128, 8, 128], F32) for i in range(1)], "n2Tf")
            n2Tb = Rot([SB(st, "n2Tb%d" % i, [128, 8, TC], BF16) for i in range(1)], "n2Tb")
            n2T_v = n2T_s.rearrange("(k p) t -> p k t", p=128)

            def c_s1(i):
                st_ = {"i": i, "xts": [], "nbs": []}
                for sub in range(NSC):
                    r0 = i * TC + sub * 128
                    xt, xk = pools["xt"].next()
                    T.dma("sp", xt[:], x[r0:r0 + 128, :], writes=[xk])
                    jk, jkk = pools["junk"].next()
                    ss, sk = pools["ss"].next()
                    T.op("act", lambda e, jk=jk, xt=xt, ss=ss: e.activation(out=jk[:], in_=xt[:], func=AF.Square, accum_out=ss[:, 0:1]), reads=[xk], writes=[sk, jkk])
                    T.op("dve", lambda e, ss=ss: e.tensor_scalar(out=ss[:, 1:2], in0=ss[:, 0:1], scalar1=1.0 / D, scalar2=EPS, op0=ALU.mult, op1=ALU.add), reads=[sk], writes=[sk])
                    T.op("pool", lambda e, ss=ss: e.tensor_tensor(out=ss[:, 2:3], in0=ss[:, 1:2], in1=mh[:, 0:1], op=ALU.pow), reads=[sk, "mh"], writes=[sk])
                    nb, nk = pools["nb"].next()
                    T.op("dve", lambda e, nb=nb, xt=xt, ss=ss, gb=gbc["g_mix"]: e.scalar_tensor_tensor(out=nb[:], in0=xt[:], scalar=ss[:, 2:3], in1=gb[:], op0=ALU.mult, op1=ALU.mult),
                         reads=[xk, sk, "bc_g_mix"], writes=[nk])
                    st_["xts"].append((xt, xk))
                    st_["nbs"].append((nb, nk))
                ys = []
                for b3 in range(3):
                    y_, yk = yT[b3].next()
                    T.dma("sp", y_[:], ysrc[b3][:, :, i * TC:(i + 1) * TC], writes=[yk])
                    ys.append((y_, yk))
                st_["ys"] = ys
                return st_

            def c_s2(st_):
                nT, nTk = nTp.next()
                nTkeys = []
                for sub in range(NSC):
                    nb, nk = st_["nbs"][sub]
                    pt, pk = pools["ps"].next()
                    ptb = pt[:].bitcast(BF16)
                    T.group("pe", [(lambda e, ptb=ptb, nb=nb, k=k: e.transpose(out=ptb[:, k * 128:(k + 1) * 128], in_=nb[:, k * 128:(k + 1) * 128], identity=idb[:])) for k in range(8)],
                            reads=[nk, "idb"], writes=[pk])
                    dk = "%s_s%d" % (nTk, sub)
                    T.op("act", lambda e, ptb=ptb, nT=nT, sub=sub: e.activation(out=nT[:, :, sub * 128:(sub + 1) * 128], in_=ptb.rearrange("p (k t) -> p k t", k=8), func=AF.Copy),
                         reads=[pk], writes=[dk])
                    nTkeys.append(dk)
                ys = st_["ys"]
                m_, mk = mT.next()
                for c in range(8):
                    tbs = []
                    for b3 in range(3):
                        p, pk = psr.next()
                        mm(p[:, 0:TC], [(wg[:, k, b3 * 1024 + c * 128:b3 * 1024 + (c + 1) * 128], nT[:, k, :]) for k in range(8)], reads=wgkeys + nTkeys, writes=[pk])
                        g_, gk = gs.next()
                        T.op("act", lambda e, g_=g_, p=p: e.activation(out=g_[:], in_=p[:, 0:TC], func=AF.Sigmoid), reads=[pk], writes=[gk])
                        p2, p2k = psr.next()
                        mm(p2[:, 0:TC], [(wbr[:, b3, kc, c * 128:(c + 1) * 128], ys[b3][0][:, kc, :]) for kc in range(4)], reads=["wbr", ys[b3][1]], writes=[p2k])
                        t_, tk = tb.next()
                        T.op("dve", lambda e, t_=t_, p2=p2, g_=g_: e.tensor_tensor(out=t_[:], in0=p2[:, 0:TC], in1=g_[:], op=ALU.mult), reads=[p2k, gk], writes=[tk])
                        tbs.append((t_, tk))
                    T.op("pool", lambda e, a=tbs[0][0], b_=tbs[1][0]: e.tensor_tensor(out=a[:], in0=a[:], in1=b_[:], op=ALU.add), reads=[tbs[0][1], tbs[1][1]], writes=[tbs[0][1]])
                    T.op("pool", lambda e, a=tbs[0][0], b_=tbs[2][0], m_=m_, c=c: e.tensor_tensor(out=m_[:, c, :], in0=a[:], in1=b_[:], op=ALU.add),
                         reads=[tbs[0][1], tbs[2][1]], writes=["%s_c%d" % (mk, c)])
                st_["m"] = (m_, ["%s_c%d" % (mk, c) for c in range(8)])

            def c_s3(st_):
                i = st_["i"]
                m_, mkeys = st_["m"]
                st_["nfs"] = []
                for sub in range(NSC):
                    ti = i * NSC + sub
                    xt, xk = st_["xts"][sub]
                    h_, hk = hT.next()
                    for half in range(2):
                        p, pk = psr.next()
                        mm(p[:, :], [(m_[:, kc, sub * 128:(sub + 1) * 128], wo[:, kc, half * 512:(half + 1) * 512]) for kc in range(8)], reads=mkeys + ["wo"], writes=[pk])
                        T.op("dve", lambda e, h_=h_, p=p, xt=xt, half=half: e.tensor_tensor(out=h_[:, half * 512:(half + 1) * 512], in0=p[:, :], in1=xt[:, half * 512:(half + 1) * 512], op=ALU.add),
                             reads=[pk, xk], writes=[hk])
                    T.dma("sp", h_s[ti * 128:(ti + 1) * 128, :], h_[:], reads=[hk], writes=["h_s%d" % ti])
                    jk, jkk = pools["junk"].next()
                    ss, sk = pools["ss"].next()
                    T.op("act", lambda e, jk=jk, h_=h_, ss=ss: e.activation(out=jk[:], in_=h_[:], func=AF.Square, accum_out=ss[:, 0:1]), reads=[hk], writes=[sk, jkk])
                    T.op("dve", lambda e, ss=ss: e.tensor_scalar(out=ss[:, 1:2], in0=ss[:, 0:1], scalar1=1.0 / D, scalar2=EPS, op0=ALU.mult, op1=ALU.add), reads=[sk], writes=[sk])
                    T.op("pool", lambda e, ss=ss: e.tensor_tensor(out=ss[:, 2:3], in0=ss[:, 1:2], in1=mh[:, 0:1], op=ALU.pow), reads=[sk, "mh"], writes=[sk])
                    nf, nfk = n2f.next()
                    T.op("dve", lambda e, nf=nf, h_=h_, ss=ss: e.scalar_tensor_tensor(out=nf[:], in0=h_[:], scalar=ss[:, 2:3], in1=gbc["g_ffn"][:], op0=ALU.mult, op1=ALU.mult),
                         reads=[hk, sk, "bc_g_ffn"], writes=[nfk])
                    st_["nfs"].append((nf, nfk))

            def c_s4(st_):
                i = st_["i"]
                n2b, n2bk = n2Tb.next()
                for sub in range(NSC):
                    ti = i * NSC + sub
                    nf, nfk = st_["nfs"][sub]
                    ntf, ntfk = n2Tf.next()
                    for hh in range(2):
                        p, pk = psr.next()
                        T.group("pe", [(lambda e, p=p, nf=nf, hh=hh, k=k: e.transpose(out=p[:, k * 128:(k + 1) * 128], in_=nf[:, (hh * 4 + k) * 128:(hh * 4 + k + 1) * 128], identity=idf[:])) for k in range(4)],
                                reads=[nfk, "idf"], writes=[pk])
                        T.op("act", lambda e, p=p, ntf=ntf, hh=hh: e.activation(out=ntf[:, hh * 4:(hh + 1) * 4, :], in_=p[:, :].rearrange("p (k t) -> p k t", k=4), func=AF.Copy),
                             reads=[pk], writes=["%s_%d" % (ntfk, hh)])
                        T.op("dve", lambda e, p=p, n2b=n2b, hh=hh, sub=sub: e.tensor_copy(out=n2b[:, hh * 4:(hh + 1) * 4, sub * 128:(sub + 1) * 128], in_=p[:, :].rearrange("p (k t) -> p k t", k=4)),
                             reads=[pk], writes=["%s_%d_%d" % (n2bk, sub, hh)])
                    p, pk = psr.next()
                    mm(p[:, 0:36], [(ntf[:, k, :], wgr[:, k, :]) for k in range(8)], reads=["%s_0" % ntfk, "%s_1" % ntfk, "wgr"], writes=[pk])
                    T.op("dve", lambda e, p=p, ti=ti: e.tensor_tensor(out=lg[:, ti, :], in0=p[:, 0:36], in1=bgr_bc[:], op=ALU.add), reads=[pk, "bgr_bc"], writes=["lg%d" % ti])
                T.dma("sp", n2T_v[:, :, i * TC:(i + 1) * TC], n2b[:], reads=["%s_%d_%d" % (n2bk, s_, hh) for s_ in range(NSC) for hh in range(2)], writes=["n2T_s%d" % i])

            cur_c = c_s1(0)
            prev_c = None
            for t in range(NBC):
                c_s2(cur_c)
                nxt_c = c_s1(t + 1) if t + 1 < NBC else None
                if prev_c is not None:
                    c_s4(prev_c)
                c_s3(cur_c)
                prev_c = cur_c
                cur_c = nxt_c
            c_s4(prev_c)
            T.barrier()
            if stop == 4:
                T.finish_early()

        stCW.close()
        with ExitStack() as st:
            lgk = ["lg%d" % t for t in range(NT)]
            R = lambda nm, shp: SB(st, nm, shp, F32)
            gm = R("r_gm", [128, NT]); ge = R("r_ge", [128, NT, 4]); gsum = R("r_gsum", [128, NT]); gw = R("r_gw", [128, NT])
            mg = R("r_mg", [128, NT, 4]); eg = R("r_eg", [128, NT, 8]); tmp8 = R("r_tmp8", [128, NT, 8])
            m1 = R("r_m1", [128, NT]); m2 = R("r_m2", [128, NT]); sel = R("r_sel", [128, NT, 8]); pe_ = R("r_pe", [128, NT, 8]); psum_ = R("r_ps", [128, NT])
            glv = lg[:, :, 0:4]
            elv = lg[:, :, 4:36].rearrange("p t (g e) -> p t g e", g=4)

            def bc(ap2, n):
                return ap2.unsqueeze(2).to_broadcast([128, NT, n])
            T.op("dve", lambda e: e.tensor_reduce(out=gm[:], in_=glv, axis=AX.X, op=ALU.max), reads=lgk, writes=["gm"])
            T.op("dve", lambda e: e.tensor_tensor(out=ge[:], in0=glv, in1=bc(gm[:], 4), op=ALU.subtract), reads=lgk + ["gm"], writes=["ge"])
            T.op("dve", lambda e: e.tensor_tensor(out=mg[:], in0=glv, in1=bc(gm[:], 4), op=ALU.is_equal), reads=lgk + ["gm"], writes=["mg"])
            T.op("act", lambda e: e.activation(out=ge[:], in_=ge[:], func=AF.Exp), reads=["ge"], writes=["ge"])
            T.op("dve", lambda e: e.tensor_reduce(out=gsum[:], in_=ge[:], axis=AX.X, op=ALU.add), reads=["ge"], writes=["gsum"])
            T.op("dve", lambda e: e.reciprocal(out=gw[:], in_=gsum[:]), reads=["gsum"], writes=["gw"])
            T.op("dve", lambda e: e.tensor_tensor(out=eg[:], in0=elv[:, :, 0, :], in1=bc(mg[:, :, 0], 8), op=ALU.mult), reads=lgk + ["mg"], writes=["eg"])
            for g in range(1, 4):
                T.op("dve", lambda e, g=g: e.tensor_tensor(out=tmp8[:], in0=elv[:, :, g, :], in1=bc(mg[:, :, g], 8), op=ALU.mult), reads=lgk + ["mg"], writes=["tmp8"])
                T.op("dve", lambda e: e.tensor_tensor(out=eg[:], in0=eg[:], in1=tmp8[:], op=ALU.add), reads=["eg", "tmp8"], writes=["eg"])
            T.op("dve", lambda e: e.tensor_reduce(out=m1[:], in_=eg[:], axis=AX.X, op=ALU.max), reads=["eg"], writes=["m1"])
            T.op("dve", lambda e: e.tensor_tensor(out=tmp8[:], in0=eg[:], in1=bc(m1[:], 8), op=ALU.is_equal), reads=["eg", "m1"], writes=["tmp8"])
            T.op("dve", lambda e: e.scalar_tensor_tensor(out=tmp8[:], in0=tmp8[:], scalar=-1.0e30, in1=eg[:], op0=ALU.mult, op1=ALU.add), reads=["tmp8", "eg"], writes=["tmp8"])
            T.op("dve", lambda e: e.tensor_reduce(out=m2[:], in_=tmp8[:], axis=AX.X, op=ALU.max), reads=["tmp8"], writes=["m2"])
            T.op("dve", lambda e: e.tensor_tensor(out=sel[:], in0=eg[:], in1=bc(m2[:], 8), op=ALU.is_ge), reads=["eg", "m2"], writes=["sel"])
            T.op("dve", lambda e: e.tensor_tensor(out=pe_[:], in0=eg[:], in1=bc(m1[:], 8), op=ALU.subtract), reads=["eg", "m1"], writes=["pe_"])
            T.op("act", lambda e: e.activation(out=pe_[:], in_=pe_[:], func=AF.Exp), reads=["pe_"], writes=["pe_"])
            T.op("dve", lambda e: e.tensor_tensor(out=pe_[:], in0=pe_[:], in1=sel[:], op=ALU.mult), reads=["pe_", "sel"], writes=["pe_"])
            T.op("dve", lambda e: e.tensor_reduce(out=psum_[:], in_=pe_[:], axis=AX.X, op=ALU.add), reads=["pe_"], writes=["psum_"])
            T.op("dve", lambda e: e.reciprocal(out=psum_[:], in_=psum_[:]), reads=["psum_"], writes=["psum_"])
            T.op("dve", lambda e: e.tensor_tensor(out=psum_[:], in0=psum_[:], in1=gw[:], op=ALU.mult), reads=["psum_", "gw"], writes=["psum_"])
            T.op("dve", lambda e: e.tensor_tensor(out=pe_[:], in0=pe_[:], in1=bc(psum_[:], 8), op=ALU.mult), reads=["pe_", "psum_"], writes=["pe_"])
            cv = comb[:].rearrange("p t (g e) -> p t g e", g=4)
            for g in range(4):
                T.op("dve", lambda e, g=g: e.tensor_tensor(out=cv[:, :, g, :], in0=pe_[:], in1=bc(mg[:, :, g], 8), op=ALU.mult), reads=["pe_", "mg"], writes=["comb"])
            if dbg:
                T.dma("sp", comb_s, comb[:], reads=["comb"], writes=["comb_s"])
            T.barrier()
            if stop == 5:
                T.finish_early()

        with ExitStack() as st:
            load_g(st, "g_fin", "d")
            TH = S // NHALF
            NS = TH // 128
            NBH = TH // 512
            n2T = SB(st, "n2T", [128, 8, TH], BF16)
            acc = SB(st, "acc", [128, NS, D], F32)
            stg = [SB(st, "stg%d" % i, [128, 8 * 256], F32) for i in range(3)]
            wgb = [SB(st, "wgb%d" % i, [128, 8, 256], BF16) for i in range(2)]
            wub = [SB(st, "wub%d" % i, [128, 8, 256], BF16) for i in range(2)]
            wdb = [SB(st, "wdb%d" % i, [128, 2, D], BF16) for i in range(2)]
            sg = Rot([SB(st, "sg%d" % i, [128, 512], F32) for i in range(2)], "sg")
            hid = Rot([SB(st, "hid%d" % i, [128, 2, 512], BF16) for i in range(2)], "hid")
            ot = Rot([SB(st, "ot%d" % i, [128, D], F32) for i in range(1)], "ot")
            jkp = Rot([SB(st, "djk", [128, D], BF16)], "djk")
            ssp = Rot([SB(st, "dss%d" % i, [128, 4], F32) for i in range(4)], "dss")
            psr = Rot(PS[0:8], "psd")
            n2T_v = n2T_s.rearrange("(k p) t -> p k t", p=128)
            for hf in range(NHALF):
                t0 = hf * TH
                for k in range(8):
                    T.dma("sp", n2T[:, k, :], n2T_v[:, k, t0:t0 + TH], writes=["n2T_k%d" % k])
                n2keys = ["n2T_k%d" % k for k in range(8)]
                def d_weights(ex):
                    b = ex % 2
                    T.dma("sp", stg[0][:].rearrange("p (k c) -> p k c", k=8), w_eg[ex].rearrange("(k p) c -> p k c", p=128), writes=["stg0"])
                    T.op("pool", lambda e, b=b: e.tensor_copy(out=wgb[b][:], in_=stg[0][:].rearrange("p (k c) -> p k c", k=8)), reads=["stg0"], writes=["wgb%d" % b])
                    T.dma("sp", stg[1][:].rearrange("p (k c) -> p k c", k=8), w_eu[ex].rearrange("(k p) c -> p k c", p=128), writes=["stg1"])
                    T.op("pool", lambda e, b=b: e.tensor_copy(out=wub[b][:], in_=stg[1][:].rearrange("p (k c) -> p k c", k=8)), reads=["stg1"], writes=["wub%d" % b])
                    T.dma("sp", stg[2][:].rearrange("p (k c) -> p k c", k=2), w_ed[ex].rearrange("(k p) c -> p k c", p=128), writes=["stg2"])
                    T.op("pool", lambda e, b=b: e.tensor_copy(out=wdb[b][:], in_=stg[2][:].rearrange("p (k c) -> p k c", k=2)), reads=["stg2"], writes=["wdb%d" % b])
                    if ex == 0:
                        for s_ in range(NS):
                            T.dma("sp", acc[:, s_, :], h_s[t0 + s_ * 128:t0 + (s_ + 1) * 128, :], writes=["acc%d_0" % s_, "acc%d_1" % s_])

                def d_up(ex, t):
                    b = ex % 2
                    hd, hdk = hid.next()
                    for oc in range(2):
                        pg, pgk = psr.next()
                        mm(pg[:, :], [(wgb[b][:, k, oc * 128:(oc + 1) * 128], n2T[:, k, t * 512:(t + 1) * 512]) for k in range(8)], reads=["wgb%d" % b] + n2keys, writes=[pgk])
                        pu, puk = psr.next()
                        mm(pu[:, :], [(wub[b][:, k, oc * 128:(oc + 1) * 128], n2T[:, k, t * 512:(t + 1) * 512]) for k in range(8)], reads=["wub%d" % b] + n2keys, writes=[puk])
                        s1, s1k = sg.next()
                        T.op("act", lambda e, s1=s1, pg=pg: e.activation(out=s1[:], in_=pg[:, :], func=AF.Silu), reads=[pgk], writes=[s1k])
                        T.op("dve", lambda e, s1=s1, pu=pu, hd=hd, oc=oc: e.tensor_tensor(out=hd[:, oc, :], in0=pu[:, :], in1=s1[:], op=ALU.mult), reads=[puk, s1k], writes=["%s_%d" % (hdk, oc)])
                    return hd, hdk

                def d_down(ex, t, hd, hdk):
                    b = ex % 2
                    for sub in range(4):
                        s_ = t * 4 + sub
                        ti = hf * NS + s_
                        for half in range(2):
                            pd, pdk = psr.next()
                            mm(pd[:, :], [(hd[:, jc, sub * 128:(sub + 1) * 128], wdb[b][:, jc, half * 512:(half + 1) * 512]) for jc in range(2)],
                               reads=["%s_0" % hdk, "%s_1" % hdk, "wdb%d" % b], writes=[pdk])
                            ak = "acc%d_%d" % (s_, half)
                            T.op("dve", lambda e, pd=pd, s_=s_, half=half, ti=ti, ex=ex: e.scalar_tensor_tensor(
                                out=acc[:, s_, half * 512:(half + 1) * 512], in0=pd[:, :], scalar=comb[:, ti, ex:ex + 1],
                                in1=acc[:, s_, half * 512:(half + 1) * 512], op0=ALU.mult, op1=ALU.add), reads=[pdk, "comb", ak], writes=[ak])

                items_d = [(ex, t) for ex in range(NE) for t in range(NBH)]
                d_weights(0)
                nxt_d = d_up(*items_d[0])
                for n, (ex, t) in enumerate(items_d):
                    cur_d = nxt_d
                    if n + 1 < len(items_d):
                        ex2, t2 = items_d[n + 1]
                        if t2 == 0:
                            d_weights(ex2)
                        nxt_d = d_up(ex2, t2)
                    d_down(ex, t, *cur_d)
                for s_ in range(NS):
                    aks = ["acc%d_0" % s_, "acc%d_1" % s_]
                    jk, jkk = jkp.next()
                    ss, sk = ssp.next()
                    T.op("act", lambda e, jk=jk, s_=s_, ss=ss: e.activation(out=jk[:], in_=acc[:, s_, :], func=AF.Square, accum_out=ss[:, 0:1]), reads=aks, writes=[sk, jkk])
                    T.op("dve", lambda e, ss=ss: e.tensor_scalar(out=ss[:, 1:2], in0=ss[:, 0:1], scalar1=1.0 / D, scalar2=EPS, op0=ALU.mult, op1=ALU.add), reads=[sk], writes=[sk])
                    T.op("pool", lambda e, ss=ss: e.tensor_tensor(out=ss[:, 2:3], in0=ss[:, 1:2], in1=mh[:, 0:1], op=ALU.pow), reads=[sk, "mh"], writes=[sk])
                    o_, ok = ot.next()
                    T.op("dve", lambda e, o_=o_, s_=s_, ss=ss: e.scalar_tensor_tensor(out=o_[:], in0=acc[:, s_, :], scalar=ss[:, 2:3], in1=gbc["g_fin"][:], op0=ALU.mult, op1=ALU.mult),
                         reads=aks + [sk, "bc_g_fin"], writes=[ok])
                    T.dma("sp", out[t0 + s_ * 128:t0 + (s_ + 1) * 128, :], o_[:], reads=[ok], writes=["out%d" % (t0 // 128 + s_)])

        with nc.Block() as block:
            T.finish(block)
    return nc


def _host_inputs(inputs):
    f = lambda a: np.ascontiguousarray(np.asarray(a))
    x = f(inputs["x"]); mem = f(inputs["mem"]); positions = f(inputs["positions"])
    B = x.shape[0]
    shared = {
        "g_mix": f(inputs["g_mix"])[0], "g_mem": f(inputs["g_mem"])[0], "g_ffn": f(inputs["g_ffn"])[0], "g_fin": f(inputs["g_final"]),
        "g_q": f(inputs["g_q"])[0], "g_kv": f(inputs["g_kv"])[0],
        "w_in": f(inputs["w_in"])[0],
        "cw": f(f(inputs["conv_w"])[0].T.reshape(4, 128, 4).transpose(1, 0, 2)),
        "cb": f(f(inputs["conv_b"])[0].reshape(4, 128).T),
        "lba": f(f(inputs["lru_ba"])[0].reshape(4, 128).T),
        "lbx": f(f(inputs["lru_bx"])[0].reshape(4, 128).T),
        "llam": f(f(inputs["lru_lambda"])[0].reshape(4, 128).T),
        "lwa": f(inputs["lru_wa"])[0], "lwx": f(inputs["lru_wx"])[0],
        "w_uq": f(inputs["w_uq"])[0], "w_ukv": f(inputs["w_ukv"])[0], "w_mkv": f(inputs["w_mem_kv"])[0],
        "w_br": f(inputs["w_branch"])[0], "w_o": f(inputs["w_o"])[0],
        "w_gr": f(np.concatenate([f(inputs["w_group"])[0], f(inputs["w_router"])[0]], axis=1)),
        "b_gr": f(np.concatenate([f(inputs["b_group"])[0], f(inputs["b_router"])[0]], axis=0)),
        "w_eg": f(inputs["w_e_gate"])[0], "w_eu": f(inputs["w_e_up"])[0], "w_ed": f(inputs["w_e_down"])[0],
    }
    ident = np.eye(128, dtype=np.float32)
    kk = np.arange(128)[:, None]; qq = np.arange(512)[None, :]
    cmask = np.stack([((128 * r + kk) <= qq).astype(np.float32) for r in range(4)], axis=0)
    invf = np.broadcast_to((10000.0 ** (-np.arange(0, 32, 2, dtype=np.float32) / 32.0)).astype(np.float32)[None, :], (128, 16)).copy()
    shared.update({"ident": ident, "cmask": cmask, "invf": invf})
    shared = {k: np.ascontiguousarray(v, dtype=np.float32) for k, v in shared.items()}
    maps = []
    for b in range(B):
        m = dict(shared)
        m["x"] = x[b]
        m["mem"] = mem[b]
        m["pos"] = np.ascontiguousarray(positions[b].reshape(NT, 128).T.astype(np.int32))
        maps.append(m)
    return maps


_NC_CACHE = {}


def kernel(**inputs):
    maps = _host_inputs(inputs)
    if "nc" not in _NC_CACHE:
        _NC_CACHE["nc"] = build_nc(False)
    nc = _NC_CACHE["nc"]
    res = run_bass_kernel_spmd(nc, maps, core_ids=list(range(len(maps))))
    return np.stack([np.asarray(r["out"], dtype=np.float32) for r in res.results], axis=0)
```

```python
import math
import numpy as np
from contextlib import ExitStack
import concourse.bass as bass
import concourse.mybir as mybir
from concourse.bass_utils import run_bass_kernel_spmd

F32 = mybir.dt.float32
BF16 = mybir.dt.bfloat16
I32 = mybir.dt.int32
AF = mybir.ActivationFunctionType
ALU = mybir.AluOpType
AX = mybir.AxisListType

S = 4096
D = 1024
NT = S // 128
NB = S // 512
EPS = 1e-6
DIN = 5024
NE = 32
NHALF = 2


class Trk:
    ENG = ("pe", "act", "dve", "pool", "sp")
    SELF = True
    STEP = 0
    FINAL = None

    def __init__(self, nc, stack, n_dma_sems=12):
        self.nc = nc
        self.prog = {e: [] for e in self.ENG}
        self.sem = {e: stack.enter_context(nc.semaphore("sem_" + e)) for e in self.ENG}
        self.nops = {e: 0 for e in self.ENG}
        self.waited = {e: set() for e in self.ENG}
        self.known_c = {e: {e2: 0 for e2 in self.ENG} for e in self.ENG}
        self.known_d = {e: {} for e in self.ENG}
        self.dsem = {}
        self.drr = {}
        self.dcnt = {}
        self.dobj = {}
        for q in ("sp", "pool", "act"):
            self.dsem[q] = [stack.enter_context(nc.semaphore("dsem_%s_%d" % (q, i))) for i in range(n_dma_sems)]
            self.drr[q] = 0
            for s in self.dsem[q]:
                self.dcnt[id(s)] = 0
                self.dobj[id(s)] = s
        self.tiles = {}
        self.mute = False

    def _st(self, k):
        st = self.tiles.get(k)
        if st is None:
            st = {"w": None, "r": []}
            self.tiles[k] = st
        return st

    def _deps(self, reads, writes):
        evs = []
        for k in reads:
            st = self._st(k)
            if st["w"] is not None:
                evs.append(st["w"])
            if k.startswith("ps"):
                evs.extend(st["r"])
        for k in writes:
            st = self._st(k)
            if st["w"] is not None:
                evs.append(st["w"])
            evs.extend(st["r"])
        return evs

    def _emit_waits(self, e, evs, is_dma=False):
        cmax = {}
        dmax = {}
        for ev in evs:
            if ev[0] == "c":
                _, e2, idx = ev
                if e2 == e and (e == "pe" or (not is_dma and not self.SELF)):
                    continue
                if cmax.get(e2, 0) < idx:
                    cmax[e2] = idx
            else:
                _, sid, v = ev
                if dmax.get(sid, 0) < v:
                    dmax[sid] = v
        for e2, idx in cmax.items():
            if self.known_c[e][e2] >= idx:
                continue
            k0 = self.known_c[e][e2]
            if self.STEP and e2 != e:
                for v in range(k0 + self.STEP, idx, self.STEP):
                    self.waited[e2].add(v)
                    self.prog[e].append(("wc", e2, v))
            self.known_c[e][e2] = idx
            self.waited[e2].add(idx)
            self.prog[e].append(("wc", e2, idx))
        for sid, v in dmax.items():
            if self.known_d[e].get(sid, 0) >= v:
                continue
            self.known_d[e][sid] = v
            self.prog[e].append(("wd", sid, v))

    def _mark(self, ev, reads, writes):
        for k in reads:
            r = self._st(k)["r"]
            r.append(ev)
            if len(r) > 24:
                best = {}
                for x in r:
                    key = (x[0], x[1])
                    if key not in best or best[key][2] < x[2]:
                        best[key] = x
                r[:] = list(best.values())
        for k in writes:
            st = self._st(k)
            st["w"] = ev
            st["r"] = []

    def op(self, e, fn, reads=(), writes=()):
        return self.group(e, [fn], reads, writes)

    def group(self, e, fns, reads=(), writes=()):
        if self.mute:
            return None
        self._emit_waits(e, self._deps(reads, writes))
        self.nops[e] += 1
        idx = self.nops[e]
        self.prog[e].append(("op", list(fns), idx))
        ev = ("c", e, idx)
        self._mark(ev, reads, writes)
        return ev

    def dma(self, q, out, in_, reads=(), writes=(), **kw):
        if self.mute:
            return None
        pool = self.dsem[q]
        s = pool[self.drr[q] % len(pool)]
        self.drr[q] += 1
        sid = id(s)
        evs = self._deps(reads, writes)
        if self.dcnt[sid] > 0:
            evs.append(("d", sid, self.dcnt[sid]))
        self._emit_waits(q, evs, is_dma=True)
        self.dcnt[sid] += 16
        ev = ("d", sid, self.dcnt[sid])
        self.prog[q].append(("dma", s, out, in_, kw))
        self._mark(ev, reads, writes)
        return ev

    def barrier(self, engines=None):
        if self.mute:
            return
        evs = [("c", e2, self.nops[e2]) for e2 in self.ENG if self.nops[e2] > 0]
        evs += [("d", sid, v) for sid, v in self.dcnt.items() if v > 0]
        for e in (engines or self.ENG):
            self._emit_waits(e, evs)
        self.tiles = {}

    def finish_early(self):
        self.barrier(self.FINAL)
        self.mute = True

    def finish(self, block):
        self.mute = False
        self.barrier(self.FINAL)
        rank = {e: {idx: i + 1 for i, idx in enumerate(sorted(self.waited[e]))} for e in self.ENG}
        t = self

        def run(e, en):
            for it in t.prog[e]:
                if it[0] == "wc":
                    en.wait_ge(t.sem[it[1]], rank[it[1]][it[2]])
                elif it[0] == "wd":
                    en.wait_ge(t.dobj[it[1]], it[2])
                elif it[0] == "dma":
                    en.dma_start(out=it[2], in_=it[3], **it[4]).then_inc(it[1], 16)
                else:
                    fns, idx = it[1], it[2]
                    for i, fn in enumerate(fns):
                        r = fn(en)
                        if i == len(fns) - 1 and idx in rank[e]:
                            r.then_inc(t.sem[e], 1)

        @block.sync
        def _(en):
            run("sp", en)

        @block.tensor
        def _(en):
            run("pe", en)

        @block.scalar
        def _(en):
            run("act", en)

        @block.vector
        def _(en):
            run("dve", en)

        @block.gpsimd
        def _(en):
            run("pool", en)


class Rot:
    def __init__(self, bufs, name, shared=False):
        self.bufs = bufs
        self.name = name
        self.i = 0
        self.shared = shared

    def next(self):
        j = self.i % len(self.bufs)
        self.i += 1
        return self.bufs[j], "%s#%d" % (self.name, 0 if self.shared else j)


def build_nc(dbg=False, stop=99, substop=99):
    EXP = ""
    nc = bass.Bass("TRN2", target_bir_lowering=False)

    def din(name, shape, dt=F32):
        return nc.dram_tensor(name, list(shape), dt, kind="ExternalInput").ap()

    def dscr(name, shape, dt):
        return nc.dram_tensor(name, list(shape), dt, kind=("ExternalOutput" if dbg else "Internal")).ap()

    x = din("x", [S, D])
    mem = din("mem", [256, D])
    pos = din("pos", [128, NT], I32)
    g_mix = din("g_mix", [D]); g_mem = din("g_mem", [D]); g_ffn = din("g_ffn", [D]); g_fin = din("g_fin", [D])
    g_q = din("g_q", [256]); g_kv = din("g_kv", [128])
    w_in = din("w_in", [D, DIN])
    cw = din("cw", [128, 4, 4]); cb = din("cb", [128, 4]); lba = din("lba", [128, 4]); lbx = din("lbx", [128, 4]); llam = din("llam", [128, 4])
    lwa = din("lwa", [8, 64, 64]); lwx = din("lwx", [8, 64, 64])
    w_uq = din("w_uq", [256, 768]); w_ukv = din("w_ukv", [128, 1024]); w_mkv = din("w_mkv", [D, D])
    w_br = din("w_br", [3, 512, D]); w_o = din("w_o", [D, D])
    w_gr = din("w_gr", [D, 36]); b_gr = din("b_gr", [36])
    w_eg = din("w_eg", [NE, D, 256]); w_eu = din("w_eu", [NE, D, 256]); w_ed = din("w_ed", [NE, 256, D])
    ident = din("ident", [128, 128]); cmask = din("cmask", [4, 128, 512]); invf = din("invf", [128, 16])
    out = nc.dram_tensor("out", [S, D], F32, kind="ExternalOutput").ap()

    ya_s = dscr("ya_s", [512, S], BF16); yb_s = dscr("yb_s", [512, S], BF16); yc_s = dscr("yc_s", [512, S], BF16)
    h_s = dscr("h_s", [S, D], F32); n2T_s = dscr("n2T_s", [D, S], BF16)
    comb_s = dscr("comb_s", [128, NT, NE], F32) if dbg else None

    with ExitStack() as top:
        T = Trk(nc, top)

        def SB(st, name, shape, dt):
            return st.enter_context(nc.sbuf_tensor(name, list(shape), dt))

        PS = [top.enter_context(nc.psum_tensor("ps%d" % i, [128, 512], F32)) for i in range(8)]

        def mm(out_ap, pairs, reads, writes):
            n = len(pairs)
            fns = []
            for i, (l, r) in enumerate(pairs):
                fns.append(lambda e, l=l, r=r, i=i: e.matmul(out_ap, lhsT=l, rhs=r, start=(i == 0), stop=(i == n - 1)))
            return T.group("pe", fns, reads, writes)

        idf = SB(top, "idf", [128, 128], F32)
        idb = SB(top, "idb", [128, 128], BF16)
        onesb = SB(top, "onesb", [128, 128], BF16)
        mh = SB(top, "mh", [128, 2], F32)
        T.dma("sp", idf[:], ident, writes=["idf"])
        T.dma("pool", idb[:], ident, writes=["idb"])
        T.op("dve", lambda e: e.memset(onesb[:], 1.0), writes=["onesb"])
        T.op("dve", lambda e: e.memset(mh[:], -0.5), writes=["mh"])
        gbc = {}
        gsrc = {"g_mix": g_mix, "g_mem": g_mem, "g_ffn": g_ffn, "g_fin": g_fin}

        def load_g(st_, nm, tag):
            t_ = SB(st_, "bc_%s_%s" % (nm, tag), [128, D], F32)
            T.dma("sp", t_[:], gsrc[nm].partition_broadcast(128), writes=["bc_" + nm])
            gbc[nm] = t_
        gq_bc = SB(top, "gq_bc", [128, 256], F32); T.dma("sp", gq_bc[:], g_q.partition_broadcast(128), writes=["gq_bc"])
        gkv_bc = SB(top, "gkv_bc", [128, 128], F32); T.dma("sp", gkv_bc[:], g_kv.partition_broadcast(128), writes=["gkv_bc"])
        bgr_bc = SB(top, "bgr_bc", [128, 36], F32); T.dma("sp", bgr_bc[:], b_gr.partition_broadcast(128), writes=["bgr_bc"])
        stAB = ExitStack()
        cosT = SB(stAB, "cosT", [128, NT, 16], F32)
        sinT = SB(stAB, "sinT", [128, NT, 16], F32)
        cqnT = SB(stAB, "cqnT", [128, 2, S], BF16)
        ckvnT = SB(stAB, "ckvnT", [128, S], BF16)
        kropeT = SB(stAB, "kropeT", [96, S], BF16)

        with ExitStack() as st:
            posi = SB(st, "posi", [128, NT], I32)
            posf = SB(st, "posf", [128, NT], F32)
            ivf = SB(st, "ivf", [128, 16], F32)
            ang = SB(st, "ang", [128, NT, 16], F32)
            kf = SB(st, "kf", [128, NT, 16], F32)
            ki = SB(st, "ki", [128, NT, 16], I32)
            T.dma("sp", posi[:], pos, writes=["posi"])
            T.dma("sp", ivf[:], invf, writes=["ivf"])
            T.op("dve", lambda e: e.tensor_copy(out=posf[:], in_=posi[:]), reads=["posi"], writes=["posf"])
            T.op("dve", lambda e: e.tensor_tensor(out=ang[:], in0=posf[:].unsqueeze(2).to_broadcast([128, NT, 16]),
                                                  in1=ivf[:].unsqueeze(1).to_broadcast([128, NT, 16]), op=ALU.mult),
                 reads=["posf", "ivf"], writes=["ang"])
            TWO_PI = 2.0 * math.pi
            for shift, dst, nm in ((0.0, sinT, "sinT"), (math.pi / 2.0, cosT, "cosT")):
                T.op("dve", lambda e, shift=shift: e.tensor_scalar(out=kf[:], in0=ang[:], scalar1=shift, scalar2=1.0 / TWO_PI, op0=ALU.add, op1=ALU.mult),
                     reads=["ang"], writes=["kf"])
                T.op("dve", lambda e: e.tensor_copy(out=ki[:], in_=kf[:]), reads=["kf"], writes=["ki"])
                T.op("dve", lambda e: e.tensor_copy(out=kf[:], in_=ki[:]), reads=["ki"], writes=["kf"])
                T.op("dve", lambda e, shift=shift: e.tensor_scalar(out=kf[:], in0=kf[:], scalar1=-TWO_PI, scalar2=shift, op0=ALU.mult, op1=ALU.add),
                     reads=["kf"], writes=["kf"])
                T.op("dve", lambda e: e.tensor_tensor(out=kf[:], in0=kf[:], in1=ang[:], op=ALU.add), reads=["kf", "ang"], writes=["kf"])
                T.op("dve", lambda e: e.tensor_scalar(out=kf[:], in0=kf[:], scalar1=math.pi, scalar2=-math.pi, op0=ALU.min, op1=ALU.max),
                     reads=["kf"], writes=["kf"])
                T.op("act", lambda e, dst=dst: e.activation(out=dst[:], in_=kf[:], func=AF.Sin), reads=["kf"], writes=[nm])
            T.barrier()
            if stop == 0:
                T.finish_early()

        def norm_T(st_pools, src_rows, gb, gkey, dstT, dst_cols, dkey, keep_x=None):
            xt, xk = st_pools["xt"].next() if keep_x is None else keep_x
            T.dma("sp", xt[:], src_rows, writes=[xk])
            jk, jkk = st_pools["junk"].next()
            ss, sk = st_pools["ss"].next()
            T.op("act", lambda e: e.activation(out=jk[:], in_=xt[:], func=AF.Square, accum_out=ss[:, 0:1]), reads=[xk], writes=[sk, jkk])
            T.op("dve", lambda e: e.tensor_scalar(out=ss[:, 1:2], in0=ss[:, 0:1], scalar1=1.0 / D, scalar2=EPS, op0=ALU.mult, op1=ALU.add), reads=[sk], writes=[sk])
            T.op("pool", lambda e: e.tensor_tensor(out=ss[:, 2:3], in0=ss[:, 1:2], in1=mh[:, 0:1], op=ALU.pow), reads=[sk, "mh"], writes=[sk])
            nb, nk = st_pools["nb"].next()
            T.op("dve", lambda e: e.scalar_tensor_tensor(out=nb[:], in0=xt[:], scalar=ss[:, 2:3], in1=gb[:], op0=ALU.mult, op1=ALU.mult),
                 reads=[xk, sk, gkey], writes=[nk])
            pt, pk = st_pools["ps"].next()
            ptb = pt[:].bitcast(BF16)
            T.group("pe", [(lambda e, k=k: e.transpose(out=ptb[:, k * 128:(k + 1) * 128], in_=nb[:, k * 128:(k + 1) * 128], identity=idb[:])) for k in range(8)],
                    reads=[nk, "idb"], writes=[pk])
            T.op("act", lambda e: e.activation(out=dstT[:, :, dst_cols], in_=ptb.rearrange("p (k t) -> p k t", k=8), func=AF.Copy),
                 reads=[pk], writes=[dkey])
            return xt, xk

        kmemT = SB(stAB, "kmemT", [128, 4, 256], BF16)
        vmem = SB(stAB, "vmem", [128, 2, 512], BF16)
        with ExitStack() as st:
            load_g(st, "g_mem", "a0")
            pools = {
                "xt": Rot([SB(st, "a0xt%d" % i, [128, D], F32) for i in range(2)], "a0xt"),
                "junk": Rot([SB(st, "a0jk", [128, D], BF16)], "a0jk"),
                "ss": Rot([SB(st, "a0ss%d" % i, [128, 4], F32) for i in range(2)], "a0ss"),
                "nb": Rot([SB(st, "a0nb%d" % i, [128, D], BF16) for i in range(2)], "a0nb"),
                "ps": Rot(PS[0:4], "ps"),
            }
            psr = Rot(PS[4:8], "psb")
            memT = SB(st, "memT", [128, 8, 256], BF16)
            wmk = SB(st, "wmk", [128, 8, D], BF16)
            for k in range(8):
                T.dma("pool", wmk[:, k, :], w_mkv[k * 128:(k + 1) * 128, :], writes=["wmk%d" % k])
            for mt in range(2):
                norm_T(pools, mem[mt * 128:(mt + 1) * 128, :], gbc["g_mem"], "bc_g_mem", memT, slice(mt * 128, (mt + 1) * 128), "memT%d" % mt)
            wk = ["wmk%d" % k for k in range(8)]
            for h in range(4):
                p, pk = psr.next()
                mm(p[:, 0:256], [(wmk[:, k, h * 128:(h + 1) * 128], memT[:, k, :]) for k in range(8)], reads=wk + ["memT0", "memT1"], writes=[pk])
                T.op("act", lambda e, p=p, h=h: e.activation(out=kmemT[:, h, :], in_=p[:, 0:256], func=AF.Copy), reads=[pk], writes=["kmemT"])
            for mt in range(2):
                p, pk = psr.next()
                mm(p[:, :], [(memT[:, k, mt * 128:(mt + 1) * 128], wmk[:, k, 512:1024]) for k in range(8)], reads=wk + ["memT%d" % mt], writes=[pk])
                T.op("act", lambda e, p=p, mt=mt: e.activation(out=vmem[:, mt, :], in_=p[:, :], func=AF.Copy), reads=[pk], writes=["vmem"])
            T.barrier()
            if stop == 1:
                T.finish_early()

        with ExitStack() as st:
            load_g(st, "g_mix", "a")
            pools = {
                "xt": Rot([SB(st, "axt%d" % i, [128, D], F32) for i in range(4)], "axt"),
                "junk": Rot([SB(st, "ajk", [128, D], BF16)], "ajk"),
                "ss": Rot([SB(st, "ass%d" % i, [128, 4], F32) for i in range(8)], "ass"),
                "nb": Rot([SB(st, "anb%d" % i, [128, D], BF16) for i in range(4)], "anb"),
                "ps": Rot(PS[0:2], "psn"),
            }
            psr = Rot(PS[2:8], "psa")
            NA = 1952
            win = SB(st, "win", [128, 8, NA], BF16)
            for k in range(8):
                for (c0, c1) in ((0, 1024), (1024, NA)):
                    T.dma("pool", win[:, k, c0:c1], w_in[k * 128:(k + 1) * 128, c0:c1], writes=["win%d_%d" % (k, c0)])
            winkeys = ["win%d_%d" % (k, c0) for k in range(8) for c0 in (0, 1024)]
            wabd = SB(st, "wabd", [128, 4, 128], BF16)
            wxbd = SB(st, "wxbd", [128, 4, 128], BF16)
            T.op("dve", lambda e: e.memset(wabd[:], 0.0), writes=["wabd"])
            T.op("dve", lambda e: e.memset(wxbd[:], 0.0), writes=["wxbd"])
            for c in range(4):
                for j in range(2):
                    T.dma("pool", wabd[j * 64:(j + 1) * 64, c, j * 64:(j + 1) * 64], lwa[2 * c + j], reads=["wabd"], writes=["wabd"])
                    T.dma("pool", wxbd[j * 64:(j + 1) * 64, c, j * 64:(j + 1) * 64], lwx[2 * c + j], reads=["wxbd"], writes=["wxbd"])
            cws = SB(st, "cws", [128, 4, 4], F32); T.dma("sp", cws[:], cw, writes=["cws"])
            cbs = SB(st, "cbs", [128, 4], F32); T.dma("sp", cbs[:], cb, writes=["cbs"])
            bas = SB(st, "bas", [128, 4], F32); T.dma("sp", bas[:], lba, writes=["bas"])
            bxs = SB(st, "bxs", [128, 4], F32); T.dma("sp", bxs[:], lbx, writes=["bxs"])
            lam = SB(st, "lam", [128, 4], F32); T.dma("sp", lam[:], llam, writes=["lam"])
            sp1 = SB(st, "sp1", [128, 4], F32); sp2 = SB(st, "sp2", [128, 4], F32); c1s = SB(st, "c1s", [128, 4], F32)
            T.op("dve", lambda e: e.tensor_scalar(out=sp1[:], in0=lam[:], scalar1=-1.0, scalar2=None, op0=ALU.mult), reads=["lam"], writes=["sp1"])
            T.op("dve", lambda e: e.tensor_tensor(out=sp1[:], in0=sp1[:], in1=lam[:], op=ALU.max), reads=["lam", "sp1"], writes=["sp1"])
            T.op("act", lambda e: e.activation(out=sp1[:], in_=sp1[:], func=AF.Exp, scale=-1.0), reads=["sp1"], writes=["sp1"])
            T.op("act", lambda e: e.activation(out=sp1[:], in_=sp1[:], func=AF.Ln, bias=1.0, scale=1.0), reads=["sp1"], writes=["sp1"])
            T.op("dve", lambda e: e.tensor_scalar(out=sp2[:], in0=lam[:], scalar1=-1.0, scalar2=0.0, op0=ALU.mult, op1=ALU.max), reads=["lam"], writes=["sp2"])
            T.op("dve", lambda e: e.tensor_tensor(out=c1s[:], in0=sp1[:], in1=sp2[:], op=ALU.add), reads=["sp1", "sp2"], writes=["c1s"])
            T.op("dve", lambda e: e.tensor_scalar(out=c1s[:], in0=c1s[:], scalar1=-8.0, scalar2=None, op0=ALU.mult), reads=["c1s"], writes=["c1s"])

            if stop == 2 and substop == 1:
                T.finish_early()
            nTp = Rot([SB(st, "anT%d" % i, [128, 8, 512], BF16) for i in range(1)], "anT")
            xl = [SB(st, "xl%d" % c, [128, 515], F32) for c in range(4)]
            hprev = SB(st, "hprev", [128, 4], F32)
            T.op("dve", lambda e: e.memset(hprev[:], 0.0), writes=["hprev"])
            for c in range(4):
                T.op("dve", lambda e, c=c: e.memset(xl[c][:], 0.0), writes=["xl%d" % c])
            xa = [SB(st, "xa%d" % c, [128, 512], F32) for c in range(4)]
            xab = [SB(st, "xab%d" % c, [128, 512], BF16) for c in range(4)]
            rr = [SB(st, "rr%d" % c, [128, 512], F32) for c in range(4)]
            ig = [SB(st, "ig%d" % c, [128, 512], F32) for c in range(4)]
            gl = [SB(st, "gl%d" % c, [128, 512], BF16) for c in range(4)]
            sq = [SB(st, "sq%d" % c, [128, 512], F32) for c in range(4)]
            hs = sq
            yaT = Rot([SB(st, "yaT%d" % i, [128, 4, 512], BF16) for i in range(1)], "yaT")
            ycT = Rot([SB(st, "ycT%d" % i, [128, 4, 512], BF16) for i in range(1)], "ycT")
            qmT = Rot([SB(st, "qmT%d" % i, [128, 512], BF16) for i in range(3)], "qmT")
            pTm = Rot([SB(st, "pTm%d" % i, [128, 512], BF16) for i in range(6)], "pTm")
            rden = Rot([SB(st, "rdm%d" % i, [128, 512], F32) for i in range(2)], "rdm")
            latn = Rot([SB(st, "latn%d" % i, [128, 480], BF16) for i in range(4)], "latn")
            for i in range(4):
                T.op("pool", lambda e, i=i: e.memset(latn.bufs[i][:], 0.0), writes=["latn#%d" % i])
            ss2 = Rot([SB(st, "ss2_%d" % i, [128, 8], F32) for i in range(4)], "ss2")
            rt = Rot([SB(st, "rt%d" % i, [128, 4, 16], F32) for i in range(4)], "rt")
            ya_v = ya_s.rearrange("(c p) t -> p c t", p=128)
            yc_v = yc_s.rearrange("(c p) t -> p c t", p=128)

            if stop == 2 and substop == 11:
                T.finish_early()
            def a_s1(i):
                nbs = []
                for sub_ in range(4):
                    r0 = i * 512 + sub_ * 128
                    xt, xk = pools["xt"].next()
                    T.dma("sp", xt[:], x[r0:r0 + 128, :], writes=[xk])
                    jk, jkk = pools["junk"].next()
                    ss, sk = pools["ss"].next()
                    T.op("act", lambda e, jk=jk, xt=xt, ss=ss: e.activation(out=jk[:], in_=xt[:], func=AF.Square, accum_out=ss[:, 0:1]), reads=[xk], writes=[sk, jkk])
                    T.op("dve", lambda e, ss=ss: e.tensor_scalar(out=ss[:, 1:2], in0=ss[:, 0:1], scalar1=1.0 / D, scalar2=EPS, op0=ALU.mult, op1=ALU.add), reads=[sk], writes=[sk])
                    T.op("pool", lambda e, ss=ss: e.tensor_tensor(out=ss[:, 2:3], in0=ss[:, 1:2], in1=mh[:, 0:1], op=ALU.pow), reads=[sk, "mh"], writes=[sk])
                    nb, nk = pools["nb"].next()
                    T.op("dve", lambda e, nb=nb, xt=xt, ss=ss, gb=gbc["g_mix"]: e.scalar_tensor_tensor(out=nb[:], in0=xt[:], scalar=ss[:, 2:3], in1=gb[:], op0=ALU.mult, op1=ALU.mult),
                         reads=[xk, sk, "bc_g_mix"], writes=[nk])
                    nbs.append((nb, nk))
                return nbs

            def a_s2(nbs):
                nT, nTk = nTp.next()
                nTkeys = []
                for sub_ in range(4):
                    nb, nk = nbs[sub_]
                    pt, pk = pools["ps"].next()
                    ptb = pt[:].bitcast(BF16)
                    T.group("pe", [(lambda e, ptb=ptb, nb=nb, k=k: e.transpose(out=ptb[:, k * 128:(k + 1) * 128], in_=nb[:, k * 128:(k + 1) * 128], identity=idb[:])) for k in range(8)],
                            reads=[nk, "idb"], writes=[pk])
                    dk = "%s_s%d" % (nTk, sub_)
                    T.op("act", lambda e, ptb=ptb, nT=nT, sub_=sub_: e.activation(out=nT[:, :, sub_ * 128:(sub_ + 1) * 128], in_=ptb.rearrange("p (k t) -> p k t", k=8), func=AF.Copy),
                         reads=[pk], writes=[dk])
                    nTkeys.append(dk)
                return nT, nTk, nTkeys

            nbs_next = a_s1(0)
            for i in range(NB):
                nT, nTk, nTkeys = a_s2(nbs_next)
                if stop == 2 and substop == 2 and i == 0:
                    T.finish_early()
                LS = []
                for sub in range(4):
                    ti = i * 4 + sub
                    p, pk = psr.next()
                    mm(p[:, 0:416], [(nT[:, k, sub * 128:(sub + 1) * 128], win[:, k, 1024:1440]) for k in range(8)], reads=winkeys + [nTkeys[sub]], writes=[pk])
                    LS.append({"ti": ti, "tok": slice(ti * 128, (ti + 1) * 128), "p": p, "pk": pk})
                for L_ in LS:
                    p, pk = L_["p"], L_["pk"]
                    s2, s2k = ss2.next()
                    jk, jkk = pools["junk"].next()
                    T.op("act", lambda e, p=p, s2=s2, jk=jk: e.activation(out=jk[:, 0:256], in_=p[:, 0:256], func=AF.Square, accum_out=s2[:, 0:1]), reads=[pk], writes=[s2k, jkk])
                    T.op("act", lambda e, p=p, s2=s2, jk=jk: e.activation(out=jk[:, 256:384], in_=p[:, 256:384], func=AF.Square, accum_out=s2[:, 1:2]), reads=[pk], writes=[s2k, jkk])
                    L_["s2"], L_["s2k"] = s2, s2k
                for L_ in LS:
                    s2, s2k = L_["s2"], L_["s2k"]
                    T.op("dve", lambda e, s2=s2: e.tensor_scalar(out=s2[:, 2:3], in0=s2[:, 0:1], scalar1=1.0 / 256, scalar2=EPS, op0=ALU.mult, op1=ALU.add), reads=[s2k], writes=[s2k])
                    T.op("dve", lambda e, s2=s2: e.tensor_scalar(out=s2[:, 3:4], in0=s2[:, 1:2], scalar1=1.0 / 128, scalar2=EPS, op0=ALU.mult, op1=ALU.add), reads=[s2k], writes=[s2k])
                for L_ in LS:
                    s2, s2k = L_["s2"], L_["s2k"]
                    T.op("pool", lambda e, s2=s2: e.tensor_tensor(out=s2[:, 4:6], in0=s2[:, 2:4], in1=mh[:, 0:2], op=ALU.pow), reads=[s2k, "mh"], writes=[s2k])
                for L_ in LS:
                    p, pk, s2, s2k = L_["p"], L_["pk"], L_["s2"], L_["s2k"]
                    ln_, lk = latn.next()
                    T.op("dve", lambda e, p=p, s2=s2, ln_=ln_: e.scalar_tensor_tensor(out=ln_[:, 0:256], in0=p[:, 0:256], scalar=s2[:, 4:5], in1=gq_bc[:], op0=ALU.mult, op1=ALU.mult),
                         reads=[pk, s2k, "gq_bc"], writes=[lk])
                    T.op("dve", lambda e, p=p, s2=s2, ln_=ln_: e.scalar_tensor_tensor(out=ln_[:, 256:384], in0=p[:, 256:384], scalar=s2[:, 5:6], in1=gkv_bc[:], op0=ALU.mult, op1=ALU.mult),
                         reads=[pk, s2k, "gkv_bc"], writes=[lk])
                    L_["ln"], L_["lk"] = ln_, lk
                for L_ in LS:
                    p, pk, ti = L_["p"], L_["pk"], L_["ti"]
                    r_, rk = rt.next()
                    cs = cosT[:, ti, :]; sn = sinT[:, ti, :]
                    T.op("dve", lambda e, p=p, r_=r_, cs=cs: e.tensor_tensor(out=r_[:, 0, :], in0=p[:, 384:400], in1=cs, op=ALU.mult), reads=[pk, "cosT"], writes=[rk])
                    T.op("dve", lambda e, p=p, r_=r_, sn=sn: e.tensor_tensor(out=r_[:, 1, :], in0=p[:, 400:416], in1=sn, op=ALU.mult), reads=[pk, "sinT"], writes=[rk])
                    T.op("dve", lambda e, p=p, r_=r_, cs=cs: e.tensor_tensor(out=r_[:, 2, :], in0=p[:, 400:416], in1=cs, op=ALU.mult), reads=[pk, "cosT"], writes=[rk])
                    T.op("dve", lambda e, p=p, r_=r_, sn=sn: e.tensor_tensor(out=r_[:, 3, :], in0=p[:, 384:400], in1=sn, op=ALU.mult), reads=[pk, "sinT"], writes=[rk])
                    L_["r"], L_["rk"] = r_, rk
                for L_ in LS:
                    r_, rk, ln_, lk = L_["r"], L_["rk"], L_["ln"], L_["lk"]
                    T.op("pool", lambda e, r_=r_, ln_=ln_: e.tensor_tensor(out=ln_[:, 448:464], in0=r_[:, 0, :], in1=r_[:, 1, :], op=ALU.subtract), reads=[rk], writes=[lk])
                    T.op("pool", lambda e, r_=r_, ln_=ln_: e.tensor_tensor(out=ln_[:, 464:480], in0=r_[:, 2, :], in1=r_[:, 3, :], op=ALU.add), reads=[rk], writes=[lk])
                if i + 1 < NB:
                    nbs_next = a_s1(i + 1)
                if stop == 2 and substop == 3 and i == 0:
                    T.finish_early()
                for c in range(4):
                    p, pk = psr.next()
                    mm(p[:, :], [(win[:, k, c * 128:(c + 1) * 128], nT[:, k, :]) for k in range(8)], reads=winkeys + nTkeys, writes=[pk])
                    xk = "xl%d" % c
                    T.op("pool", lambda e, c=c: e.tensor_copy(out=xl[c][:, 0:3], in_=xl[c][:, 512:515]), reads=[xk], writes=[xk])
                    T.op("act", lambda e, c=c, p=p: e.activation(out=xl[c][:, 3:515], in_=p[:, :], func=AF.Copy), reads=[pk, xk], writes=[xk])
                for c in range(4):
                    xk = "xl%d" % c
                    T.op("dve", lambda e, c=c: e.tensor_scalar(out=xa[c][:], in0=xl[c][:, 0:512], scalar1=cws[:, c, 0:1], scalar2=cbs[:, c:c + 1], op0=ALU.mult, op1=ALU.add),
                         reads=[xk, "cws", "cbs"], writes=["xa%d" % c])
                    for j in range(1, 4):
                        T.op("dve", lambda e, c=c, j=j: e.scalar_tensor_tensor(out=xa[c][:], in0=xl[c][:, j:j + 512], scalar=cws[:, c, j:j + 1], in1=xa[c][:], op0=ALU.mult, op1=ALU.add),
                             reads=[xk, "cws", "xa%d" % c], writes=["xa%d" % c])
                for c in range(4):
                    T.op("pool", lambda e, c=c: e.tensor_copy(out=xab[c][:], in_=xa[c][:]), reads=["xa%d" % c], writes=["xab%d" % c])
                for c in range(4):
                    p, pk = psr.next()
                    mm(p[:, :], [(win[:, k, 512 + c * 128:512 + (c + 1) * 128], nT[:, k, :]) for k in range(8)], reads=winkeys + nTkeys, writes=[pk])
                    T.op("act", lambda e, c=c, p=p: e.activation(out=gl[c][:], in_=p[:, :], func=AF.Gelu_apprx_tanh), reads=[pk], writes=["gl%d" % c])
                for L_ in LS:
                    ln_, lk = L_["ln"], L_["lk"]
                    pt, ptk = pools["ps"].next()
                    ptb = pt[:].bitcast(BF16)
                    T.group("pe", [
                        lambda e, ptb=ptb, ln_=ln_: e.transpose(out=ptb[:, 0:128], in_=ln_[:, 0:128], identity=idb[:]),
                        lambda e, ptb=ptb, ln_=ln_: e.transpose(out=ptb[:, 128:256], in_=ln_[:, 128:256], identity=idb[:]),
                        lambda e, ptb=ptb, ln_=ln_: e.transpose(out=ptb[:, 256:384], in_=ln_[:, 256:384], identity=idb[:]),
                        lambda e, ptb=ptb, ln_=ln_: e.transpose(out=ptb[0:96, 384:512], in_=ln_[:, 384:480], identity=idb[:]),
                    ], reads=[lk, "idb"], writes=[ptk])
                    tok, ti = L_["tok"], L_["ti"]
                    T.op("act", lambda e, ptb=ptb, tok=tok: e.activation(out=cqnT[:, :, tok], in_=ptb[:, 0:256].rearrange("p (k t) -> p k t", k=2), func=AF.Copy),
                         reads=[ptk], writes=["cqnT%d" % ti])
                    T.op("act", lambda e, ptb=ptb, tok=tok: e.activation(out=ckvnT[:, tok], in_=ptb[:, 256:384], func=AF.Copy), reads=[ptk], writes=["ckvnT%d" % ti])
                    T.op("act", lambda e, ptb=ptb, tok=tok: e.activation(out=kropeT[64:96, tok], in_=ptb[64:96, 384:512], func=AF.Copy), reads=[ptk], writes=["kropeT%d" % ti])
                for c in range(4):
                    p, pk = psr.next()
                    mm(p[:, :], [(wabd[:, c, :], xab[c][:])], reads=["wabd", "xab%d" % c], writes=[pk])
                    T.op("act", lambda e, c=c, p=p: e.activation(out=rr[c][:], in_=p[:, :], func=AF.Sigmoid, bias=bas[:, c:c + 1]), reads=[pk, "bas"], writes=["rr%d" % c])
                    p, pk = psr.next()
                    mm(p[:, :], [(wxbd[:, c, :], xab[c][:])], reads=["wxbd", "xab%d" % c], writes=[pk])
                    T.op("act", lambda e, c=c, p=p: e.activation(out=ig[c][:], in_=p[:, :], func=AF.Sigmoid, bias=bxs[:, c:c + 1]), reads=[pk, "bxs"], writes=["ig%d" % c])
                for c in range(4):
                    T.op("act", lambda e, c=c: e.activation(out=rr[c][:], in_=rr[c][:], func=AF.Exp, scale=c1s[:, c:c + 1]), reads=["rr%d" % c, "c1s"], writes=["rr%d" % c])
                if stop == 2 and substop == 4 and i == 0:
                    T.finish_early()
                ycb, yck = ycT.next()

                def ma_q(h):
                    p, pk = psr.next()
                    mm(p[:, :], [(win[:, k, 1440 + h * 128:1440 + (h + 1) * 128], nT[:, k, :]) for k in range(8)], reads=winkeys + nTkeys, writes=[pk])
                    qm, qmk = qmT.next()
                    T.op("dve", lambda e, qm=qm, p=p: e.tensor_copy(out=qm[:], in_=p[:, :]), reads=[pk], writes=[qmk])
                    return qm, qmk

                def ma_s(h, qm, qmk):
                    pts = []
                    for mc in range(2):
                        p2, p2k = psr.next()
                        mm(p2[:, :], [(kmemT[:, h, mc * 128:(mc + 1) * 128], qm[:])], reads=[qmk], writes=[p2k])
                        pT, pTk = pTm.next()
                        T.op("act", lambda e, pT=pT, p2=p2: e.activation(out=pT[:], in_=p2[:, :], func=AF.Exp, scale=1.0 / math.sqrt(128.0)), reads=[p2k], writes=[pTk])
                        pts.append((pT, pTk))
                    return pts

                def ma_o(h, pts):
                    pn, pnk = psr.next()
                    mm(pn[:, :], [(vmem[:, mc, h * 128:(h + 1) * 128], pts[mc][0][:]) for mc in range(2)], reads=[pts[0][1], pts[1][1]], writes=[pnk])
                    pd, pdk = psr.next()
                    mm(pd[:, :], [(onesb[:], pts[mc][0][:]) for mc in range(2)], reads=[pts[0][1], pts[1][1], "onesb"], writes=[pdk])
                    rd, rdk = rden.next()
                    T.op("dve", lambda e, rd=rd, pd=pd: e.reciprocal(out=rd[:], in_=pd[:, :]), reads=[pdk], writes=[rdk])
                    T.op("dve", lambda e, rd=rd, pn=pn, ycb=ycb, h=h: e.tensor_tensor(out=ycb[:, h, :], in0=pn[:, :], in1=rd[:], op=ALU.mult), reads=[pnk, rdk], writes=[yck])

                q_ = {0: ma_q(0)}
                q_[1] = ma_q(1)
                s_ = {0: ma_s(0, *q_[0])}
                for h in range(4):
                    if h + 2 < 4:
                        q_[h + 2] = ma_q(h + 2)
                    if h + 1 < 4:
                        s_[h + 1] = ma_s(h + 1, *q_[h + 1])
                    ma_o(h, s_[h])
                T.dma("sp", yc_v[:, :, i * 512:(i + 1) * 512], ycb[:], reads=[yck], writes=["yc_s%d" % i])
                if stop == 2 and substop == 5 and i == 0:
                    T.finish_early()
                yab, yak = yaT.next()
                for c in range(4):
                    T.op("pool", lambda e, c=c: e.tensor_tensor(out=sq[c][:], in0=rr[c][:], in1=rr[c][:], op=ALU.mult), reads=["rr%d" % c], writes=["sq%d" % c])
                for c in range(4):
                    T.op("act", lambda e, c=c: e.activation(out=sq[c][:], in_=sq[c][:], func=AF.Sqrt, scale=-1.0, bias=1.0), reads=["sq%d" % c], writes=["sq%d" % c])
                for c in range(4):
                    T.op("pool", lambda e, c=c: e.tensor_tensor(out=ig[c][:], in0=ig[c][:], in1=xa[c][:], op=ALU.mult), reads=["ig%d" % c, "xa%d" % c], writes=["ig%d" % c])
                    T.op("pool", lambda e, c=c: e.tensor_tensor(out=ig[c][:], in0=ig[c][:], in1=sq[c][:], op=ALU.mult), reads=["ig%d" % c, "sq%d" % c], writes=["ig%d" % c])
                    T.op("dve", lambda e, c=c: e.tensor_tensor_scan(out=hs[c][:], data0=rr[c][:], data1=ig[c][:], initial=hprev[:, c:c + 1], op0=ALU.mult, op1=ALU.add),
                         reads=["rr%d" % c, "ig%d" % c, "hprev", "sq%d" % c], writes=["sq%d" % c])
                    T.op("dve", lambda e, c=c: e.tensor_copy(out=hprev[:, c:c + 1], in_=hs[c][:, 511:512]), reads=["sq%d" % c], writes=["hprev"])
                    T.op("pool", lambda e, c=c, yab=yab: e.tensor_tensor(out=yab[:, c, :], in0=hs[c][:], in1=gl[c][:], op=ALU.mult), reads=["sq%d" % c, "gl%d" % c], writes=[yak])
                T.dma("sp", ya_v[:, :, i * 512:(i + 1) * 512], yab[:], reads=[yak], writes=["ya_s%d" % i])
            T.barrier()
            if stop == 2:
                T.finish_early()

        stCW = ExitStack()
        wg = stCW.enter_context(nc.sbuf_tensor("wg", [128, 8, 3072], BF16, side="right"))
        wbr = stCW.enter_context(nc.sbuf_tensor("wbr", [128, 3, 4, D], BF16, side="right"))
        wo = stCW.enter_context(nc.sbuf_tensor("wo", [128, 8, D], BF16, side="right"))
        with ExitStack() as st:
            wuq = SB(st, "wuq", [128, 2, 768], BF16)
            wukv = SB(st, "wukv", [128, 1024], BF16)
            for k in range(2):
                T.dma("pool", wuq[:, k, :], w_uq[k * 128:(k + 1) * 128, :], writes=["wuq"])
            T.dma("pool", wukv[:], w_ukv, writes=["wukv"])
            msk = SB(st, "msk", [128, 4, 512], BF16)
            for r in range(4):
                T.dma("pool", msk[:, r, :], cmask[r], writes=["msk"])
            prefetch_c_weights = True
            KT = [SB(st, "KT%d" % i, [128, S], BF16) for i in range(2)]
            QT = [SB(st, "QT%d" % i, [128, S], BF16) for i in range(2)]
            for i in range(2):
                T.op("pool", lambda e, i=i: e.memset(KT[i][:], 0.0), writes=["KT%d" % i])
                T.op("pool", lambda e, i=i: e.memset(QT[i][:], 0.0), writes=["QT%d" % i])
            VH = [SB(st, "VH%d" % i, [128, NT, 128], BF16) for i in range(2)]
            for i in range(2):
                T.op("pool", lambda e, i=i: e.memset(VH[i][:], 1.0), writes=["VH%d" % i])
            for k in range(8):
                for b3 in range(3):
                    T.dma("pool", wg[:, k, b3 * 1024:(b3 + 1) * 1024], w_in[k * 128:(k + 1) * 128, 1952 + b3 * 1024:1952 + (b3 + 1) * 1024], writes=["wg%d_%d" % (k, b3)])
            for b3 in range(3):
                for kc in range(4):
                    T.dma("pool", wbr[:, b3, kc, :], w_br[b3, kc * 128:(kc + 1) * 128, :], writes=["wbr%d_%d" % (b3, kc)])
            for k in range(8):
                T.dma("pool", wo[:, k, :], w_o[k * 128:(k + 1) * 128, :], writes=["wo%d" % k])
            NQB = 1 if "q1" in EXP else 2
            qtok = Rot([SB(st, "qtok%d" % i, [128, 4, 96], BF16) for i in range(NQB)], "qtok", shared=True)
            rt = Rot([SB(st, "brt%d" % i, [128, 4, 4, 16], F32) for i in range(NQB)], "brt", shared=True)
            pTb = Rot([SB(st, "pTb%d" % i, [128, 512], BF16) for i in range(6)], "pTb")
            rdb = Rot([SB(st, "rdb%d" % i, [128, 512], F32) for i in range(2)], "rdb")
            ybT = Rot([SB(st, "ybT%d" % i, [64, 512], BF16) for i in range(2)], "ybT")
            psr = Rot(PS[0:4] + PS[6:8], "psb")
            pso = Rot(PS[4:6], "pso")
            scale = 1.0 / math.sqrt(96.0)
            allck = ["ckvnT%d" % t for t in range(NT)]
            allcq = ["cqnT%d" % t for t in range(NT)]
            allkr = ["kropeT%d" % t for t in range(NT)]
            def prep(h):
                b = h % 2
                KTh, QTh, VHh = KT[b], QT[b], VH[b]
                kk, qk, vk = "KT%d" % b, "QT%d" % b, "VH%d" % b
                for i in range(NB):
                    p, pk = psr.next()
                    mm(p[0:64, :], [(wukv[:, h * 128:h * 128 + 64], ckvnT[:, i * 512:(i + 1) * 512])], reads=["wukv"] + allck[i * 4:(i + 1) * 4], writes=[pk])
                    T.op("act", lambda e, p=p, i=i, KTh=KTh: e.activation(out=KTh[0:64, i * 512:(i + 1) * 512], in_=p[0:64, :], func=AF.Copy), reads=[pk], writes=[kk])
                T.op("act", lambda e, KTh=KTh: e.activation(out=KTh[64:96, :], in_=kropeT[64:96, :], func=AF.Copy), reads=allkr, writes=[kk])
                for g in range(4):
                    p, pk = psr.next()
                    T.group("pe", [(lambda e, p=p, j=j, g=g, h=h: e.matmul(p[:, j * 64:(j + 1) * 64], lhsT=ckvnT[:, (g * 8 + j) * 128:(g * 8 + j + 1) * 128],
                                                                        rhs=wukv[:, h * 128 + 64:h * 128 + 128], start=True, stop=True)) for j in range(8)],
                            reads=["wukv"] + allck[g * 8:(g + 1) * 8], writes=[pk])
                    T.op("dve", lambda e, p=p, g=g, VHh=VHh: e.tensor_copy(out=VHh[:, g * 8:(g + 1) * 8, 0:64], in_=p[:, :].rearrange("p (j d) -> p j d", j=8)), reads=[pk], writes=[vk])
                for g in range(NB):
                    p, pk = psr.next()
                    fns = []
                    for j in range(4):
                        ti = g * 4 + j
                        for k in range(2):
                            fns.append(lambda e, p=p, j=j, k=k, ti=ti, h=h: e.matmul(p[:, j * 96:(j + 1) * 96], lhsT=cqnT[:, k, ti * 128:(ti + 1) * 128],
                                                                                  rhs=wuq[:, k, h * 96:(h + 1) * 96], start=(k == 0), stop=(k == 1)))
                    T.group("pe", fns, reads=["wuq"] + allcq[g * 4:(g + 1) * 4], writes=[pk])
                    pv = p[:, 0:384].rearrange("p (j d) -> p j d", j=4)
                    qt, qtk = qtok.next()
                    T.op("act", lambda e, pv=pv, qt=qt: e.activation(out=qt[:, :, 0:64], in_=pv[:, :, 0:64], func=AF.Copy), reads=[pk], writes=[qtk])
                    r_, rk = rt.next()
                    cs = cosT[:, g * 4:(g + 1) * 4, :]; sn = sinT[:, g * 4:(g + 1) * 4, :]
                    T.op("dve", lambda e, pv=pv, r_=r_, cs=cs: e.tensor_tensor(out=r_[:, 0, :, :], in0=pv[:, :, 64:80], in1=cs, op=ALU.mult), reads=[pk, "cosT"], writes=[rk])
                    T.op("dve", lambda e, pv=pv, r_=r_, sn=sn: e.tensor_tensor(out=r_[:, 1, :, :], in0=pv[:, :, 80:96], in1=sn, op=ALU.mult), reads=[pk, "sinT"], writes=[rk])
                    T.op("dve", lambda e, pv=pv, r_=r_, cs=cs: e.tensor_tensor(out=r_[:, 2, :, :], in0=pv[:, :, 80:96], in1=cs, op=ALU.mult), reads=[pk, "cosT"], writes=[rk])
                    T.op("dve", lambda e, pv=pv, r_=r_, sn=sn: e.tensor_tensor(out=r_[:, 3, :, :], in0=pv[:, :, 64:80], in1=sn, op=ALU.mult), reads=[pk, "sinT"], writes=[rk])
                    T.op("dve", lambda e, r_=r_, qt=qt: e.tensor_tensor(out=qt[:, :, 64:80], in0=r_[:, 0, :, :], in1=r_[:, 1, :, :], op=ALU.subtract), reads=[rk], writes=[qtk])
                    T.op("dve", lambda e, r_=r_, qt=qt: e.tensor_tensor(out=qt[:, :, 80:96], in0=r_[:, 2, :, :], in1=r_[:, 3, :, :], op=ALU.add), reads=[rk], writes=[qtk])
                    pt, ptk = psr.next()
                    ptb = pt[:].bitcast(BF16)
                    T.group("pe", [(lambda e, ptb=ptb, qt=qt, j=j: e.transpose(out=ptb[0:96, j * 128:(j + 1) * 128], in_=qt[:, j, :], identity=idb[:])) for j in range(4)],
                            reads=[qtk, "idb"], writes=[ptk])
                    T.op("dve", lambda e, ptb=ptb, g=g, QTh=QTh: e.tensor_copy(out=QTh[0:96, g * 512:(g + 1) * 512], in_=ptb[0:96, 0:512]), reads=[ptk], writes=[qk])

            LOOK = 5

            def attn(h):
                b = h % 2
                KTh, QTh, VHh = KT[b], QT[b], VH[b]
                kk, qk, vk = "KT%d" % b, "QT%d" % b, "VH%d" % b
                items = [(i, kt) for i in range(NB) for kt in range(4 * i + 4)]
                N = len(items)
                S1 = {}
                acc = {}

                def stage1(n):
                    i, kt = items[n]
                    p, pk = psr.next()
                    mm(p[:, :], [(KTh[:, kt * 128:(kt + 1) * 128], QTh[:, i * 512:(i + 1) * 512])], reads=[kk, qk], writes=[pk])
                    pT, pTk = pTb.next()
                    T.op("act", lambda e, pT=pT, p=p: e.activation(out=pT[:], in_=p[:, :], func=AF.Exp, scale=scale), reads=[pk], writes=[pTk])
                    if kt >= 4 * i:
                        r = kt - 4 * i
                        T.op("dve", lambda e, pT=pT, r=r: e.tensor_tensor(out=pT[:], in0=pT[:], in1=msk[:, r, :], op=ALU.mult), reads=[pTk, "msk"], writes=[pTk])
                    S1[n] = (pT, pTk)

                def stage2(n):
                    i, kt = items[n]
                    nk = 4 * i + 4
                    if kt == 0:
                        acc[i] = pso.next()
                    po, pok = acc[i]
                    pT, pTk = S1.pop(n)
                    T.group("pe", [lambda e, po=po, pT=pT, kt=kt, nk=nk, VHh=VHh: e.matmul(po[:, :], lhsT=VHh[:, kt, :], rhs=pT[:], start=(kt == 0), stop=(kt == nk - 1))],
                            reads=[pTk, vk], writes=[pok])
                    if kt == nk - 1:
                        rd, rdk = rdb.next()
                        T.op("dve", lambda e, rd=rd, po=po: e.reciprocal(out=rd[64:128, :], in_=po[64:128, :]), reads=[pok], writes=[rdk])
                        yb, ybk = ybT.next()
                        T.op("dve", lambda e, rd=rd, po=po, yb=yb: e.tensor_tensor(out=yb[:], in0=po[0:64, :], in1=rd[64:128, :], op=ALU.mult), reads=[pok, rdk], writes=[ybk])
                        T.dma("sp", yb_s[h * 64:(h + 1) * 64, i * 512:(i + 1) * 512], yb[:], reads=[ybk], writes=["yb_s_%d_%d" % (h, i)])

                for n in range(min(LOOK, N)):
                    stage1(n)
                for n in range(N):
                    if n + LOOK < N:
                        stage1(n + LOOK)
                    stage2(n)

            prep(0)
            for h in range(8):
                if h + 1 < 8:
                    prep(h + 1)
                attn(h)
            T.barrier()
            if stop == 3:
                T.finish_early()

        stAB.close()
        comb = SB(top, "comb", [128, NT, NE], F32)
        lg = SB(top, "lg", [128, NT, 36], F32)
        with ExitStack() as st:
            load_g(st, "g_mix", "c")
            load_g(st, "g_ffn", "c")
            pools = {
                "xt": Rot([SB(st, "cxt%d" % i, [128, D], F32) for i in range(4)], "cxt"),
                "junk": Rot([SB(st, "cjk", [128, D], BF16)], "cjk"),
                "ss": Rot([SB(st, "css%d" % i, [128, 4], F32) for i in range(8)], "css"),
                "nb": Rot([SB(st, "cnb%d" % i, [128, D], BF16) for i in range(2)], "cnb"),
                "ps": Rot(PS[0:2], "psn"),
            }
            psr = Rot(PS[2:8], "psc")
            wgkeys = ["wg%d_%d" % (k, b3) for k in range(8) for b3 in range(3)]
            wgr = SB(st, "wgr", [128, 8, 36], F32)
            T.dma("sp", wgr[:], w_gr.rearrange("(k p) c -> p k c", p=128), writes=["wgr"])
            TC = 256
            NSC = TC // 128
            NBC = S // TC
            nTp = Rot([SB(st, "cnT%d" % i, [128, 8, TC], BF16) for i in range(1)], "cnT")
            yT = [Rot([SB(st, "yT%d_%d" % (b3, i), [128, 4, TC], BF16) for i in range(2)], "yT%d" % b3) for b3 in range(3)]
            ysrc = [ya_s.rearrange("(c p) t -> p c t", p=128), yb_s.rearrange("(c p) t -> p c t", p=128), yc_s.rearrange("(c p) t -> p c t", p=128)]
            gs = Rot([SB(st, "gs%d" % i, [128, TC], F32) for i in range(3)], "gs")
            tb = Rot([SB(st, "tb%d" % i, [128, TC], F32) for i in range(6)], "tb")
            mT = Rot([SB(st, "mT%d" % i, [128, 8, TC], BF16) for i in range(1)], "mT")
            hT = Rot([SB(st, "hT%d" % i, [128, D], F32) for i in range(2)], "hT")
            n2f = Rot([SB(st, "n2f%d" % i, [128, D], F32) for i in range(2)], "n2f")
            n2Tf = Rot([SB(st, "n2Tf%d" % i, [128, 8, 128], F32) for i in range(1)], "n2Tf")
            n2Tb = Rot([SB(st, "n2Tb%d" % i, [128, 8, TC], BF16) for i in range(1)], "n2Tb")
            n2T_v = n2T_s.rearrange("(k p) t -> p k t", p=128)

            def c_s1(i):
                st_ = {"i": i, "xts": [], "nbs": []}
                for sub in range(NSC):
                    r0 = i * TC + sub * 128
                    xt, xk = pools["xt"].next()
                    T.dma("sp", xt[:], x[r0:r0 + 128, :], writes=[xk])
                    jk, jkk = pools["junk"].next()
                    ss, sk = pools["ss"].next()
                    T.op("act", lambda e, jk=jk, xt=xt, ss=ss: e.activation(out=jk[:], in_=xt[:], func=AF.Square, accum_out=ss[:, 0:1]), reads=[xk], writes=[sk, jkk])
                    T.op("dve", lambda e, ss=ss: e.tensor_scalar(out=ss[:, 1:2], in0=ss[:, 0:1], scalar1=1.0 / D, scalar2=EPS, op0=ALU.mult, op1=ALU.add), reads=[sk], writes=[sk])
                    T.op("pool", lambda e, ss=ss: e.tensor_tensor(out=ss[:, 2:3], in0=ss[:, 1:2], in1=mh[:, 0:1], op=ALU.pow), reads=[sk, "mh"], writes=[sk])
                    nb, nk = pools["nb"].next()
                    T.op("dve", lambda e, nb=nb, xt=xt, ss=ss, gb=gbc["g_mix"]: e.scalar_tensor_tensor(out=nb[:], in0=xt[:], scalar=ss[:, 2:3], in1=gb[:], op0=ALU.mult, op1=ALU.mult),
                         reads=[xk, sk, "bc_g_mix"], writes=[nk])
                    st_["xts"].append((xt, xk))
                    st_["nbs"].append((nb, nk))
                ys = []
                for b3 in range(3):
                    y_, yk = yT[b3].next()
                    T.dma("sp", y_[:], ysrc[b3][:, :, i * TC:(i + 1) * TC], writes=[yk])
                    ys.append((y_, yk))
                st_["ys"] = ys
                return st_

            def c_s2(st_):
                nT, nTk = nTp.next()
                nTkeys = []
                for sub in range(NSC):
                    nb, nk = st_["nbs"][sub]
                    pt, pk = pools["ps"].next()
                    ptb = pt[:].bitcast(BF16)
                    T.group("pe", [(lambda e, ptb=ptb, nb=nb, k=k: e.transpose(out=ptb[:, k * 128:(k + 1) * 128], in_=nb[:, k * 128:(k + 1) * 128], identity=idb[:])) for k in range(8)],
                            reads=[nk, "idb"], writes=[pk])
                    dk = "%s_s%d" % (nTk, sub)
                    T.op("act", lambda e, ptb=ptb, nT=nT, sub=sub: e.activation(out=nT[:, :, sub * 128:(sub + 1) * 128], in_=ptb.rearrange("p (k t) -> p k t", k=8), func=AF.Copy),
                         reads=[pk], writes=[dk])
                    nTkeys.append(dk)
                ys = st_["ys"]
                m_, mk = mT.next()
                for c in range(8):
                    tbs = []
                    for b3 in range(3):
                        p, pk = psr.next()
                        mm(p[:, 0:TC], [(wg[:, k, b3 * 1024 + c * 128:b3 * 1024 + (c + 1) * 128], nT[:, k, :]) for k in range(8)], reads=wgkeys + nTkeys, writes=[pk])
                        g_, gk = gs.next()
                        T.op("act", lambda e, g_=g_, p=p: e.activation(out=g_[:], in_=p[:, 0:TC], func=AF.Sigmoid), reads=[pk], writes=[gk])
                        p2, p2k = psr.next()
                        mm(p2[:, 0:TC], [(wbr[:, b3, kc, c * 128:(c + 1) * 128], ys[b3][0][:, kc, :]) for kc in range(4)], reads=["wbr", ys[b3][1]], writes=[p2k])
                        t_, tk = tb.next()
                        T.op("dve", lambda e, t_=t_, p2=p2, g_=g_: e.tensor_tensor(out=t_[:], in0=p2[:, 0:TC], in1=g_[:], op=ALU.mult), reads=[p2k, gk], writes=[tk])
                        tbs.append((t_, tk))
                    T.op("pool", lambda e, a=tbs[0][0], b_=tbs[1][0]: e.tensor_tensor(out=a[:], in0=a[:], in1=b_[:], op=ALU.add), reads=[tbs[0][1], tbs[1][1]], writes=[tbs[0][1]])
                    T.op("pool", lambda e, a=tbs[0][0], b_=tbs[2][0], m_=m_, c=c: e.tensor_tensor(out=m_[:, c, :], in0=a[:], in1=b_[:], op=ALU.add),
                         reads=[tbs[0][1], tbs[2][1]], writes=["%s_c%d" % (mk, c)])
                st_["m"] = (m_, ["%s_c%d" % (mk, c) for c in range(8)])

            def c_s3(st_):
                i = st_["i"]
                m_, mkeys = st_["m"]
                st_["nfs"] = []
                for sub in range(NSC):
                    ti = i * NSC + sub
                    xt, xk = st_["xts"][sub]
                    h_, hk = hT.next()
                    for half in range(2):
                        p, pk = psr.next()
                        mm(p[:, :], [(m_[:, kc, sub * 128:(sub + 1) * 128], wo[:, kc, half * 512:(half + 1) * 512]) for kc in range(8)], reads=mkeys + ["wo"], writes=[pk])
                        T.op("dve", lambda e, h_=h_, p=p, xt=xt, half=half: e.tensor_tensor(out=h_[:, half * 512:(half + 1) * 512], in0=p[:, :], in1=xt[:, half * 512:(half + 1) * 512], op=ALU.add),
                             reads=[pk, xk], writes=[hk])
                    T.dma("sp", h_s[ti * 128:(ti + 1) * 128, :], h_[:], reads=[hk], writes=["h_s%d" % ti])
                    jk, jkk = pools["junk"].next()
                    ss, sk = pools["ss"].next()
                    T.op("act", lambda e, jk=jk, h_=h_, ss=ss: e.activation(out=jk[:], in_=h_[:], func=AF.Square, accum_out=ss[:, 0:1]), reads=[hk], writes=[sk, jkk])
                    T.op("dve", lambda e, ss=ss: e.tensor_scalar(out=ss[:, 1:2], in0=ss[:, 0:1], scalar1=1.0 / D, scalar2=EPS, op0=ALU.mult, op1=ALU.add), reads=[sk], writes=[sk])
                    T.op("pool", lambda e, ss=ss: e.tensor_tensor(out=ss[:, 2:3], in0=ss[:, 1:2], in1=mh[:, 0:1], op=ALU.pow), reads=[sk, "mh"], writes=[sk])
                    nf, nfk = n2f.next()
                    T.op("dve", lambda e, nf=nf, h_=h_, ss=ss: e.scalar_tensor_tensor(out=nf[:], in0=h_[:], scalar=ss[:, 2:3], in1=gbc["g_ffn"][:], op0=ALU.mult, op1=ALU.mult),
                         reads=[hk, sk, "bc_g_ffn"], writes=[nfk])
                    st_["nfs"].append((nf, nfk))

            def c_s4(st_):
                i = st_["i"]
                n2b, n2bk = n2Tb.next()
                for sub in range(NSC):
                    ti = i * NSC + sub
                    nf, nfk = st_["nfs"][sub]
                    ntf, ntfk = n2Tf.next()
                    for hh in range(2):
                        p, pk = psr.next()
                        T.group("pe", [(lambda e, p=p, nf=nf, hh=hh, k=k: e.transpose(out=p[:, k * 128:(k + 1) * 128], in_=nf[:, (hh * 4 + k) * 128:(hh * 4 + k + 1) * 128], identity=idf[:])) for k in range(4)],
                                reads=[nfk, "idf"], writes=[pk])
                        T.op("act", lambda e, p=p, ntf=ntf, hh=hh: e.activation(out=ntf[:, hh * 4:(hh + 1) * 4, :], in_=p[:, :].rearrange("p (k t) -> p k t", k=4), func=AF.Copy),
                             reads=[pk], writes=["%s_%d" % (ntfk, hh)])
                        T.op("dve", lambda e, p=p, n2b=n2b, hh=hh, sub=sub: e.tensor_copy(out=n2b[:, hh * 4:(hh + 1) * 4, sub * 128:(sub + 1) * 128], in_=p[:, :].rearrange("p (k t) -> p k t", k=4)),
                             reads=[pk], writes=["%s_%d_%d" % (n2bk, sub, hh)])
                    p, pk = psr.next()
                    mm(p[:, 0:36], [(ntf[:, k, :], wgr[:, k, :]) for k in range(8)], reads=["%s_0" % ntfk, "%s_1" % ntfk, "wgr"], writes=[pk])
                    T.op("dve", lambda e, p=p, ti=ti: e.tensor_tensor(out=lg[:, ti, :], in0=p[:, 0:36], in1=bgr_bc[:], op=ALU.add), reads=[pk, "bgr_bc"], writes=["lg%d" % ti])
                T.dma("sp", n2T_v[:, :, i * TC:(i + 1) * TC], n2b[:], reads=["%s_%d_%d" % (n2bk, s_, hh) for s_ in range(NSC) for hh in range(2)], writes=["n2T_s%d" % i])

            cur_c = c_s1(0)
            prev_c = None
            for t in range(NBC):
                c_s2(cur_c)
                nxt_c = c_s1(t + 1) if t + 1 < NBC else None
                if prev_c is not None:
                    c_s4(prev_c)
                c_s3(cur_c)
                prev_c = cur_c
                cur_c = nxt_c
            c_s4(prev_c)
            T.barrier()
            if stop == 4:
                T.finish_early()

        stCW.close()
        with ExitStack() as st:
            lgk = ["lg%d" % t for t in range(NT)]
            R = lambda nm, shp: SB(st, nm, shp, F32)
            gm = R("r_gm", [128, NT]); ge = R("r_ge", [128, NT, 4]); gsum = R("r_gsum", [128, NT]); gw = R("r_gw", [128, NT])
            mg = R("r_mg", [128, NT, 4]); eg = R("r_eg", [128, NT, 8]); tmp8 = R("r_tmp8", [128, NT, 8])
            m1 = R("r_m1", [128, NT]); m2 = R("r_m2", [128, NT]); sel = R("r_sel", [128, NT, 8]); pe_ = R("r_pe", [128, NT, 8]); psum_ = R("r_ps", [128, NT])
            glv = lg[:, :, 0:4]
            elv = lg[:, :, 4:36].rearrange("p t (g e) -> p t g e", g=4)

            def bc(ap2, n):
                return ap2.unsqueeze(2).to_broadcast([128, NT, n])
            T.op("dve", lambda e: e.tensor_reduce(out=gm[:], in_=glv, axis=AX.X, op=ALU.max), reads=lgk, writes=["gm"])
            T.op("dve", lambda e: e.tensor_tensor(out=ge[:], in0=glv, in1=bc(gm[:], 4), op=ALU.subtract), reads=lgk + ["gm"], writes=["ge"])
            T.op("dve", lambda e: e.tensor_tensor(out=mg[:], in0=glv, in1=bc(gm[:], 4), op=ALU.is_equal), reads=lgk + ["gm"], writes=["mg"])
            T.op("act", lambda e: e.activation(out=ge[:], in_=ge[:], func=AF.Exp), reads=["ge"], writes=["ge"])
            T.op("dve", lambda e: e.tensor_reduce(out=gsum[:], in_=ge[:], axis=AX.X, op=ALU.add), reads=["ge"], writes=["gsum"])
            T.op("dve", lambda e: e.reciprocal(out=gw[:], in_=gsum[:]), reads=["gsum"], writes=["gw"])
            T.op("dve", lambda e: e.tensor_tensor(out=eg[:], in0=elv[:, :, 0, :], in1=bc(mg[:, :, 0], 8), op=ALU.mult), reads=lgk + ["mg"], writes=["eg"])
            for g in range(1, 4):
                T.op("dve", lambda e, g=g: e.tensor_tensor(out=tmp8[:], in0=elv[:, :, g, :], in1=bc(mg[:, :, g], 8), op=ALU.mult), reads=lgk + ["mg"], writes=["tmp8"])
                T.op("dve", lambda e: e.tensor_tensor(out=eg[:], in0=eg[:], in1=tmp8[:], op=ALU.add), reads=["eg", "tmp8"], writes=["eg"])
            T.op("dve", lambda e: e.tensor_reduce(out=m1[:], in_=eg[:], axis=AX.X, op=ALU.max), reads=["eg"], writes=["m1"])
            T.op("dve", lambda e: e.tensor_tensor(out=tmp8[:], in0=eg[:], in1=bc(m1[:], 8), op=ALU.is_equal), reads=["eg", "m1"], writes=["tmp8"])
            T.op("dve", lambda e: e.scalar_tensor_tensor(out=tmp8[:], in0=tmp8[:], scalar=-1.0e30, in1=eg[:], op0=ALU.mult, op1=ALU.add), reads=["tmp8", "eg"], writes=["tmp8"])
            T.op("dve", lambda e: e.tensor_reduce(out=m2[:], in_=tmp8[:], axis=AX.X, op=ALU.max), reads=["tmp8"], writes=["m2"])
            T.op("dve", lambda e: e.tensor_tensor(out=sel[:], in0=eg[:], in1=bc(m2[:], 8), op=ALU.is_ge), reads=["eg", "m2"], writes=["sel"])
            T.op("dve", lambda e: e.tensor_tensor(out=pe_[:], in0=eg[:], in1=bc(m1[:], 8), op=ALU.subtract), reads=["eg", "m1"], writes=["pe_"])
            T.op("act", lambda e: e.activation(out=pe_[:], in_=pe_[:], func=AF.Exp), reads=["pe_"], writes=["pe_"])
            T.op("dve", lambda e: e.tensor_tensor(out=pe_[:], in0=pe_[:], in1=sel[:], op=ALU.mult), reads=["pe_", "sel"], writes=["pe_"])
            T.op("dve", lambda e: e.tensor_reduce(out=psum_[:], in_=pe_[:], axis=AX.X, op=ALU.add), reads=["pe_"], writes=["psum_"])
            T.op("dve", lambda e: e.reciprocal(out=psum_[:], in_=psum_[:]), reads=["psum_"], writes=["psum_"])
            T.op("dve", lambda e: e.tensor_tensor(out=psum_[:], in0=psum_[:], in1=gw[:], op=ALU.mult), reads=["psum_", "gw"], writes=["psum_"])
            T.op("dve", lambda e: e.tensor_tensor(out=pe_[:], in0=pe_[:], in1=bc(psum_[:], 8), op=ALU.mult), reads=["pe_", "psum_"], writes=["pe_"])
            cv = comb[:].rearrange("p t (g e) -> p t g e", g=4)
            for g in range(4):
                T.op("dve", lambda e, g=g: e.tensor_tensor(out=cv[:, :, g, :], in0=pe_[:], in1=bc(mg[:, :, g], 8), op=ALU.mult), reads=["pe_", "mg"], writes=["comb"])
            if dbg:
                T.dma("sp", comb_s, comb[:], reads=["comb"], writes=["comb_s"])
            T.barrier()
            if stop == 5:
                T.finish_early()

        with ExitStack() as st:
            load_g(st, "g_fin", "d")
            TH = S // NHALF
            NS = TH // 128
            NBH = TH // 512
            n2T = SB(st, "n2T", [128, 8, TH], BF16)
            acc = SB(st, "acc", [128, NS, D], F32)
            stg = [SB(st, "stg%d" % i, [128, 8 * 256], F32) for i in range(3)]
            wgb = [SB(st, "wgb%d" % i, [128, 8, 256], BF16) for i in range(2)]
            wub = [SB(st, "wub%d" % i, [128, 8, 256], BF16) for i in range(2)]
            wdb = [SB(st, "wdb%d" % i, [128, 2, D], BF16) for i in range(2)]
            sg = Rot([SB(st, "sg%d" % i, [128, 512], F32) for i in range(2)], "sg")
            hid = Rot([SB(st, "hid%d" % i, [128, 2, 512], BF16) for i in range(2)], "hid")
            ot = Rot([SB(st, "ot%d" % i, [128, D], F32) for i in range(1)], "ot")
            jkp = Rot([SB(st, "djk", [128, D], BF16)], "djk")
            ssp = Rot([SB(st, "dss%d" % i, [128, 4], F32) for i in range(4)], "dss")
            psr = Rot(PS[0:8], "psd")
            n2T_v = n2T_s.rearrange("(k p) t -> p k t", p=128)
            for hf in range(NHALF):
                t0 = hf * TH
                for k in range(8):
                    T.dma("sp", n2T[:, k, :], n2T_v[:, k, t0:t0 + TH], writes=["n2T_k%d" % k])
                n2keys = ["n2T_k%d" % k for k in range(8)]
                def d_weights(ex):
                    b = ex % 2
                    T.dma("sp", stg[0][:].rearrange("p (k c) -> p k c", k=8), w_eg[ex].rearrange("(k p) c -> p k c", p=128), writes=["stg0"])
                    T.op("pool", lambda e, b=b: e.tensor_copy(out=wgb[b][:], in_=stg[0][:].rearrange("p (k c) -> p k c", k=8)), reads=["stg0"], writes=["wgb%d" % b])
                    T.dma("sp", stg[1][:].rearrange("p (k c) -> p k c", k=8), w_eu[ex].rearrange("(k p) c -> p k c", p=128), writes=["stg1"])
                    T.op("pool", lambda e, b=b: e.tensor_copy(out=wub[b][:], in_=stg[1][:].rearrange("p (k c) -> p k c", k=8)), reads=["stg1"], writes=["wub%d" % b])
                    T.dma("sp", stg[2][:].rearrange("p (k c) -> p k c", k=2), w_ed[ex].rearrange("(k p) c -> p k c", p=128), writes=["stg2"])
                    T.op("pool", lambda e, b=b: e.tensor_copy(out=wdb[b][:], in_=stg[2][:].rearrange("p (k c) -> p k c", k=2)), reads=["stg2"], writes=["wdb%d" % b])
                    if ex == 0:
                        for s_ in range(NS):
                            T.dma("sp", acc[:, s_, :], h_s[t0 + s_ * 128:t0 + (s_ + 1) * 128, :], writes=["acc%d_0" % s_, "acc%d_1" % s_])

                def d_up(ex, t):
                    b = ex % 2
                    hd, hdk = hid.next()
                    for oc in range(2):
                        pg, pgk = psr.next()
                        mm(pg[:, :], [(wgb[b][:, k, oc * 128:(oc + 1) * 128], n2T[:, k, t * 512:(t + 1) * 512]) for k in range(8)], reads=["wgb%d" % b] + n2keys, writes=[pgk])
                        pu, puk = psr.next()
                        mm(pu[:, :], [(wub[b][:, k, oc * 128:(oc + 1) * 128], n2T[:, k, t * 512:(t + 1) * 512]) for k in range(8)], reads=["wub%d" % b] + n2keys, writes=[puk])
                        s1, s1k = sg.next()
                        T.op("act", lambda e, s1=s1, pg=pg: e.activation(out=s1[:], in_=pg[:, :], func=AF.Silu), reads=[pgk], writes=[s1k])
                        T.op("dve", lambda e, s1=s1, pu=pu, hd=hd, oc=oc: e.tensor_tensor(out=hd[:, oc, :], in0=pu[:, :], in1=s1[:], op=ALU.mult), reads=[puk, s1k], writes=["%s_%d" % (hdk, oc)])
                    return hd, hdk

                def d_down(ex, t, hd, hdk):
                    b = ex % 2
                    for sub in range(4):
                        s_ = t * 4 + sub
                        ti = hf * NS + s_
                        for half in range(2):
                            pd, pdk = psr.next()
                            mm(pd[:, :], [(hd[:, jc, sub * 128:(sub + 1) * 128], wdb[b][:, jc, half * 512:(half + 1) * 512]) for jc in range(2)],
                               reads=["%s_0" % hdk, "%s_1" % hdk, "wdb%d" % b], writes=[pdk])
                            ak = "acc%d_%d" % (s_, half)
                            T.op("dve", lambda e, pd=pd, s_=s_, half=half, ti=ti, ex=ex: e.scalar_tensor_tensor(
                                out=acc[:, s_, half * 512:(half + 1) * 512], in0=pd[:, :], scalar=comb[:, ti, ex:ex + 1],
                                in1=acc[:, s_, half * 512:(half + 1) * 512], op0=ALU.mult, op1=ALU.add), reads=[pdk, "comb", ak], writes=[ak])

                items_d = [(ex, t) for ex in range(NE) for t in range(NBH)]
                d_weights(0)
                nxt_d = d_up(*items_d[0])
                for n, (ex, t) in enumerate(items_d):
                    cur_d = nxt_d
                    if n + 1 < len(items_d):
                        ex2, t2 = items_d[n + 1]
                        if t2 == 0:
                            d_weights(ex2)
                        nxt_d = d_up(ex2, t2)
                    d_down(ex, t, *cur_d)
                for s_ in range(NS):
                    aks = ["acc%d_0" % s_, "acc%d_1" % s_]
                    jk, jkk = jkp.next()
                    ss, sk = ssp.next()
                    T.op("act", lambda e, jk=jk, s_=s_, ss=ss: e.activation(out=jk[:], in_=acc[:, s_, :], func=AF.Square, accum_out=ss[:, 0:1]), reads=aks, writes=[sk, jkk])
                    T.op("dve", lambda e, ss=ss: e.tensor_scalar(out=ss[:, 1:2], in0=ss[:, 0:1], scalar1=1.0 / D, scalar2=EPS, op0=ALU.mult, op1=ALU.add), reads=[sk], writes=[sk])
                    T.op("pool", lambda e, ss=ss: e.tensor_tensor(out=ss[:, 2:3], in0=ss[:, 1:2], in1=mh[:, 0:1], op=ALU.pow), reads=[sk, "mh"], writes=[sk])
                    o_, ok = ot.next()
                    T.op("dve", lambda e, o_=o_, s_=s_, ss=ss: e.scalar_tensor_tensor(out=o_[:], in0=acc[:, s_, :], scalar=ss[:, 2:3], in1=gbc["g_fin"][:], op0=ALU.mult, op1=ALU.mult),
                         reads=aks + [sk, "bc_g_fin"], writes=[ok])
                    T.dma("sp", out[t0 + s_ * 128:t0 + (s_ + 1) * 128, :], o_[:], reads=[ok], writes=["out%d" % (t0 // 128 + s_)])

        with nc.Block() as block:
            T.finish(block)
    return nc


def _host_inputs(inputs):
    f = lambda a: np.ascontiguousarray(np.asarray(a))
    x = f(inputs["x"]); mem = f(inputs["mem"]); positions = f(inputs["positions"])
    B = x.shape[0]
    shared = {
        "g_mix": f(inputs["g_mix"])[0], "g_mem": f(inputs["g_mem"])[0], "g_ffn": f(inputs["g_ffn"])[0], "g_fin": f(inputs["g_final"]),
        "g_q": f(inputs["g_q"])[0], "g_kv": f(inputs["g_kv"])[0],
        "w_in": f(inputs["w_in"])[0],
        "cw": f(f(inputs["conv_w"])[0].T.reshape(4, 128, 4).transpose(1, 0, 2)),
        "cb": f(f(inputs["conv_b"])[0].reshape(4, 128).T),
        "lba": f(f(inputs["lru_ba"])[0].reshape(4, 128).T),
        "lbx": f(f(inputs["lru_bx"])[0].reshape(4, 128).T),
        "llam": f(f(inputs["lru_lambda"])[0].reshape(4, 128).T),
        "lwa": f(inputs["lru_wa"])[0], "lwx": f(inputs["lru_wx"])[0],
        "w_uq": f(inputs["w_uq"])[0], "w_ukv": f(inputs["w_ukv"])[0], "w_mkv": f(inputs["w_mem_kv"])[0],
        "w_br": f(inputs["w_branch"])[0], "w_o": f(inputs["w_o"])[0],
        "w_gr": f(np.concatenate([f(inputs["w_group"])[0], f(inputs["w_router"])[0]], axis=1)),
        "b_gr": f(np.concatenate([f(inputs["b_group"])[0], f(inputs["b_router"])[0]], axis=0)),
        "w_eg": f(inputs["w_e_gate"])[0], "w_eu": f(inputs["w_e_up"])[0], "w_ed": f(inputs["w_e_down"])[0],
    }
    ident = np.eye(128, dtype=np.float32)
    kk = np.arange(128)[:, None]; qq = np.arange(512)[None, :]
    cmask = np.stack([((128 * r + kk) <= qq).astype(np.float32) for r in range(4)], axis=0)
    invf = np.broadcast_to((10000.0 ** (-np.arange(0, 32, 2, dtype=np.float32) / 32.0)).astype(np.float32)[None, :], (128, 16)).copy()
    shared.update({"ident": ident, "cmask": cmask, "invf": invf})
    shared = {k: np.ascontiguousarray(v, dtype=np.float32) for k, v in shared.items()}
    maps = []
    for b in range(B):
        m = dict(shared)
        m["x"] = x[b]
        m["mem"] = mem[b]
        m["pos"] = np.ascontiguousarray(positions[b].reshape(NT, 128).T.astype(np.int32))
        maps.append(m)
    return maps


_NC_CACHE = {}


def kernel(**inputs):
    maps = _host_inputs(inputs)
    if "nc" not in _NC_CACHE:
        _NC_CACHE["nc"] = build_nc(False)
    nc = _NC_CACHE["nc"]
    res = run_bass_kernel_spmd(nc, maps, core_ids=list(range(len(maps))))
    return np.stack([np.asarray(r["out"], dtype=np.float32) for r in res.results], axis=0)
```
